# Optimizing a Trainium2 kernel written in Bass

```python
import math
import jax, jax.numpy as jnp
from jax import lax
import numpy as np

D_MODEL = 1024
BATCH = 16
SEQ = 2048
DEPTH = 4

D_MIX = D_MODEL
D_NSA = D_MIX // 2
D_CONV = D_MIX - D_NSA
HEAD_DIM = 64
N_HEADS = D_NSA // HEAD_DIM
N_KV = 2
Q_PER_KV = N_HEADS // N_KV
D_KV = N_KV * HEAD_DIM
N_BRANCH = 3
L_CMP = 32
CMP_STRIDE = 16
L_SEL = 64
N_SEL = 8
WINDOW = 512
Q_BLOCK = 128
SEL_Q_BLOCK = 64
CONV_WIDTH = 31
NUM_BUCKETS = 32
MAX_DISTANCE = 128
D_IN = D_NSA + 6 * D_KV + N_HEADS * N_BRANCH + 2 * D_CONV
D_FF = 2816
N_EXPERTS = 8
TOP_K = 2
D_FF_EXPERT = 3584
N_DENSE = DEPTH - DEPTH // 2
N_MOE = DEPTH // 2
DEEPNORM_ALPHA = (2 * DEPTH) ** 0.25
DEEPNORM_BETA = (8 * DEPTH) ** -0.25
LN_EPS = 1e-5
NEG_INF = -1e30

kernel_name = 'hybrid_nsa_conformer_moe_trunk'


def layer_norm(x, g, b):
    xf = x.astype(jnp.float32)
    mu = jnp.mean(xf, axis=-1, keepdims=True)
    var = jnp.mean(jnp.square(xf - mu), axis=-1, keepdims=True)
    y = (xf - mu) * lax.rsqrt(var + LN_EPS)
    return (y * g.astype(jnp.float32) + b.astype(jnp.float32)).astype(x.dtype)


def masked_softmax(logits, valid):
    p = jax.nn.softmax(jnp.where(valid, logits, NEG_INF), axis=-1)
    return jnp.where(valid, p, 0.0)


def rel_bucket(dist):
    n = jnp.maximum(dist, 0)
    max_exact = NUM_BUCKETS // 2
    nf = jnp.maximum(n, 1).astype(jnp.float32)
    large = max_exact + (jnp.log(nf / max_exact) / math.log(MAX_DISTANCE / max_exact)
                         * (NUM_BUCKETS - max_exact)).astype(jnp.int32)
    large = jnp.minimum(large, NUM_BUCKETS - 1)
    return jnp.where(n < max_exact, n, large)


def compress(k, pos, w1, w2):
    B, T = k.shape[:2]
    n_cmp = (T - L_CMP) // CMP_STRIDE + 1
    idx = CMP_STRIDE * np.arange(n_cmp)[:, None] + np.arange(L_CMP)[None, :]
    blk = k[:, idx] + pos[None, None, :, None, :]
    blk = blk.transpose(0, 1, 3, 2, 4).reshape(B, n_cmp, N_KV, L_CMP * HEAD_DIM)
    return jax.nn.gelu(blk @ w1) @ w2


def compressed_branch(q5, kc, vc, rel_bias):
    T = q5.shape[1]
    n_cmp = kc.shape[1]
    starts = CMP_STRIDE * np.arange(n_cmp)
    ends = starts + L_CMP - 1
    dist = jnp.asarray(np.arange(T)[:, None] - ends[None, :], dtype=jnp.int32)
    valid = dist >= 0
    bias = rel_bias[rel_bucket(dist)].reshape(T, n_cmp, N_KV, Q_PER_KV).transpose(2, 3, 0, 1)
    logits = jnp.einsum('btgrd,bngd->bgrtn', q5, kc).astype(jnp.float32) + bias.astype(jnp.float32)
    p = masked_softmax(logits, valid)
    o = jnp.einsum('bgrtn,bngd->btgrd', p.astype(vc.dtype), vc)
    nb = T // L_SEL
    bstart = L_SEL * np.arange(nb)
    overlap = ((starts[:, None] < bstart[None, :] + L_SEL)
               & (starts[:, None] + L_CMP > bstart[None, :])).astype(np.float32)
    imp = jnp.einsum('bgrtn,nj->bgtj', p, jnp.asarray(overlap))
    return o, imp


def selected_branch(q5, ks, vs, imp, rel_bias):
    B, T = q5.shape[:2]
    nb = T // L_SEL
    n_sel = min(N_SEL, nb)
    tb = np.arange(T) // L_SEL
    j = np.arange(nb)
    future = jnp.asarray(j[None, :] > tb[:, None])
    forced = jnp.asarray((j[None, :] == 0) | (j[None, :] == tb[:, None]) | (j[None, :] == tb[:, None] - 1))
    score = jnp.where(future, -jnp.inf, jnp.where(forced, jnp.inf, imp))
    _, idx = lax.top_k(score, n_sel)

    def to_blocks(a):
        return a.reshape(B, nb, L_SEL, N_KV, HEAD_DIM).transpose(0, 3, 1, 2, 4).reshape(B, N_KV, nb, L_SEL * HEAD_DIM)

    kb, vb = to_blocks(ks), to_blocks(vs)
    nc = T // SEL_Q_BLOCK
    qc = q5.reshape(B, nc, SEL_Q_BLOCK, N_KV, Q_PER_KV, HEAD_DIM).transpose(1, 0, 2, 3, 4, 5)
    ic = idx.reshape(B, N_KV, nc, SEL_Q_BLOCK, n_sel).transpose(2, 0, 1, 3, 4)
    t0s = jnp.arange(nc, dtype=jnp.int32) * SEL_Q_BLOCK
    rel_g = rel_bias.reshape(NUM_BUCKETS, N_KV, Q_PER_KV)
    b_ix = jnp.arange(B)[:, None, None]
    g_ix = jnp.arange(N_KV)[None, :, None]
    g_ix5 = jnp.arange(N_KV)[None, :, None, None, None]

    def chunk(args):
        qb, ib, t0 = args
        tq = t0 + jnp.arange(SEL_Q_BLOCK)
        flat = ib.reshape(B, N_KV, SEL_Q_BLOCK * n_sel)
        kg = kb[b_ix, g_ix, flat].reshape(B, N_KV, SEL_Q_BLOCK, n_sel, L_SEL, HEAD_DIM)
        vg = vb[b_ix, g_ix, flat].reshape(B, N_KV, SEL_Q_BLOCK, n_sel, L_SEL, HEAD_DIM)
        pos = ib[..., None] * L_SEL + jnp.arange(L_SEL)
        dist = tq[None, None, :, None, None] - pos
        bias = jnp.moveaxis(rel_g[rel_bucket(dist), g_ix5], -1, 3)
        logits = jnp.einsum('bqgrd,bgqskd->bgqrsk', qb, kg).astype(jnp.float32) + bias.astype(jnp.float32)
        logits = logits.reshape(B, N_KV, SEL_Q_BLOCK, Q_PER_KV, n_sel * L_SEL)
        valid = (dist >= 0).reshape(B, N_KV, SEL_Q_BLOCK, 1, n_sel * L_SEL)
        p = masked_softmax(logits, valid).reshape(B, N_KV, SEL_Q_BLOCK, Q_PER_KV, n_sel, L_SEL)
        return jnp.einsum('bgqrsk,bgqskd->bqgrd', p.astype(vg.dtype), vg)

    o = lax.map(chunk, (qc, ic, t0s))
    return o.transpose(1, 0, 2, 3, 4, 5).reshape(B, T, N_KV, Q_PER_KV, HEAD_DIM)


def window_branch(q5, kw, vw, rel_bias):
    B, T = q5.shape[:2]
    nqb = T // Q_BLOCK
    kp = jnp.pad(kw, ((0, 0), (WINDOW, 0), (0, 0), (0, 0)))
    vp = jnp.pad(vw, ((0, 0), (WINDOW, 0), (0, 0), (0, 0)))
    qw = q5.reshape(B, nqb, Q_BLOCK, N_KV, Q_PER_KV, HEAD_DIM).transpose(1, 0, 2, 3, 4, 5)
    t0s = jnp.arange(nqb, dtype=jnp.int32) * Q_BLOCK

    def block(args):
        qb, t0 = args
        kband = lax.dynamic_slice_in_dim(kp, t0, Q_BLOCK + WINDOW, axis=1)
        vband = lax.dynamic_slice_in_dim(vp, t0, Q_BLOCK + WINDOW, axis=1)
        tq = t0 + jnp.arange(Q_BLOCK)
        pos = t0 - WINDOW + jnp.arange(Q_BLOCK + WINDOW)
        dist = tq[:, None] - pos[None, :]
        valid = (dist >= 0) & (dist < WINDOW) & (pos[None, :] >= 0)
        bias = rel_bias[rel_bucket(dist)].reshape(Q_BLOCK, Q_BLOCK + WINDOW, N_KV, Q_PER_KV).transpose(2, 3, 0, 1)
        logits = jnp.einsum('bqgrd,bkgd->bgrqk', qb, kband).astype(jnp.float32) + bias.astype(jnp.float32)
        p = masked_softmax(logits, valid)
        return jnp.einsum('bgrqk,bkgd->bqgrd', p.astype(vband.dtype), vband)

    o = lax.map(block, (qw, t0s))
    return o.transpose(1, 0, 2, 3, 4, 5).reshape(B, T, N_KV, Q_PER_KV, HEAD_DIM)


def conformer_conv(u, w, b, g, beta):
    a, gt = jnp.split(u, 2, axis=-1)
    y = a * jax.nn.sigmoid(gt)
    y = lax.conv_general_dilated(y, w[:, None, :], window_strides=(1,), padding=[(CONV_WIDTH - 1, 0)],
                                 dimension_numbers=('NWC', 'WIO', 'NWC'), feature_group_count=D_CONV) + b
    return jax.nn.silu(layer_norm(y, g, beta))


def hybrid_mixer(h, w_in, cmp_pos, cmp_w1, cmp_w2, conv_w, conv_b, conv_ln_g, conv_ln_b, w_out, rel_bias):
    B, T, _ = h.shape
    u = h @ w_in
    splits = [int(s) for s in np.cumsum([D_NSA] + [D_KV] * 6 + [N_HEADS * N_BRANCH])]
    q, kc, vc, ks, vs, kw, vw, gl, conv_in = jnp.split(u, splits, axis=-1)

    def kv(a):
        return a.reshape(B, T, N_KV, HEAD_DIM)

    q5 = q.reshape(B, T, N_KV, Q_PER_KV, HEAD_DIM) * (HEAD_DIM ** -0.5)
    kcmp = compress(kv(kc), cmp_pos[0], cmp_w1[0], cmp_w2[0])
    vcmp = compress(kv(vc), cmp_pos[1], cmp_w1[1], cmp_w2[1])
    o_cmp, imp = compressed_branch(q5, kcmp, vcmp, rel_bias)
    o_slc = selected_branch(q5, kv(ks), kv(vs), imp, rel_bias)
    o_win = window_branch(q5, kv(kw), kv(vw), rel_bias)
    g = jax.nn.sigmoid(gl.reshape(B, T, N_KV, Q_PER_KV, N_BRANCH))
    o_nsa = g[..., 0:1] * o_cmp + g[..., 1:2] * o_slc + g[..., 2:3] * o_win
    o_conv = conformer_conv(conv_in, conv_w, conv_b, conv_ln_g, conv_ln_b)
    return jnp.concatenate([o_nsa.reshape(B, T, D_NSA), o_conv], axis=-1) @ w_out


def swiglu(h, w1, w2):
    gate, up = jnp.split(h @ w1, 2, axis=-1)
    return (jax.nn.silu(gate) * up) @ w2


def moe_ffn(h, router_w, w1, w2):
    B, T, D = h.shape
    hf = h.reshape(B * T, D)
    logits = (hf @ router_w).astype(jnp.float32)
    top_vals, top_idx = lax.top_k(logits, TOP_K)
    wts = jax.nn.softmax(top_vals, axis=-1)
    out = jnp.zeros_like(hf)
    for e in range(N_EXPERTS):
        we = jnp.sum(jnp.where(top_idx == e, wts, 0.0), axis=-1).astype(hf.dtype)
        out = out + we[:, None] * swiglu(hf, w1[e], w2[e])
    return out.reshape(B, T, D)


def setup_inputs(seed: int = 0) -> dict:
    key = jax.random.key(seed)
    ks = jax.random.split(key, 24)
    nrm = lambda k, s, sc: jax.random.normal(k, s, jnp.float32) * sc
    return {
        'x': nrm(ks[0], (BATCH, SEQ, D_MODEL), 1.0),
        'c': nrm(ks[1], (BATCH, D_MODEL), 1.0),
        'w_in': nrm(ks[2], (DEPTH, D_MODEL, D_IN), D_MODEL ** -0.5),
        'cmp_pos': nrm(ks[3], (DEPTH, 2, L_CMP, HEAD_DIM), 0.1),
        'cmp_w1': nrm(ks[4], (DEPTH, 2, L_CMP * HEAD_DIM, HEAD_DIM), (L_CMP * HEAD_DIM) ** -0.5),
        'cmp_w2': nrm(ks[5], (DEPTH, 2, HEAD_DIM, HEAD_DIM), HEAD_DIM ** -0.5),
        'conv_w': nrm(ks[6], (DEPTH, CONV_WIDTH, D_CONV), CONV_WIDTH ** -0.5),
        'conv_b': nrm(ks[7], (DEPTH, D_CONV), 0.02),
        'conv_ln_g': 1.0 + nrm(ks[8], (DEPTH, D_CONV), 0.02),
        'conv_ln_b': nrm(ks[9], (DEPTH, D_CONV), 0.02),
        'w_out': nrm(ks[10], (DEPTH, D_MIX, D_MODEL), D_MIX ** -0.5 * DEEPNORM_BETA),
        'rel_bias': nrm(ks[11], (NUM_BUCKETS, N_HEADS), 0.5),
        'ada_w': nrm(ks[12], (DEPTH, D_MODEL, 6 * D_MODEL), 0.2 * D_MODEL ** -0.5),
        'ada_b': nrm(ks[13], (DEPTH, 6 * D_MODEL), 0.02),
        'ln_g': 1.0 + nrm(ks[14], (DEPTH, 2, D_MODEL), 0.02),
        'ln_b': nrm(ks[15], (DEPTH, 2, D_MODEL), 0.02),
        'ffn_w1': nrm(ks[16], (N_DENSE, D_MODEL, 2 * D_FF), D_MODEL ** -0.5),
        'ffn_w2': nrm(ks[17], (N_DENSE, D_FF, D_MODEL), D_FF ** -0.5 * DEEPNORM_BETA),
        'router_w': nrm(ks[18], (N_MOE, D_MODEL, N_EXPERTS), D_MODEL ** -0.5),
        'moe_w1': nrm(ks[19], (N_MOE, N_EXPERTS, D_MODEL, 2 * D_FF_EXPERT), D_MODEL ** -0.5),
        'moe_w2': nrm(ks[20], (N_MOE, N_EXPERTS, D_FF_EXPERT, D_MODEL), D_FF_EXPERT ** -0.5 * DEEPNORM_BETA),
    }


def reference(x, c, w_in, cmp_pos, cmp_w1, cmp_w2, conv_w, conv_b, conv_ln_g, conv_ln_b, w_out, rel_bias,
              ada_w, ada_b, ln_g, ln_b, ffn_w1, ffn_w2, router_w, moe_w1, moe_w2):
    sc = jax.nn.silu(c)
    for l in range(DEPTH):
        mod = sc @ ada_w[l] + ada_b[l]
        shift1, scale1, gate1, shift2, scale2, gate2 = [m[:, None, :] for m in jnp.split(mod, 6, axis=-1)]
        h = x * (1.0 + scale1) + shift1
        y = hybrid_mixer(h, w_in[l], cmp_pos[l], cmp_w1[l], cmp_w2[l], conv_w[l], conv_b[l],
                         conv_ln_g[l], conv_ln_b[l], w_out[l], rel_bias)
        x = layer_norm(DEEPNORM_ALPHA * x + (1.0 + gate1) * y, ln_g[l, 0], ln_b[l, 0])
        h = x * (1.0 + scale2) + shift2
        if l % 2 == 0:
            f = swiglu(h, ffn_w1[l // 2], ffn_w2[l // 2])
        else:
            f = moe_ffn(h, router_w[l // 2], moe_w1[l // 2], moe_w2[l // 2])
        x = layer_norm(DEEPNORM_ALPHA * x + (1.0 + gate2) * f, ln_g[l, 1], ln_b[l, 1])
    return x
```

```python
import math
import contextlib
import types
import numpy as np
import concourse.bass as bass
import concourse.mybir as mybir
from concourse.bass_utils import run_bass_kernel_spmd

F32 = mybir.dt.float32
BF16 = mybir.dt.bfloat16
ALU = mybir.AluOpType
AF = mybir.ActivationFunctionType

D = 1024
T = 2048
DEPTH = 4
NH = 8
HD = 64
D_IN = 2328
D_FF = 2816
D_FFE = 3584
NE = 8
NCMP = 127
ALPHA = (2 * DEPTH) ** 0.25
LN_EPS = 1e-5
NEG = -30000.0
CHUNK_ELEMS = 4096 * 1024
ORDER = [0, 1, 4, 5, 2, 3, 6, 7]
EPOCH = 16000
SEG_CH = 28
SEG_ELEMS = SEG_CH * CHUNK_ELEMS


class Buf:
    __slots__ = ("name", "w", "wf", "r", "sem", "semcnt", "excl")

    def __init__(self, name, excl=False):
        self.name = name
        self.excl = excl
        self.w = {}
        self.wf = {}
        self.r = {}
        self.sem = None
        self.semcnt = 0


def _snap(fn):
    if fn is None or fn.__closure__ is None:
        return fn
    cells = []
    for c in fn.__closure__:
        try:
            cells.append(types.CellType(c.cell_contents))
        except ValueError:
            cells.append(c)
    g = types.FunctionType(fn.__code__, fn.__globals__, fn.__name__, fn.__defaults__, tuple(cells))
    g.__kwdefaults__ = fn.__kwdefaults__
    return g


class Prog:
    def __init__(self, nc):
        self.nc = nc
        self.stack = contextlib.ExitStack()
        self.engnames = ["pe", "act", "dve", "pool", "sp"]
        self.streams = {k: [] for k in self.engnames}
        self.count = {k: 0 for k in self.engnames}
        self.esems = {k: [] for k in self.engnames}
        self.seen = {k: {} for k in self.engnames}
        self.dmabufs = []
        self.nobarrier = set()
        self.nsem = 0

    def new_sem(self, name):
        self.nsem += 1
        return self.stack.enter_context(self.nc.semaphore(name))

    def _esem(self, k, epoch):
        while len(self.esems[k]) <= epoch:
            self.esems[k].append(self.new_sem(f"e_{k}_{len(self.esems[k])}"))
        return self.esems[k][epoch]

    def _need(self, k, wt, waits):
        if wt[0] == "e":
            _, e2, c = wt
            if e2 == "pe" and k == "pe":
                return
            key = e2
        else:
            _, b, c = wt
            key = ("d", id(b))
        if self.seen[k].get(key, 0) >= c:
            return
        self.seen[k][key] = c
        waits.append(wt)

    def _deps(self, k, reads, writes, pwrites):
        waits = []
        for b in reads:
            for wt in b.w.values():
                self._need(k, wt, waits)
            if b.excl:
                for kk, wt in b.r.items():
                    if kk != k:
                        self._need(k, wt, waits)
        for b in writes:
            for wt in b.w.values():
                self._need(k, wt, waits)
            for wt in b.r.values():
                self._need(k, wt, waits)
        for b in pwrites:
            for wt in b.r.values():
                self._need(k, wt, waits)
            for wt in b.wf.values():
                self._need(k, wt, waits)
        return waits

    def _record(self, key, me, reads, writes, pwrites):
        for b in reads:
            b.r[key] = me
        for b in writes:
            b.w = {key: me}
            b.wf = {key: me}
            b.r = {}
        for b in pwrites:
            if b.r:
                b.w = {key: me}
                b.wf = {}
                b.r = {}
            else:
                b.w[key] = me

    def op(self, k, fn, reads=(), writes=(), pwrites=(), extra=()):
        waits = self._deps(k, reads, writes, pwrites)
        for wt in extra:
            self._need(k, wt, waits)
        self.count[k] += 1
        me = ("e", k, self.count[k])
        self._record(k, me, reads, writes, pwrites)
        self.streams[k].append((waits, _snap(fn), me))

    def dma(self, q, out_ap, in_ap, reads=(), writes=(), pwrites=(), owner=None, extra=(), fn=None, inc=16, **kw):
        if owner is None:
            owner = (list(writes) + list(pwrites))[0]
        if owner.sem is None:
            owner.sem = self.new_sem("d_" + owner.name)
            self.dmabufs.append(owner)
        waits = self._deps(q, reads, writes, pwrites)
        for wt in extra:
            self._need(q, wt, waits)
        owner.semcnt += inc
        me = ("d", owner, owner.semcnt)
        self._record(("d", id(owner)), me, reads, writes, pwrites)
        if fn is None:
            fn = lambda eng, o=out_ap, i=in_ap, kw=kw: eng.dma_start(out=o, in_=i, **kw)
        self.streams[q].append((waits, _snap(fn), ("dinc", owner, inc)))

    def barrier(self):
        snap = dict(self.count)
        dsn = [(b, b.semcnt) for b in self.dmabufs if b.name not in self.nobarrier]
        for k in self.engnames:
            waits = []
            for e2 in self.engnames:
                if e2 != k and snap[e2] > 0:
                    self._need(k, ("e", e2, snap[e2]), waits)
            for b, c in dsn:
                if c > 0:
                    self._need(k, ("d", b, c), waits)
            self.streams[k].append((waits, None, None))

    def wait_all(self, k, bufs):
        waits = self._deps(k, bufs, (), ())
        self.streams[k].append((waits, None, None))

    def emit(self):
        nc = self.nc
        engattr = {"pe": "tensor", "act": "scalar", "dve": "vector", "pool": "gpsimd", "sp": "sync"}
        for k in self.engnames:
            self._esem(k, max(self.count[k] - 1, 0) // EPOCH)
        with nc.Block() as block:
            for k in self.engnames:
                stream = self.streams[k]

                def body(eng, k=k, stream=stream):
                    for waits, fn, inc in stream:
                        for wt in waits:
                            if wt[0] == "e":
                                ep, cc = divmod(wt[2] - 1, EPOCH)
                                eng.wait_ge(self.esems[wt[1]][ep], cc + 1)
                            else:
                                eng.wait_ge(wt[1].sem, wt[2])
                        if fn is None:
                            continue
                        ins = fn(eng)
                        if inc[0] == "e":
                            ep, cc = divmod(inc[2] - 1, EPOCH)
                            ins.then_inc(self.esems[k][ep], 1)
                        else:
                            ins.then_inc(inc[1].sem, inc[2])

                getattr(block, engattr[k])(body)
        self.stack.close()


def flat_layout(layers):
    off = 0
    lay = {}

    def add(name, n):
        nonlocal off
        if (off // SEG_ELEMS) != ((off + n - 1) // SEG_ELEMS):
            off = ((off + n - 1) // SEG_ELEMS) * SEG_ELEMS
        lay[name] = off
        off += n

    for l in layers:
        add(("ada", l), D * 6 * D)
        add(("w_in", l), D * D_IN)
        add(("cmp_w1", l), 2 * 2048 * 64)
        add(("w_out", l), D * D)
        if l % 2 == 0:
            add(("ffn1", l), D * 2 * D_FF)
            add(("ffn2", l), D_FF * D)
        else:
            for e in range(NE):
                add(("moe1", l, e), D * 2 * D_FFE)
                add(("moe2", l, e), D_FFE * D)
    nch = (off + CHUNK_ELEMS - 1) // CHUNK_ELEMS
    return lay, off, nch


def w_in_perm():
    cols = []
    for fo in range(4):
        cols += list(range(fo * 64, fo * 64 + 64)) + list(range((4 + fo) * 64, (4 + fo) * 64 + 64))
    q_end = 512
    kc, vc, ks, vs, kw, vw = [q_end + i * 128 for i in range(6)]
    gl = q_end + 6 * 128
    cv = gl + 24
    cols += list(range(kc, kc + 128)) + list(range(vc, vc + 128)) + list(range(ks, ks + 128)) + list(range(kw, kw + 128))
    cols += list(range(cv, cv + 1024))
    cols += list(range(vs, vs + 128)) + list(range(vw, vw + 128)) + list(range(gl, gl + 24))
    assert len(cols) == D_IN
    return np.asarray(cols)


def pack_flat(inp, layers):
    lay, total, nch = flat_layout(layers)
    flat = np.zeros(nch * CHUNK_ELEMS, np.float32)

    def put(key, arr):
        a = np.ascontiguousarray(arr, dtype=np.float32).reshape(-1)
        flat[lay[key]:lay[key] + a.size] = a

    perm = w_in_perm()
    for l in layers:
        put(("ada", l), inp["ada_w"][l])
        put(("w_in", l), np.asarray(inp["w_in"][l])[:, perm])
        put(("cmp_w1", l), inp["cmp_w1"][l])
        put(("w_out", l), inp["w_out"][l])
        if l % 2 == 0:
            put(("ffn1", l), inp["ffn_w1"][l // 2])
            put(("ffn2", l), inp["ffn_w2"][l // 2])
        else:
            for e in range(NE):
                put(("moe1", l, e), inp["moe_w1"][l // 2, e])
                put(("moe2", l, e), inp["moe_w2"][l // 2, e])
    return flat.reshape(nch, 8, 512, 1024), nch


def rel_bucket_np(dist):
    n = np.maximum(dist, 0)
    nf = np.maximum(n, 1).astype(np.float32)
    large = 16 + (np.log(nf / np.float32(16)) / np.float32(math.log(128 / 16)) * np.float32(16)).astype(np.int32)
    large = np.minimum(large, 31)
    return np.where(n < 16, n, large)


def host_consts():
    c = {}
    n = np.arange(NCMP)[:, None]
    q = np.arange(T)[None, :]
    dist = q - (16 * n + 31)
    c["bk_c"] = np.where(dist >= 0, rel_bucket_np(dist), 32).astype(np.float32)
    k = np.arange(128)[:, None]
    qq = np.arange(128)[None, :]
    d0 = qq - k
    c["bk_d0"] = np.where(d0 >= 0, rel_bucket_np(d0), 32).astype(np.float32)
    c["bk_d1"] = rel_bucket_np(128 + qq - k).astype(np.float32)
    c["edge"] = np.where(qq >= k, NEG, 0.0).astype(np.float32)
    j = np.arange(32)[:, None]
    kk = np.arange(T)[None, :]
    c["emat"] = (kk // 64 == j).astype(np.float32)
    tb = (np.arange(T) // 64)[:, None]
    jj = np.arange(32)[None, :]
    fm = np.where(jj > tb, -1e9, np.where((jj == 0) | (jj == tb) | (jj == tb - 1), 1e9, 0.0))
    c["fm"] = fm.astype(np.float32)
    starts = 16 * np.arange(NCMP)[:, None]
    bstart = 64 * np.arange(32)[None, :]
    c["ov"] = ((starts < bstart + 64) & (starts + 32 > bstart)).astype(np.float32)
    c["ident"] = np.eye(128, dtype=np.float32)
    return c
SB_BASE = 16512
SB_END = 229376


def build(cfg):
    NSEQ = cfg["nseq"]
    layers = cfg["layers"]
    gather = cfg["gather"]
    lay, total, NCH = flat_layout(layers)
    nc = bass.Bass("TRN2", target_bir_lowering=False)
    P = Prog(nc)
    NT = NSEQ * T

    def dram_in(name, shape, dt=F32):
        return nc.dram_tensor(name, list(shape), dt, kind="ExternalInput").ap()

    x_in = dram_in("x", [NT, D])
    c_in = dram_in("c", [NSEQ, D])
    if gather:
        wsh = dram_in("wsh", [NCH * 512, 1024])
    else:
        wfull = dram_in("wfull", [NCH * 4096, 1024])
    cmp_pos = dram_in("cmp_pos", [4, 2, 32, 64])
    cmp_w2 = dram_in("cmp_w2", [4, 2, 64, 64])
    conv_w = dram_in("conv_w", [4, 31, 512])
    conv_b = dram_in("conv_b", [4, 512])
    conv_g = dram_in("conv_ln_g", [4, 512])
    conv_be = dram_in("conv_ln_b", [4, 512])
    rel_bias = dram_in("rel_bias", [1, 256])
    ada_b = dram_in("ada_b", [4, 6144])
    ln_g = dram_in("ln_g", [4, 2, 1024])
    ln_b = dram_in("ln_b", [4, 2, 1024])
    router_w = dram_in("router_w", [2, 1024, 8])
    bk_c = dram_in("bk_c", [NCMP, T])
    bk_d0 = dram_in("bk_d0", [128, 128])
    bk_d1 = dram_in("bk_d1", [128, 128])
    edge_in = dram_in("edge", [128, 128])
    emat_in = dram_in("emat", [32, T])
    fm_in = dram_in("fm", [T, 32])
    ov_in = dram_in("ov", [NCMP, 32])
    ident_in = dram_in("ident", [128, 128])
    out = nc.dram_tensor("out", [NT, D], F32, kind="ExternalOutput").ap()

    NSEG = (NCH + SEG_CH - 1) // SEG_CH
    wflat_t = [nc.dram_tensor(f"wflat{i}", [min(SEG_CH, NCH - i * SEG_CH) * 4096, 1024], BF16).ap() for i in range(NSEG)]
    xa = nc.dram_tensor("xa", [NT, D], F32).ap()
    xb = nc.dram_tensor("xb", [NT, D], F32).ap()
    modrow = nc.dram_tensor("modrow", [len(layers) * NSEQ, 6144], F32).ap()
    bc_dram = nc.dram_tensor("bc_dram", [8 * 128, T], BF16).ap()
    b_xa, b_xb, b_out, b_modrow, b_bc = Buf("xa"), Buf("xb"), Buf("outd"), Buf("modrow"), Buf("bcd")

    b_wflat = Buf("wflat")
    b_cc = Buf("cc")
    chunk_ready = {}
    P.nobarrier.update(["cc", "ib0", "wflat"])
    if gather:
        ib = nc.dram_tensor("ib", [NCH * 512, 1024], BF16).ap()
        ob1 = nc.dram_tensor("ob1", [NCH * 2048, 1024], BF16).ap()
        NP_ = (NCH * 512 + 2047) // 2048
        b_ibp = [Buf(f"ib{i}") for i in range(NP_)]
        cast_done = set()

        def cast(pi):
            if pi in cast_done or pi >= NP_:
                return
            cast_done.add(pi)
            i = pi * 2048
            j = min(i + 2048, NCH * 512)
            P.dma("pool", ib[i:j, :], wsh[i:j, :], writes=[b_ibp[pi]], owner=b_ibp[0], max_dma_last_dim=4096)

        s1idx = {}

        def s1(ch):
            cast(ch // 4)
            fn = lambda eng, ch=ch: eng.collective_compute(
                "AllGather", ALU.bypass, replica_groups=[[0, 1, 2, 3], [4, 5, 6, 7]],
                ins=[ib[ch * 512:(ch + 1) * 512, :]], outs=[ob1[ch * 2048:(ch + 1) * 2048, :]])
            P.dma("pool", None, None, reads=[b_ibp[ch // 4]], writes=[], owner=b_cc, fn=fn, inc=1)
            s1idx[ch] = b_cc.semcnt

        def s2(ch, hf):
            fn = lambda eng, ch=ch, hf=hf: eng.collective_compute(
                "AllGather", ALU.bypass, replica_groups=[[0, 4], [1, 5], [2, 6], [3, 7]],
                ins=[ob1[ch * 2048 + hf * 1024:ch * 2048 + (hf + 1) * 1024, :]],
                outs=[wflat_t[ch // SEG_CH][(2 * (ch % SEG_CH) + hf) * 2048:(2 * (ch % SEG_CH) + hf + 1) * 2048, :]])
            P.dma("pool", None, None, reads=[], writes=[], owner=b_cc, fn=fn, inc=1,
                  extra=[("d", b_cc, s1idx[ch])])

        s1(0)
        if NCH > 1:
            s1(1)
        for ch in range(NCH):
            s2(ch, 0)
            s2(ch, 1)
            chunk_ready[ch] = b_cc.semcnt
            if ch + 2 < NCH:
                s1(ch + 2)
            cast((ch + 6) // 4)
    else:
        for i in range(0, NCH * 4096, 2048):
            sg, li_ = divmod(i, SEG_CH * 4096)
            P.dma("pool", wflat_t[sg][li_:li_ + 2048, :], wfull[i:i + 2048, :], pwrites=[b_wflat], max_dma_last_dim=4096)

    flat1 = [w_.rearrange("r c -> (r c)") for w_ in wflat_t]

    def wview(key, K, N, sub=0):
        sg, o = divmod(lay[key] + sub, SEG_ELEMS)
        return flat1[sg][o:o + K * N].rearrange("(k n) -> k n", n=N)

    def wdep(key, nelem):
        if gather:
            ch = (lay[key] + nelem - 1) // CHUNK_ELEMS
            return dict(extra=[("d", b_cc, chunk_ready[ch])])
        return dict(reads=[b_wflat])

    _cache = {}
    _bufs = {}

    def getbuf(name):
        if name not in _bufs:
            _bufs[name] = Buf(name)
        return _bufs[name]

    class Arena:
        def __init__(self, base, end):
            self.base, self.off, self.end = base, base, end

        def alloc(self, name, shape, dt):
            esz = 4 if dt == F32 else 2
            n = esz
            for s in shape[1:]:
                n *= s
            n = (n + 63) // 64 * 64
            assert self.off + n <= self.end, (name, self.off, n, self.end)
            if name in _cache:
                ap_, off_ = _cache[name]
                assert off_ == self.off, (name, off_, self.off)
            else:
                ap_ = nc.alloc_sbuf_tensor_at(name, list(shape), dt, offset=self.off).ap()
                _cache[name] = (ap_, self.off)
            self.off += n
            return ap_, getbuf(name)

    AP_ = Arena(SB_BASE, SB_END)
    al = AP_.alloc
    ident_f, b_identf = al("ident_f", [128, 128], F32)
    ident_b, b_identb = al("ident_b", [128, 128], BF16)
    onesm, b_onesm = al("onesm", [128, 128], F32)
    emat_b, b_emat = al("emat_b", [128, T], BF16)
    fm_sb, b_fm = al("fm_sb", [128, 16, 32], F32)
    edge_b, b_edge = al("edge_b", [128, 128], BF16)
    bt0, b_bt0 = al("bt0", [128, 8, 128], BF16)
    bt1, b_bt1 = al("bt1", [128, 8, 128], BF16)
    relb, b_relb = al("relb", [128, 32, 8], F32)
    relc, b_relc = al("relc", [128, 32, 8], F32)
    modcol, b_modcol = al("modcol", [128, NSEQ, 6, 8], F32)
    scT, b_scT = al("scT", [128, 8, NSEQ], BF16)
    vcmp, b_vcmp = al("vcmp", [128, 2, 97], BF16)
    convw, b_convw = al("convw", [128, 4, 31], F32)
    convp, b_convp = al("convp", [128, 3, 4], F32)
    w1sb, b_w1sb = al("w1sb", [128, 2, 32, 64], BF16)
    posT, b_posT = al("posT", [128, 2, 34], BF16)
    posTf, b_posTf = al("posTf", [128, 2, 32], F32)
    w2sb, b_w2sb = al("w2sb", [128, 2, 64], BF16)
    w2f, b_w2f = al("w2f", [128, 2, 64], F32)
    c1col, b_c1col = al("c1col", [128, 2], F32)
    rw_sb, b_rw = al("rw_sb", [128, 8, 8], F32)
    epsc, b_epsc = al("epsc", [128, 1], F32)
    stgT, b_stgT = al("stgT", [128, 128], F32)
    bcast = [al(f"bcast{i}", [128, 1024], F32) for i in range(3)]
    xt = [al(f"xt{i}", [128, 1024], F32) for i in range(2)]
    tt_ = [al(f"tt{i}", [128, 1024], F32) for i in range(2)]
    we_sb, b_we = al("we_sb", [128, 16, 8], F32)
    st_bn, b_stbn = al("st_bn", [128, 2, 6], F32)
    st_mv, b_stmv = al("st_mv", [128, 4], F32)
    sm8, b_sm8 = al("sm8", [128, 16], F32)
    PH = AP_.off

    banks = []
    for i in range(8):
        banks.append((nc.alloc_psum_tensor(f"bank{i}", [128, 512], F32).ap(), Buf(f"bank{i}", excl=True)))

    def A(k, f, reads=(), writes=(), pwrites=(), extra=()):
        P.op(k, f, reads=reads, writes=writes, pwrites=pwrites, extra=extra)

    def load_T(dst, b_dst, n, srcs, full=False):
        A("dve", lambda e: e.memset(stgT, 0.0), [], [b_stgT])
        for cs_, rs_, ap_ in srcs:
            P.dma("sp", stgT[rs_, cs_], ap_, pwrites=[b_stgT])
        n2 = (n + 1) // 2 * 2
        p4, bp4 = banks[4]
        A("pe", lambda e: e.transpose(out=p4[:, 0:n2], in_=stgT[0:n2, :], identity=ident_f[0:n2, 0:n2]), [b_stgT, b_identf], [bp4])
        if full:
            A("dve", lambda e: e.tensor_copy(out=dst, in_=p4[:, 0:n]), [bp4], [b_dst])
        else:
            A("dve", lambda e: e.tensor_copy(out=dst, in_=p4[:, 0:n]), [bp4], pwrites=[b_dst])

    MA = Arena(PH, SB_END)
    P.dma("sp", ident_f, ident_in, writes=[b_identf])
    A("dve", lambda e: e.tensor_copy(out=ident_b, in_=ident_f), [b_identf], [b_identb])
    A("dve", lambda e: e.memset(onesm, 1.0 / 512.0), [], [b_onesm])
    A("dve", lambda e: e.memset(epsc, LN_EPS), [], [b_epsc])
    stg, b_stg = MA.alloc("stg", [128, T], F32)
    P.dma("sp", stg[0:32, :], emat_in, writes=[b_stg])
    A("dve", lambda e: e.memset(emat_b, 0.0), [], [b_emat])
    A("dve", lambda e: e.tensor_copy(out=emat_b[0:32, :], in_=stg[0:32, :]), [b_stg], [b_emat])
    P.dma("sp", fm_sb, fm_in.rearrange("(t p) j -> p t j", p=128), writes=[b_fm])
    stg2, b_stg2 = MA.alloc("stg2", [128, 128], F32)
    P.dma("sp", stg2, edge_in, writes=[b_stg2])
    A("dve", lambda e: e.tensor_copy(out=edge_b, in_=stg2), [b_stg2], [b_edge])
    P.dma("sp", relb.rearrange("p b h -> p (b h)"), rel_bias.partition_broadcast(128).rearrange("p o n -> p (o n)"), writes=[b_relb])
    for b in range(32):
        A("dve", lambda e, b=b: e.tensor_tensor(out=relc[:, b, :], in0=relb[:, b, :], in1=relb[:, 31, :], op=ALU.subtract), [b_relb], [b_relc])
    stg3, b_stg3 = MA.alloc("stg3", [128, 32], F32)
    P.dma("sp", stg3[0:NCMP, :], ov_in, writes=[b_stg3])
    A("dve", lambda e: e.memset(vcmp, 1.0), [], [b_vcmp])
    for g in range(2):
        A("dve", lambda e, g=g: e.tensor_copy(out=vcmp[0:NCMP, g, 65:97], in_=stg3[0:NCMP, :]), [b_stg3], [b_vcmp])
    bkt, b_bkt = MA.alloc("bkt", [128, 2, 128], F32)
    P.dma("sp", bkt[:, 0, :], bk_d0, pwrites=[b_bkt])
    P.dma("sp", bkt[:, 1, :], bk_d1, pwrites=[b_bkt])
    acc01, b_acc01 = MA.alloc("acc01", [128, 2, 8, 128], F32)
    msk, b_msk = MA.alloc("msk", [128, 2, 128], F32)
    A("dve", lambda e: e.tensor_scalar(out=msk, in0=bkt, scalar1=32.0, scalar2=NEG, op0=ALU.is_equal, op1=ALU.mult), [b_bkt], [b_msk])
    for h in range(8):
        A("dve", lambda e, h=h: e.tensor_copy(out=acc01[:, :, h, :], in_=msk), [b_msk], [b_acc01])
    for b in range(31):
        A("dve", lambda e, b=b: e.tensor_scalar(out=msk, in0=bkt, scalar1=float(b), scalar2=None, op0=ALU.is_equal), [b_bkt], [b_msk])
        for h in range(8):
            A("dve", lambda e, b=b, h=h: e.scalar_tensor_tensor(out=acc01[:, :, h, :], in0=msk, scalar=relc[:, b, h:h + 1], in1=acc01[:, :, h, :],
                                                              op0=ALU.mult, op1=ALU.add), [b_msk, b_relc, b_acc01], [b_acc01])
    A("dve", lambda e: e.tensor_copy(out=bt0, in_=acc01[:, 0, :, :]), [b_acc01], [b_bt0])
    A("dve", lambda e: e.tensor_copy(out=bt1, in_=acc01[:, 1, :, :]), [b_acc01], [b_bt1])
    bkc, b_bkc = MA.alloc("bkc", [128, T], F32)
    P.dma("sp", bkc[0:NCMP, :], bk_c, writes=[b_bkc])
    mskc, b_mskc = MA.alloc("mskc", [128, T], F32)
    accc, b_accc = MA.alloc("accc", [128, 8, T], F32)
    A("dve", lambda e: e.tensor_scalar(out=mskc[0:NCMP], in0=bkc[0:NCMP], scalar1=32.0, scalar2=NEG, op0=ALU.is_equal, op1=ALU.mult), [b_bkc], [b_mskc])
    for h in range(8):
        A("dve", lambda e, h=h: e.tensor_copy(out=accc[0:NCMP, h, :], in_=mskc[0:NCMP]), [b_mskc], [b_accc])
    for b in range(31):
        A("dve", lambda e, b=b: e.tensor_scalar(out=mskc[0:NCMP], in0=bkc[0:NCMP], scalar1=float(b), scalar2=None, op0=ALU.is_equal), [b_bkc], [b_mskc])
        for h in range(8):
            A("dve", lambda e, b=b, h=h: e.scalar_tensor_tensor(out=accc[0:NCMP, h, :], in0=mskc[0:NCMP], scalar=relc[0:NCMP, b, h:h + 1], in1=accc[0:NCMP, h, :],
                                                              op0=ALU.mult, op1=ALU.add), [b_mskc, b_relc, b_accc], [b_accc])
    bcb, b_bcb = MA.alloc("bcb", [128, 8, T], BF16)
    A("act", lambda e: e.activation(out=bcb[0:NCMP], in_=accc[0:NCMP], func=AF.Copy), [b_accc], [b_bcb])
    for h in range(8):
        P.dma("sp", bc_dram[h * 128:h * 128 + NCMP, :], bcb[0:NCMP, h, :], reads=[b_bcb], pwrites=[b_bc], owner=b_bcb)
    csb, b_csb = MA.alloc("csb", [128, D], F32)
    A("dve", lambda e: e.memset(csb[0:32, :], 0.0), [], [b_csb])
    P.dma("sp", csb[0:NSEQ, :], c_in, writes=[b_csb])
    A("act", lambda e: e.activation(out=csb[0:32, :], in_=csb[0:32, :], func=AF.Silu), [b_csb], [b_csb])
    pb, b_pb = banks[4]
    for kc in range(8):
        A("pe", lambda e, kc=kc: e.transpose(out=pb[:, kc * 32:(kc + 1) * 32], in_=csb[0:32, kc * 128:(kc + 1) * 128], identity=ident_f[0:32, 0:32]),
          [b_csb, b_identf], [b_pb] if kc == 0 else (), pwrites=() if kc == 0 else [b_pb])
    A("dve", lambda e: e.tensor_copy(out=scT, in_=pb[:, 0:256].rearrange("p (k s) -> p k s", s=32)[:, :, 0:NSEQ]), [b_pb], [b_scT])
    P.barrier()
    def rowb(dst, b_dst, src_row, reads=()):
        P.dma("sp", dst, src_row.partition_broadcast(128).rearrange("p o n -> p (o n)"), reads=list(reads), writes=[b_dst])

    def layer_norm_tile(tt, b_tt, lng, b_lng, lnb, b_lnb):
        for hf in range(2):
            A("dve", lambda e, hf=hf: e.bn_stats(out=st_bn[:, hf, :], in_=tt[:, hf * 512:(hf + 1) * 512]), [b_tt], pwrites=[b_stbn])
        A("dve", lambda e: e.bn_aggr(out=st_mv[:, 0:2], in_=st_bn.rearrange("p a b -> p (a b)")), [b_stbn], [b_stmv])
        A("act", lambda e: e.activation(out=st_mv[:, 2:3], in_=st_mv[:, 1:2], func=AF.Sqrt, bias=epsc[:, 0:1]), [b_stmv, b_epsc], [b_stmv])
        A("dve", lambda e: e.reciprocal(out=st_mv[:, 3:4], in_=st_mv[:, 2:3]), [b_stmv], [b_stmv])
        A("dve", lambda e: e.tensor_scalar(out=tt, in0=tt, scalar1=st_mv[:, 0:1], scalar2=st_mv[:, 3:4], op0=ALU.subtract, op1=ALU.mult), [b_tt, b_stmv], [b_tt])
        A("dve", lambda e: e.tensor_tensor(out=tt, in0=tt, in1=lng, op=ALU.mult), [b_tt, b_lng], [b_tt])
        A("dve", lambda e: e.tensor_tensor(out=tt, in0=tt, in1=lnb, op=ALU.add), [b_tt, b_lnb], [b_tt])

    NS = 0.125

    def ada_phase(li, l):
        R = Arena(PH, SB_END)
        adab, b_adab = R.alloc("adab", [128, 6144], F32)
        mrow, b_mrow = R.alloc("mrow", [128, 6144], F32)
        wp = [R.alloc(f"adaw{i}", [128, 8, 512], BF16) for i in range(2)]
        P.dma("sp", adab[0:NSEQ, :], ada_b[l:l + 1, :].partition_broadcast(NSEQ).rearrange("p o n -> p (o n)"), writes=[b_adab])
        wv = wview(("ada", l), D, 6144)
        for j in range(12):
            t, bt = wp[j % 2]
            P.dma("sp", t, wv[:, j * 512:(j + 1) * 512].rearrange("(kc p) n -> p kc n", p=128), writes=[bt], **wdep(("ada", l), D * 6144))
            ps, bps = banks[j % 2]
            for kc in range(8):
                A("pe", lambda e, kc=kc, t=t, ps=ps: e.matmul(ps[0:NSEQ, :], lhsT=scT[:, kc, :], rhs=t[:, kc, :], start=(kc == 0), stop=(kc == 7)),
                  [b_scT, bt], [bps] if kc == 0 else (), pwrites=() if kc == 0 else [bps])
            A("dve", lambda e, j=j, ps=ps: e.tensor_tensor(out=mrow[0:NSEQ, j * 512:(j + 1) * 512], in0=ps[0:NSEQ, :], in1=adab[0:NSEQ, j * 512:(j + 1) * 512], op=ALU.add),
              [bps, b_adab], pwrites=[b_mrow])
        for seg in (1, 2, 4, 5):
            A("dve", lambda e, seg=seg: e.tensor_scalar(out=mrow[0:NSEQ, seg * 1024:(seg + 1) * 1024], in0=mrow[0:NSEQ, seg * 1024:(seg + 1) * 1024],
                                                        scalar1=1.0, scalar2=None, op0=ALU.add), [b_mrow], [b_mrow])
        P.dma("sp", modrow[li * NSEQ:(li + 1) * NSEQ, :], mrow[0:NSEQ, :], reads=[b_mrow], writes=[b_modrow], owner=b_mrow)
        P.barrier()
        for s in range(NSEQ):
            load_T(modcol[:, s, :, :].rearrange("p a b -> p (a b)"), b_modcol, 48,
                   [(slice(0, 128), slice(0, 48), modrow[li * NSEQ + s, :].rearrange("(r p) -> r p", p=128))])

    def mixer_phase(li, l, s, xsrc, b_xsrc, xdst, b_xdst):
        R1 = Arena(PH, SB_END)
        w_in_sb, b_win = R1.alloc("w_in_sb", [128, 8, D_IN], BF16)
        hT = [R1.alloc(f"hT{i}", [128, 8, 512], BF16) for i in range(2)]
        KcT, b_KcT = R1.alloc("KcT", [128, T], BF16)
        VcT, b_VcT = R1.alloc("VcT", [128, T], BF16)
        sig, b_sig = R1.alloc("sig", [128, 512], F32)
        ov_end = R1.off
        w_out_sb, b_wout = R1.alloc("w_out_sb", [128, 8, D], BF16)
        QT, b_QT = R1.alloc("QT", [128, 4, T], BF16)
        KsP = [R1.alloc(f"KsP{g}", [128, T], BF16) for g in range(2)]
        KwP = [R1.alloc(f"KwP{g}", [128, T], BF16) for g in range(2)]
        Vs_aug, b_Vs = R1.alloc("Vs_aug", [128, 16, 2, 65], BF16)
        Vw_aug, b_Vw = R1.alloc("Vw_aug", [128, 16, 2, 65], BF16)
        gsb, b_gsb = R1.alloc("gsb", [128, 16, 24], F32)
        yT, b_yT = R1.alloc("yT", [128, 4, 30 + T], BF16)
        convacc, b_cacc = R1.alloc("convacc", [128, 4, 512], F32)
        negselT, b_nsT = R1.alloc("negselT", [128, 2, 512], BF16)
        KcmpT, b_KcmpT = R1.alloc("KcmpT", [128, 2, 128], BF16)
        Gt, b_G = R1.alloc("Gt", [128, 128], BF16)
        dg = [R1.alloc(f"dg{i}", [128, 8, 128], BF16) for i in range(2)]
        R2 = Arena(PH, ov_end)
        o_nsaT, b_onT = R2.alloc("o_nsaT", [128, 4, T], BF16)
        o_convT, b_ocT = R2.alloc("o_convT", [128, 4, T], BF16)
        o_nsa, b_on = R2.alloc("o_nsa", [128, 4, 512], F32)
        Osb = [R2.alloc(f"Osb{i}", [128, 512], F32) for i in range(2)]
        PTb = [R2.alloc(f"PT{i}", [128, 512], BF16) for i in range(2)]
        Bcs = [R2.alloc(f"Bcs{i}", [128, 512], BF16) for i in range(2)]
        convsq = [R2.alloc(f"convsq{i}", [128, 512], F32) for i in range(2)]
        s_mean, b_smean = R2.alloc("s_mean", [128, 512], F32)
        s_var, b_svar = R2.alloc("s_var", [128, 512], F32)
        s_rstd, b_srstd = R2.alloc("s_rstd", [128, 512], F32)
        impacc, b_imp = R2.alloc("impacc", [128, 4, 2, 32], F32)
        score, b_score = R2.alloc("score", [128, 32], F32)
        negsel, b_negsel = R2.alloc("negsel", [128, 32], F32)
        rz, b_rz = R2.alloc("rz", [128, 4], F32)
        coef, b_coef = R2.alloc("coef", [128, 4], F32)
        print("mixer sbuf: R1 end", R1.off, "R2 end", R2.off, "ov_end", ov_end, "limit", SB_END)

        wv = wview(("w_in", l), D, D_IN)
        for kc in range(8):
            P.dma("sp", w_in_sb[:, kc, :], wv[kc * 128:(kc + 1) * 128, :], pwrites=[b_win], **wdep(("w_in", l), D * D_IN))
        wo = wview(("w_out", l), D, D)
        P.dma("sp", w_out_sb, wo.rearrange("(kc p) n -> p kc n", p=128), writes=[b_wout], **wdep(("w_out", l), D * D))
        cw = wview(("cmp_w1", l), 2 * 2048, 64).rearrange("(kv l d) o -> d kv l o", kv=2, l=32)
        for g in range(2):
            for kv in range(2):
                P.dma("sp", w1sb[g * 64:(g + 1) * 64, kv, :, :], cw[:, kv, :, :], pwrites=[b_w1sb], **wdep(("cmp_w1", l), 2 * 2048 * 64))
            P.dma("sp", w2f[g * 64:(g + 1) * 64, :, :], cmp_w2[l].rearrange("kv o p -> o kv p"), pwrites=[b_w2f])
        A("dve", lambda e: e.memset(posT, 0.0), [], [b_posT])
        for kv in range(2):
            load_T(posTf[:, kv, :], b_posTf, 32, [(slice(0, 64), slice(0, 32), cmp_pos[l, kv]), (slice(64, 128), slice(0, 32), cmp_pos[l, kv])])
        A("dve", lambda e: e.tensor_copy(out=posT[:, :, 0:32], in_=posTf), [b_posTf], [b_posT])
        A("dve", lambda e: e.tensor_copy(out=w2sb, in_=w2f), [b_w2f], [b_w2sb])
        for c4 in range(4):
            load_T(convw[:, c4, :], b_convw, 31, [(slice(0, 128), slice(0, 31), conv_w[l][:, c4 * 128:(c4 + 1) * 128])])
        load_T(convp.rearrange("p a b -> p (a b)"), b_convp, 12,
               [(slice(0, 128), slice(4 * i, 4 * i + 4), src[l, :].rearrange("(c p) -> c p", p=128)) for i, src in enumerate((conv_b, conv_g, conv_be))], full=True)
        A("dve", lambda e: e.memset(Vs_aug, 1.0), [], [b_Vs])
        A("dve", lambda e: e.memset(Vw_aug, 1.0), [], [b_Vw])
        A("dve", lambda e: e.memset(yT[:, :, 0:30], 0.0), [], [b_yT])
        for g in range(2):
            A("dve", lambda e, g=g: e.memset(KsP[g][0], 0.0), [], [KsP[g][1]])
            A("dve", lambda e, g=g: e.memset(KwP[g][0], 0.0), [], [KwP[g][1]])
        A("dve", lambda e: e.memset(KcmpT, 0.0), [], [b_KcmpT])
        A("dve", lambda e: e.memset(negselT, 0.0), [], [b_nsT])

        if cfg.get("stop") == "loads":
            P.barrier()
            return
        for tc in range(4):
            hTt, b_hTt = hT[tc % 2]
            cs = slice(tc * 512, (tc + 1) * 512)
            for t4 in range(4):
                tile = tc * 4 + t4
                xtile, b_x = xt[tile % 2]
                r0 = s * T + tile * 128
                P.dma("sp", xtile, xsrc[r0:r0 + 128, :], reads=[b_xsrc], writes=[b_x])
                for half in range(2):
                    ps, bps = banks[half]
                    for k4 in range(4):
                        kc = half * 4 + k4
                        A("pe", lambda e, ps=ps, k4=k4, kc=kc, xtile=xtile: e.transpose(out=ps[:, k4 * 128:(k4 + 1) * 128], in_=xtile[:, kc * 128:(kc + 1) * 128], identity=ident_f),
                          [b_x, b_identf], [bps] if k4 == 0 else (), pwrites=() if k4 == 0 else [bps])
                    for k4 in range(4):
                        kc = half * 4 + k4
                        o_ap = hTt[:, kc, t4 * 128:(t4 + 1) * 128]
                        i_ap = ps[:, k4 * 128:(k4 + 1) * 128]
                        if half == 0:
                            A("dve", lambda e, o_ap=o_ap, i_ap=i_ap, kc=kc: e.tensor_scalar(out=o_ap, in0=i_ap, scalar1=modcol[:, s, 1, kc:kc + 1], scalar2=modcol[:, s, 0, kc:kc + 1],
                                                                                         op0=ALU.mult, op1=ALU.add), [bps, b_modcol], pwrites=[b_hTt])
                        else:
                            A("act", lambda e, o_ap=o_ap, i_ap=i_ap, kc=kc: e.activation(out=o_ap, in_=i_ap, func=AF.Identity, scale=modcol[:, s, 1, kc:kc + 1], bias=modcol[:, s, 0, kc:kc + 1]),
                              [bps, b_modcol], pwrites=[b_hTt])

            if cfg.get("stop") == "p1a":
                P.barrier()
                return

            def proj(fo, ps, bps):
                for kc in range(8):
                    A("pe", lambda e, kc=kc: e.matmul(ps, lhsT=w_in_sb[:, kc, fo * 128:(fo + 1) * 128], rhs=hTt[:, kc, :], start=(kc == 0), stop=(kc == 7)),
                      [b_win, b_hTt], [bps] if kc == 0 else (), pwrites=() if kc == 0 else [bps])

            for fo in range(8):
                ps, bps = banks[2 + fo % 2]
                proj(fo, ps, bps)
                if fo < 4:
                    A("act", lambda e, fo=fo, ps=ps: e.activation(out=QT[:, fo, cs], in_=ps, func=AF.Copy, scale=NS), [bps], pwrites=[b_QT])
                elif fo < 6:
                    dst, bd = ((KcT, b_KcT), (VcT, b_VcT))[fo - 4]
                    A("dve", lambda e, dst=dst, ps=ps: e.tensor_copy(out=dst[:, cs], in_=ps), [bps], pwrites=[bd])
                else:
                    KP = KsP if fo == 6 else KwP
                    for g in range(2):
                        A("dve", lambda e, g=g, KP=KP, ps=ps: e.tensor_copy(out=KP[g][0][g * 64:(g + 1) * 64, cs], in_=ps[g * 64:(g + 1) * 64, :]), [bps], pwrites=[KP[g][1]])
            if cfg.get("stop") == "p1b":
                P.barrier()
                return
            for c in range(4):
                pa, bpa = banks[2 if c % 2 == 0 else 5]
                pg, bpg = banks[3 if c % 2 == 0 else 6]
                proj(8 + c, pa, bpa)
                proj(12 + c, pg, bpg)
                A("act", lambda e, pg=pg: e.activation(out=sig, in_=pg, func=AF.Sigmoid), [bpg], [b_sig])
                A("dve", lambda e, c=c, pa=pa: e.tensor_tensor(out=yT[:, c, 30 + tc * 512:30 + (tc + 1) * 512], in0=pa, in1=sig, op=ALU.mult), [bpa, b_sig], pwrites=[b_yT])
            if cfg.get("stop") == "p1c":
                P.barrier()
                return
            for t4 in range(4):
                tile = tc * 4 + t4
                ps, bps = banks[7]
                for kc in range(8):
                    A("pe", lambda e, kc=kc, t4=t4, ps=ps: e.matmul(ps[:, 0:280], lhsT=hTt[:, kc, t4 * 128:(t4 + 1) * 128], rhs=w_in_sb[:, kc, 2048:2328], start=(kc == 0), stop=(kc == 7)),
                      [b_win, b_hTt], [bps] if kc == 0 else (), pwrites=() if kc == 0 else [bps])
                A("dve", lambda e, tile=tile, ps=ps: e.tensor_copy(out=Vs_aug[:, tile, :, 0:64], in_=ps[:, 0:128].rearrange("p (g d) -> p g d", g=2)), [bps], pwrites=[b_Vs])
                A("dve", lambda e, tile=tile, ps=ps: e.tensor_copy(out=Vw_aug[:, tile, :, 0:64], in_=ps[:, 128:256].rearrange("p (g d) -> p g d", g=2)), [bps], pwrites=[b_Vw])
                A("act", lambda e, tile=tile, ps=ps: e.activation(out=gsb[:, tile, :], in_=ps[:, 256:280], func=AF.Sigmoid), [bps], pwrites=[b_gsb])

        if cfg.get("stop") == "p1":
            P.barrier()
            return
        for kv, (src, b_src) in enumerate(((KcT, b_KcT), (VcT, b_VcT))):
            for g in range(2):
                pbs = slice(g * 64, g * 64 + 64)
                psm, bpsm = banks[0 + 2 * g]
                psc, bpsc = banks[1 + 2 * g]
                for l_ in range(32):
                    A("pe", lambda e, l_=l_, pbs=pbs, psc=psc: e.matmul(psc[pbs, 0:2], lhsT=w1sb[pbs, kv, l_, :], rhs=posT[pbs, kv, l_:l_ + 2], start=(l_ == 0), stop=(l_ == 31)),
                      [b_w1sb, b_posT], [bpsc] if l_ == 0 else (), pwrites=() if l_ == 0 else [bpsc])
                for l_ in range(32):
                    A("pe", lambda e, l_=l_, pbs=pbs, psm=psm: e.matmul(psm[pbs, 0:NCMP], lhsT=w1sb[pbs, kv, l_, :], rhs=src[pbs, l_:l_ + 16 * 126 + 1:16], start=(l_ == 0), stop=(l_ == 31)),
                      [b_w1sb, b_src], [bpsm] if l_ == 0 else (), pwrites=() if l_ == 0 else [bpsm])
                A("dve", lambda e, pbs=pbs, psc=psc: e.tensor_copy(out=c1col[pbs, kv:kv + 1], in_=psc[pbs, 0:1]), [bpsc], pwrites=[b_c1col])
                A("act", lambda e, pbs=pbs, psm=psm: e.activation(out=Gt[pbs, 0:NCMP], in_=psm[pbs, 0:NCMP], func=AF.Gelu_apprx_tanh, bias=c1col[pbs, kv:kv + 1]), [bpsm, b_c1col], pwrites=[b_G])
            if kv == 0:
                for g in range(2):
                    pbs = slice(g * 64, g * 64 + 64)
                    ps2, bps2 = banks[4 + g]
                    A("pe", lambda e, pbs=pbs, ps2=ps2: e.matmul(ps2[pbs, 0:NCMP], lhsT=w2sb[pbs, 0, :], rhs=Gt[pbs, 0:NCMP], start=True, stop=True), [b_w2sb, b_G], [bps2])
                    A("dve", lambda e, pbs=pbs, ps2=ps2, g=g: e.tensor_copy(out=KcmpT[pbs, g, 0:NCMP], in_=ps2[pbs, 0:NCMP]), [bps2], pwrites=[b_KcmpT])
            else:
                for g in range(2):
                    pbs = slice(g * 64, g * 64 + 64)
                    ps3, bps3 = banks[6 + g]
                    A("pe", lambda e, pbs=pbs, ps3=ps3: e.matmul(ps3[0:NCMP, 0:64], lhsT=Gt[pbs, 0:NCMP], rhs=w2sb[pbs, 1, :], start=True, stop=True), [b_w2sb, b_G], [bps3])
                    A("dve", lambda e, g=g, ps3=ps3: e.tensor_copy(out=vcmp[0:NCMP, g, 0:64], in_=ps3[0:NCMP, 0:64]), [bps3], pwrites=[b_vcmp])
        P.barrier()
        if cfg.get("stop") == "p2":
            return

        def conv_ops(qc):
            ops = []
            base = qc * 512
            pc, bpc = banks[6]
            batches = [(0, 8), (8, 16), (16, 24), (24, 31)]

            def build(c, bi):
                j0, j1 = batches[bi]
                dgt, b_dg = dg[(c * 4 + bi) % 2]
                for j in range(j0, j1):
                    A("dve", lambda e: e.tensor_scalar(out=dgt[:, j - j0, :], in0=ident_b, scalar1=convw[:, c, j:j + 1], scalar2=None, op0=ALU.mult),
                      [b_identb, b_convw], [b_dg] if j == j0 else (), pwrites=() if j == j0 else [b_dg])

            def mm(c, bi):
                j0, j1 = batches[bi]
                dgt, b_dg = dg[(c * 4 + bi) % 2]
                for j in range(j0, j1):
                    A("pe", lambda e: e.matmul(pc, lhsT=dgt[:, j - j0, :], rhs=yT[:, c, base + j:base + j + 512], start=(j == 0), stop=(j == 30)),
                      [b_dg, b_yT], [bpc] if j == 0 else (), pwrites=() if j == 0 else [bpc])

            def evac(c):
                A("act", lambda e: e.activation(out=convacc[:, c, :], in_=pc, func=AF.Identity, bias=convp[:, 0, c:c + 1]), [bpc, b_convp], [b_cacc] if c == 0 else (), pwrites=() if c == 0 else [b_cacc])

            for c in range(4):
                ops.append(lambda c=c: build(c, 0))
                ops.append(lambda c=c: build(c, 1))
                ops.append(lambda c=c: mm(c, 0))
                ops.append(lambda c=c: build(c, 2))
                ops.append(lambda c=c: mm(c, 1))
                ops.append(lambda c=c: build(c, 3))
                ops.append(lambda c=c: mm(c, 2))
                ops.append(lambda c=c: (mm(c, 3), evac(c)))

            def stats():
                pm, bpm = banks[7]
                pe_, bpe = banks[6]
                for c in range(4):
                    sq, bsq = convsq[c % 2]
                    A("act", lambda e, c=c, sq=sq: e.activation(out=sq, in_=convacc[:, c, :], func=AF.Square), [b_cacc], [bsq])
                    A("pe", lambda e, c=c: e.matmul(pm, lhsT=onesm, rhs=convacc[:, c, :], start=(c == 0), stop=(c == 3)), [b_onesm, b_cacc], [bpm] if c == 0 else (), pwrites=() if c == 0 else [bpm])
                    A("pe", lambda e, c=c, sq=sq: e.matmul(pe_, lhsT=onesm, rhs=sq, start=(c == 0), stop=(c == 3)), [b_onesm, bsq], [bpe] if c == 0 else (), pwrites=() if c == 0 else [bpe])
                A("act", lambda e: e.activation(out=s_mean, in_=pm, func=AF.Copy), [bpm], [b_smean])
                A("dve", lambda e: e.tensor_tensor(out=s_var, in0=s_mean, in1=s_mean, op=ALU.mult), [b_smean], [b_svar])
                A("dve", lambda e: e.tensor_tensor(out=s_var, in0=pe_, in1=s_var, op=ALU.subtract), [bpe, b_svar], [b_svar])
                A("act", lambda e: e.activation(out=s_rstd, in_=s_var, func=AF.Sqrt, bias=epsc[:, 0:1]), [b_svar, b_epsc], [b_srstd])
                A("dve", lambda e: e.reciprocal(out=s_rstd, in_=s_rstd), [b_srstd], [b_srstd])
                for c in range(4):
                    A("dve", lambda e, c=c: e.tensor_tensor(out=convacc[:, c, :], in0=convacc[:, c, :], in1=s_mean, op=ALU.subtract), [b_cacc, b_smean], pwrites=[b_cacc])
                    A("dve", lambda e, c=c: e.tensor_tensor(out=convacc[:, c, :], in0=convacc[:, c, :], in1=s_rstd, op=ALU.mult), [b_cacc, b_srstd], pwrites=[b_cacc])
                    A("act", lambda e, c=c: e.activation(out=o_convT[:, c, base:base + 512], in_=convacc[:, c, :], func=AF.Silu, scale=convp[:, 1, c:c + 1], bias=convp[:, 2, c:c + 1]),
                      [b_cacc, b_convp], pwrites=[b_ocT])
            ops.append(stats)
            return ops

        cnt = {"o": 0, "s": 0}

        def finish_branch(h, br, W, qc, psO, bpsO):
            osb, b_osb = Osb[cnt["o"] % 2]
            cnt["o"] += 1
            W2 = W + 1
            A("act", lambda e: e.activation(out=osb[0:W, :], in_=psO[0:W, :], func=AF.Copy), [bpsO], [b_osb])
            pT, bpT = banks[4]
            for qt in range(4):
                A("pe", lambda e, qt=qt: e.transpose(out=pT[:, qt * 128:qt * 128 + W2], in_=osb[0:W2, qt * 128:(qt + 1) * 128], identity=ident_f[0:W2, 0:W2]),
                  [b_osb, b_identf], [bpT] if qt == 0 else (), pwrites=() if qt == 0 else [bpT])
            pT3 = pT.rearrange("p (q w) -> p q w", w=128)
            A("dve", lambda e: e.tensor_scalar(out=rz, in0=pT3[:, :, 64], scalar1=1e-30, scalar2=None, op0=ALU.max), [bpT], [b_rz])
            A("dve", lambda e: e.reciprocal(out=rz, in_=rz), [b_rz], [b_rz])
            A("dve", lambda e: e.tensor_tensor(out=coef, in0=rz, in1=gsb[:, qc * 4:(qc + 1) * 4, h * 3 + br], op=ALU.mult), [b_rz, b_gsb], [b_coef])
            for qt in range(4):
                o_ap = o_nsa[:, qt, h * 64:(h + 1) * 64]
                if br == 0:
                    A("dve", lambda e, qt=qt, o_ap=o_ap: e.tensor_scalar(out=o_ap, in0=pT3[:, qt, 0:64], scalar1=coef[:, qt:qt + 1], scalar2=None, op0=ALU.mult), [bpT, b_coef], pwrites=[b_on])
                else:
                    A("dve", lambda e, qt=qt, o_ap=o_ap: e.scalar_tensor_tensor(out=o_ap, in0=pT3[:, qt, 0:64], scalar=coef[:, qt:qt + 1], in1=o_ap, op0=ALU.mult, op1=ALU.add),
                      [bpT, b_coef], pwrites=[b_on])
                if br == 0:
                    g = h // 4
                    i_ap = impacc[:, qt, g, :]
                    if h % 4 == 0:
                        A("dve", lambda e, qt=qt, i_ap=i_ap: e.tensor_scalar(out=i_ap, in0=pT3[:, qt, 65:97], scalar1=rz[:, qt:qt + 1], scalar2=None, op0=ALU.mult), [bpT, b_rz], pwrites=[b_imp])
                    else:
                        A("dve", lambda e, qt=qt, i_ap=i_ap: e.scalar_tensor_tensor(out=i_ap, in0=pT3[:, qt, 65:97], scalar=rz[:, qt:qt + 1], in1=i_ap, op0=ALU.mult, op1=ALU.add),
                          [bpT, b_rz], pwrites=[b_imp])

        for qc in range(4):
            qs = slice(qc * 512, (qc + 1) * 512)
            cops = conv_ops(qc)
            per = (len(cops) + 23) // 24

            def drain(n):
                for _ in range(n):
                    if cops:
                        cops.pop(0)()

            if cfg.get("stop") == "att" and qc == 1:
                return
            pend = []

            def cmp_a(h):
                fo, g = h % 4, h // 4
                bcs, b_bcs = Bcs[h % 2]
                P.dma("sp", bcs[0:NCMP, :], bc_dram[h * 128:h * 128 + NCMP, qs], reads=[b_bc], writes=[b_bcs])
                psS, bpsS = banks[h % 2]
                A("pe", lambda e: e.matmul(psS[0:NCMP, :], lhsT=KcmpT[:, g, 0:NCMP], rhs=QT[:, fo, qs], start=True, stop=False, skip_group_check=True), [b_KcmpT, b_QT], [bpsS])
                A("pe", lambda e: e.matmul(psS[0:NCMP, :], lhsT=ident_b[0:NCMP, 0:NCMP], rhs=bcs[0:NCMP, :], start=False, stop=True, skip_group_check=True), [b_identb, b_bcs], pwrites=[bpsS])
                pt, b_pt = PTb[h % 2]
                A("act", lambda e: e.activation(out=pt[0:NCMP, :], in_=psS[0:NCMP, :], func=AF.Exp, bias=relb[0:NCMP, 31, h:h + 1]), [bpsS, b_relb], [b_pt])

            def cmp_b(h):
                g = h // 4
                pt, b_pt = PTb[h % 2]
                psO, bpsO = banks[2 + h % 2]
                A("pe", lambda e: e.matmul(psO[0:97, :], lhsT=vcmp[0:NCMP, g, :], rhs=pt[0:NCMP, :], start=True, stop=True), [b_vcmp, b_pt], [bpsO])
                pend.append(lambda: finish_branch(h, 0, 97, qc, psO, bpsO))

            cmp_a(0)
            for h in range(8):
                if h + 1 < 8:
                    cmp_a(h + 1)
                cmp_b(h)
                if len(pend) > 1:
                    pend.pop(0)()
                drain(per)
            while pend:
                pend.pop(0)()
            for g in range(2):
                p5, bp5 = banks[5]
                for qt in range(4):
                    A("dve", lambda e, qt=qt, g=g: e.tensor_tensor(out=score, in0=impacc[:, qt, g, :], in1=fm_sb[:, qc * 4 + qt, :], op=ALU.add), [b_imp, b_fm], [b_score])
                    A("dve", lambda e: e.max(out=sm8[:, 0:8], in_=score), [b_score], [b_sm8])
                    A("dve", lambda e: e.tensor_scalar(out=negsel, in0=score, scalar1=sm8[:, 7:8], scalar2=NEG, op0=ALU.is_lt, op1=ALU.mult), [b_score, b_sm8], [b_negsel])
                    A("pe", lambda e, qt=qt: e.transpose(out=p5[0:32, qt * 128:(qt + 1) * 128], in_=negsel, identity=ident_f), [b_negsel, b_identf], [bp5] if qt == 0 else (), pwrites=() if qt == 0 else [bp5])
                A("act", lambda e, g=g: e.activation(out=negselT[0:32, g, :], in_=p5[0:32, :], func=AF.Copy), [bp5], pwrites=[b_nsT])
            pend = []
            for br in (1, 2):
                for h in range(8):
                    fo, g = h % 4, h // 4
                    kts = list(range(0, 4 * qc + 4)) if br == 1 else list(range(max(0, 4 * qc - 4), 4 * qc + 4))
                    KT, b_KT = KsP[g] if br == 1 else KwP[g]
                    Va, b_Va = (Vs_aug, b_Vs) if br == 1 else (Vw_aug, b_Vw)
                    psO, bpsO = banks[2 + cnt["o2"] % 2] if "o2" in cnt else banks[2]
                    cnt["o2"] = cnt.get("o2", 0) + 1
                    steps = []

                    def qk(i):
                        kt = kts[i]
                        sidx = cnt["s"]
                        cnt["s"] += 1
                        psS, bpsS = banks[sidx % 2]
                        pt, b_pt = PTb[sidx % 2]
                        qlo = max(kt, 4 * qc)
                        qhi = 4 * qc + 3 if br == 1 else min(kt + 4, 4 * qc + 3)
                        c0, c1 = (qlo - 4 * qc) * 128, (qhi - 4 * qc + 1) * 128
                        mm = [(psS[:, c0:c1], KT[:, kt * 128:(kt + 1) * 128], QT[:, fo, qc * 512 + c0:qc * 512 + c1], [b_KT, b_QT])]
                        if br == 1:
                            mm.append((psS[:, c0:c1], emat_b[:, kt * 128:(kt + 1) * 128], negselT[:, g, c0:c1], [b_emat, b_nsT]))
                        if kt >= 4 * qc:
                            j = kt - 4 * qc
                            mm.append((psS[:, j * 128:(j + 1) * 128], ident_b, bt0[:, h, :], [b_identb, b_bt0]))
                        j = kt + 1 - 4 * qc
                        if 0 <= j <= 3:
                            mm.append((psS[:, j * 128:(j + 1) * 128], ident_b, bt1[:, h, :], [b_identb, b_bt1]))
                        j = kt + 4 - 4 * qc
                        if br == 2 and 0 <= j <= 3:
                            mm.append((psS[:, j * 128:(j + 1) * 128], ident_b, edge_b, [b_identb, b_edge]))
                        n = len(mm)
                        for mi, (o_, l_, r_, rd) in enumerate(mm):
                            A("pe", lambda e: e.matmul(o_, lhsT=l_, rhs=r_, start=(mi == 0), stop=(mi == n - 1), skip_group_check=True),
                              rd, [bpsS] if mi == 0 else (), pwrites=() if mi == 0 else [bpsS])
                        A("act", lambda e: e.activation(out=pt[:, c0:c1], in_=psS[:, c0:c1], func=AF.Exp, bias=relb[:, 31, h:h + 1]), [bpsS, b_relb], [b_pt])
                        steps.append((kt, c0, c1, pt, b_pt))

                    def pv(i):
                        kt, c0, c1, pt, b_pt = steps[i]
                        n = len(kts)
                        A("pe", lambda e: e.matmul(psO[0:65, c0:c1], lhsT=Va[:, kt, g, :], rhs=pt[:, c0:c1], start=(i == 0), stop=(i == n - 1), skip_group_check=True),
                          [b_Va, b_pt], [bpsO] if i == 0 else (), pwrites=() if i == 0 else [bpsO])

                    qk(0)
                    for i in range(len(kts)):
                        if i + 1 < len(kts):
                            qk(i + 1)
                        pv(i)
                        if i == min(1, len(kts) - 1) and pend:
                            pend.pop(0)()
                    pend.append(lambda h=h, br=br, psO=psO, bpsO=bpsO: finish_branch(h, br, 65, qc, psO, bpsO))
                    drain(per)
            while pend:
                pend.pop(0)()
            drain(len(cops))
            for qt in range(4):
                p5, bp5 = banks[5]
                for fc in range(4):
                    A("pe", lambda e, qt=qt, fc=fc: e.transpose(out=p5[:, fc * 128:(fc + 1) * 128], in_=o_nsa[:, qt, fc * 128:(fc + 1) * 128], identity=ident_f), [b_on, b_identf],
                      [bp5] if fc == 0 else (), pwrites=() if fc == 0 else [bp5])
                A("act", lambda e, qt=qt: e.activation(out=o_nsaT[:, :, qc * 512 + qt * 128:qc * 512 + (qt + 1) * 128], in_=p5.rearrange("p (f q) -> p f q", f=4), func=AF.Copy), [bp5], pwrites=[b_onT])

        (g1b, b_g1b), (lng, b_lng), (lnb, b_lnb) = bcast
        rowb(g1b, b_g1b, modrow[li * NSEQ + s:li * NSEQ + s + 1, 2 * 1024:3 * 1024], reads=[b_modrow])
        rowb(lng, b_lng, ln_g[l, 0:1, :])
        rowb(lnb, b_lnb, ln_b[l, 0:1, :])
        for tile in range(16):
            ts_ = slice(tile * 128, (tile + 1) * 128)
            xtile, b_x = xt[tile % 2]
            ttile, b_t = tt_[tile % 2]
            r0 = s * T + tile * 128
            P.dma("sp", xtile, xsrc[r0:r0 + 128, :], reads=[b_xsrc], writes=[b_x])
            for half in range(2):
                ps, bps = banks[(tile % 2) * 2 + half]
                for k in range(8):
                    lhsT = o_nsaT[:, k, ts_] if k < 4 else o_convT[:, k - 4, ts_]
                    A("pe", lambda e, ps=ps, lhsT=lhsT, k=k, half=half: e.matmul(ps, lhsT=lhsT, rhs=w_out_sb[:, k, half * 512:(half + 1) * 512], start=(k == 0), stop=(k == 7)),
                      [b_onT, b_ocT, b_wout], [bps] if k == 0 else (), pwrites=() if k == 0 else [bps])
                A("dve", lambda e, ps=ps, half=half, ttile=ttile: e.tensor_tensor(out=ttile[:, half * 512:(half + 1) * 512], in0=ps, in1=g1b[:, half * 512:(half + 1) * 512], op=ALU.mult),
                  [bps, b_g1b], [b_t] if half == 0 else (), pwrites=() if half == 0 else [b_t])
            A("dve", lambda e, xtile=xtile, ttile=ttile: e.scalar_tensor_tensor(out=ttile, in0=xtile, scalar=ALPHA, in1=ttile, op0=ALU.mult, op1=ALU.add), [b_x, b_t], [b_t])
            layer_norm_tile(ttile, b_t, lng, b_lng, lnb, b_lnb)
            P.dma("sp", xdst[r0:r0 + 128, :], ttile, reads=[b_t], pwrites=[b_xdst], owner=b_t)

        if cfg.get("dump") and s == 0:
            P.barrier()
            b_dmp = getbuf("dmp")
            items = [QT[:, 0, 0:1024], KsP[0][0][:, 0:1024], KwP[1][0][:, 0:1024], yT[:, 0, 30:1054], o_convT[:, 0, 0:1024], o_nsaT[:, 0, 0:1024],
                     gsb.rearrange("p a b -> p (a b)"), Vs_aug.rearrange("p a b c -> p (a b c)")[:, 0:1024], KcmpT.rearrange("p a b -> p (a b)"),
                     vcmp.rearrange("p a b -> p (a b)"), negselT.rearrange("p a b -> p (a b)"), impacc.rearrange("p a b c -> p (a b c)"),
                     o_nsaT[:, 0, 1024:2048], o_convT[:, 3, 1024:2048], QT[:, 3, 1024:2048]]
            for i, ap_ in enumerate(items):
                w_ = ap_.shape[1]
                P.dma("pool", out[T + i * 128:T + (i + 1) * 128, 0:w_], ap_, pwrites=[b_dmp], owner=b_dmp)
            P.barrier()
    def ffn_phase(li, l, s, xsrc, b_xsrc, xdst, b_xdst):
        moe = (l % 2 == 1)
        R = Arena(PH, SB_END)
        h2T, b_h2T = R.alloc("h2T", [128, 8, T], BF16)
        out_acc, _ = R.alloc("out_acc", [128, 16, D], F32)
        b_acc = [getbuf(f"acc{i}") for i in range(16)]
        w1s = [R.alloc(f"w1s{i}", [128, 8, 2, 256], BF16) for i in range(2)]
        w2s = [R.alloc(f"w2s{i}", [128, 2, D], BF16) for i in range(2)]
        actT = [R.alloc(f"actT{i}", [128, 2, 512], BF16) for i in range(2)]
        sgs = [R.alloc(f"sg{i}", [128, 512], F32) for i in range(2)]
        h2f = [R.alloc(f"h2f{i}", [128, 8, 128], F32) for i in range(2)]
        lg, b_lg = R.alloc("lg", [128, 8], F32)
        ex8, b_ex8 = R.alloc("ex8", [128, 8], F32)
        mk8, b_mk8 = R.alloc("mk8", [128, 8], F32)
        den, b_den = R.alloc("den", [128, 2], F32)
        (g2b, b_g2b), (lng, b_lng), (lnb, b_lnb) = bcast
        rowb(g2b, b_g2b, modrow[li * NSEQ + s:li * NSEQ + s + 1, 5 * 1024:6 * 1024], reads=[b_modrow])
        rowb(lng, b_lng, ln_g[l, 1:2, :])
        rowb(lnb, b_lnb, ln_b[l, 1:2, :])
        if moe:
            P.dma("sp", rw_sb, router_w[l // 2].rearrange("(kc p) e -> p kc e", p=128), writes=[b_rw])
        for tile in range(16):
            ts_ = slice(tile * 128, (tile + 1) * 128)
            xtile, b_x = xt[tile % 2]
            r0 = s * T + tile * 128
            P.dma("sp", xtile, xsrc[r0:r0 + 128, :], reads=[b_xsrc], writes=[b_x])
            hf_, b_hf = h2f[tile % 2]
            for half in range(2):
                ps, bps = banks[half]
                for k4 in range(4):
                    kc = half * 4 + k4
                    A("pe", lambda e, ps=ps, k4=k4, kc=kc, xtile=xtile: e.transpose(out=ps[:, k4 * 128:(k4 + 1) * 128], in_=xtile[:, kc * 128:(kc + 1) * 128], identity=ident_f),
                      [b_x, b_identf], [bps] if k4 == 0 else (), pwrites=() if k4 == 0 else [bps])
                for k4 in range(4):
                    kc = half * 4 + k4
                    i_ap = ps[:, k4 * 128:(k4 + 1) * 128]
                    o_ap = hf_[:, kc, :] if moe else h2T[:, kc, ts_]
                    bo = b_hf if moe else b_h2T
                    if k4 % 2 == 0:
                        A("dve", lambda e, o_ap=o_ap, i_ap=i_ap, kc=kc: e.tensor_scalar(out=o_ap, in0=i_ap, scalar1=modcol[:, s, 4, kc:kc + 1], scalar2=modcol[:, s, 3, kc:kc + 1],
                                                                                     op0=ALU.mult, op1=ALU.add), [bps, b_modcol], pwrites=[bo])
                    else:
                        A("act", lambda e, o_ap=o_ap, i_ap=i_ap, kc=kc: e.activation(out=o_ap, in_=i_ap, func=AF.Identity, scale=modcol[:, s, 4, kc:kc + 1], bias=modcol[:, s, 3, kc:kc + 1]),
                          [bps, b_modcol], pwrites=[bo])
            if moe:
                A("act", lambda e, hf_=hf_, ts_=ts_: e.activation(out=h2T[:, :, ts_], in_=hf_, func=AF.Copy), [b_hf], pwrites=[b_h2T])
                p4, bp4 = banks[4]
                for kc in range(8):
                    A("pe", lambda e, kc=kc, hf_=hf_: e.matmul(p4[:, 0:8], lhsT=hf_[:, kc, :], rhs=rw_sb[:, kc, :], start=(kc == 0), stop=(kc == 7)), [b_hf, b_rw],
                      [bp4] if kc == 0 else (), pwrites=() if kc == 0 else [bp4])
                A("dve", lambda e: e.tensor_copy(out=lg, in_=p4[:, 0:8]), [bp4], [b_lg])
                A("dve", lambda e: e.max(out=sm8[:, 0:8], in_=lg), [b_lg], [b_sm8])
                A("dve", lambda e: e.tensor_scalar(out=sm8[:, 8:9], in0=sm8[:, 0:1], scalar1=-1.0, scalar2=None, op0=ALU.mult), [b_sm8], [b_sm8])
                A("act", lambda e: e.activation(out=ex8, in_=lg, func=AF.Exp, bias=sm8[:, 8:9]), [b_lg, b_sm8], [b_ex8])
                A("dve", lambda e: e.tensor_scalar(out=mk8, in0=lg, scalar1=sm8[:, 1:2], scalar2=None, op0=ALU.is_ge), [b_lg, b_sm8], [b_mk8])
                A("dve", lambda e: e.tensor_tensor(out=mk8, in0=mk8, in1=ex8, op=ALU.mult), [b_mk8, b_ex8], [b_mk8])
                A("dve", lambda e: e.reduce_sum(out=den[:, 0:1], in_=mk8, axis=mybir.AxisListType.X), [b_mk8], [b_den])
                A("dve", lambda e: e.reciprocal(out=den[:, 1:2], in_=den[:, 0:1]), [b_den], [b_den])
                A("dve", lambda e, tile=tile: e.tensor_scalar(out=we_sb[:, tile, :], in0=mk8, scalar1=den[:, 1:2], scalar2=None, op0=ALU.mult), [b_mk8, b_den], pwrites=[b_we])
        if moe:
            specs = [(("moe1", l, e_), ("moe2", l, e_), D_FFE, e_) for e_ in range(NE)]
        else:
            specs = [(("ffn1", l), ("ffn2", l), D_FF, None)]
        slot = 0
        first = True
        for (k1, k2, F, ex) in specs:
            W1 = wview(k1, D, 2 * F)
            W2 = wview(k2, F, D)
            for f0 in range(0, F, 256):
                w1t, b_w1 = w1s[slot % 2]
                w2t, b_w2 = w2s[slot % 2]
                slot += 1
                P.dma("sp", w1t[:, :, 0, :], W1[:, f0:f0 + 256].rearrange("(kc p) n -> p kc n", p=128), pwrites=[b_w1], **wdep(k1, D * 2 * F))
                P.dma("sp", w1t[:, :, 1, :], W1[:, F + f0:F + f0 + 256].rearrange("(kc p) n -> p kc n", p=128), pwrites=[b_w1], **wdep(k1, D * 2 * F))
                P.dma("sp", w2t, W2[f0:f0 + 256, :].rearrange("(fc p) n -> p fc n", p=128), writes=[b_w2], **wdep(k2, F * D))
                for tc in range(4):
                    aT, b_aT = actT[tc % 2]
                    cs = slice(tc * 512, (tc + 1) * 512)
                    for fc in range(2):
                        psG, bpsG = banks[(fc % 2) * 2]
                        psU, bpsU = banks[(fc % 2) * 2 + 1]
                        sg, b_sg = sgs[fc % 2]
                        for gu, (ps, bps) in enumerate(((psG, bpsG), (psU, bpsU))):
                            for kc in range(8):
                                A("pe", lambda e, ps=ps, kc=kc, gu=gu, fc=fc, w1t=w1t, cs=cs: e.matmul(ps, lhsT=w1t[:, kc, gu, fc * 128:(fc + 1) * 128], rhs=h2T[:, kc, cs], start=(kc == 0), stop=(kc == 7)),
                                  [b_w1, b_h2T], [bps] if kc == 0 else (), pwrites=() if kc == 0 else [bps])
                        A("act", lambda e, sg=sg, psG=psG: e.activation(out=sg, in_=psG, func=AF.Silu), [bpsG], [b_sg])
                        A("dve", lambda e, aT=aT, fc=fc, psU=psU, sg=sg: e.tensor_tensor(out=aT[:, fc, :], in0=psU, in1=sg, op=ALU.mult), [bpsU, b_sg], [b_aT] if fc == 0 else (), pwrites=() if fc == 0 else [b_aT])
                    for t4 in range(4):
                        tile = tc * 4 + t4
                        for half in range(2):
                            psO, bpsO = banks[4 + (t4 * 2 + half) % 4]
                            for fc in range(2):
                                A("pe", lambda e, psO=psO, aT=aT, fc=fc, t4=t4, w2t=w2t, half=half: e.matmul(psO, lhsT=aT[:, fc, t4 * 128:(t4 + 1) * 128], rhs=w2t[:, fc, half * 512:(half + 1) * 512], start=(fc == 0), stop=(fc == 1)),
                                  [b_aT, b_w2], [bpsO] if fc == 0 else (), pwrites=() if fc == 0 else [bpsO])
                            o_ap = out_acc[:, tile, half * 512:(half + 1) * 512]
                            if moe:
                                wcol = we_sb[:, tile, ex:ex + 1]
                                if first:
                                    A("dve", lambda e, o_ap=o_ap, psO=psO, wcol=wcol: e.tensor_scalar(out=o_ap, in0=psO, scalar1=wcol, scalar2=None, op0=ALU.mult), [bpsO, b_we], pwrites=[b_acc[tile]])
                                else:
                                    A("dve", lambda e, o_ap=o_ap, psO=psO, wcol=wcol: e.scalar_tensor_tensor(out=o_ap, in0=psO, scalar=wcol, in1=o_ap, op0=ALU.mult, op1=ALU.add), [bpsO, b_we], pwrites=[b_acc[tile]])
                            else:
                                if first:
                                    A("dve", lambda e, o_ap=o_ap, psO=psO: e.tensor_copy(out=o_ap, in_=psO), [bpsO], pwrites=[b_acc[tile]])
                                else:
                                    A("dve", lambda e, o_ap=o_ap, psO=psO: e.tensor_tensor(out=o_ap, in0=psO, in1=o_ap, op=ALU.add), [bpsO], pwrites=[b_acc[tile]])
                first = False
        for tile in range(16):
            xtile, b_x = xt[tile % 2]
            ttile, b_t = tt_[tile % 2]
            r0 = s * T + tile * 128
            P.dma("sp", xtile, xsrc[r0:r0 + 128, :], reads=[b_xsrc], writes=[b_x])
            A("dve", lambda e, tile=tile, ttile=ttile: e.tensor_tensor(out=ttile, in0=out_acc[:, tile, :], in1=g2b, op=ALU.mult), [b_acc[tile], b_g2b], [b_t])
            A("dve", lambda e, xtile=xtile, ttile=ttile: e.scalar_tensor_tensor(out=ttile, in0=xtile, scalar=ALPHA, in1=ttile, op0=ALU.mult, op1=ALU.add), [b_x, b_t], [b_t])
            layer_norm_tile(ttile, b_t, lng, b_lng, lnb, b_lnb)
            P.dma("sp", xdst[r0:r0 + 128, :], ttile, reads=[b_t], pwrites=[b_xdst], owner=b_t)

    cur, b_cur = x_in, Buf("x_in")
    for li, l in enumerate(layers):
        if cfg.get("stop") == "setup":
            break
        ada_phase(li, l)
        P.barrier()
        if cfg.get("stop") == "ada":
            break
        last = (li == len(layers) - 1)
        dst, b_dst = (out, b_out) if last else (xb, b_xb)
        for s in range(NSEQ):
            if cfg.get("mixer_only"):
                if cfg.get("dump") and s > 0:
                    continue
                mixer_phase(li, l, s, cur, b_cur, dst, b_dst)
                P.barrier()
                continue
            if cfg.get("ffn_only"):
                ffn_phase(li, l, s, cur, b_cur, dst, b_dst)
                P.barrier()
                continue
            mixer_phase(li, l, s, cur, b_cur, xa, b_xa)
            P.barrier()
            ffn_phase(li, l, s, xa, b_xa, dst, b_dst)
            P.barrier()
        cur, b_cur = xb, b_xb
    if cfg.get("stop"):
        A("dve", lambda e: e.memset(tt_[0][0], 0.0), [], [tt_[0][1]])
        if cfg["stop"] == "ada":
            A("dve", lambda e: e.tensor_copy(out=tt_[0][0][:, 0:NSEQ * 48], in_=modcol.rearrange("p s a b -> p (s a b)")), [b_modcol], [tt_[0][1]])
        P.dma("sp", out[0:128, :], tt_[0][0], reads=[tt_[0][1]], pwrites=[b_out], owner=tt_[0][1])
        if cfg["stop"] == "ada":
            P.dma("sp", out[128:128 + NSEQ, :], modrow[0:NSEQ, 0:1024], reads=[b_modrow], pwrites=[b_out], owner=b_modrow)
    P.wait_all("sp", [b_out])
    P.emit()
    return nc, NCH


SMALL = ["cmp_pos", "cmp_w2", "conv_w", "conv_b", "conv_ln_g", "conv_ln_b", "ada_b", "ln_g", "ln_b", "router_w"]


def run(inp, cfg, n_cores, trace=False):
    layers = cfg["layers"]
    NSEQ = cfg["nseq"]
    nc, NCH = build(cfg)
    flat, nch = pack_flat(inp, layers)
    consts = host_consts()
    x = np.asarray(inp["x"], np.float32)
    c = np.asarray(inp["c"], np.float32)
    in_maps = []
    for core in range(n_cores):
        m = {"x": np.ascontiguousarray(x[core * NSEQ:(core + 1) * NSEQ].reshape(NSEQ * T, D)),
             "c": np.ascontiguousarray(c[core * NSEQ:(core + 1) * NSEQ])}
        if cfg["gather"]:
            p = ORDER.index(core)
            m["wsh"] = np.ascontiguousarray(flat[:, p].reshape(nch * 512, 1024))
        else:
            m["wfull"] = flat.reshape(nch * 4096, 1024)
        for k in SMALL:
            m[k] = np.ascontiguousarray(np.asarray(inp[k], np.float32))
        m["rel_bias"] = np.ascontiguousarray(np.asarray(inp["rel_bias"], np.float32).reshape(1, 256))
        m.update(consts)
        in_maps.append(m)
    res = run_bass_kernel_spmd(nc, in_maps, core_ids=list(range(n_cores)), trace=trace)
    outs = [r["out"].reshape(NSEQ, T, D) for r in res.results]
    return np.concatenate(outs, 0), res


def kernel(**inputs):
    cfg = dict(nseq=2, layers=[0, 1, 2, 3], gather=True)
    o, _ = run(inputs, cfg, 8)
    return o.astype(np.float32)
```

```python
import math
import contextlib
import types
import numpy as np
import concourse.bass as bass
import concourse.mybir as mybir
from concourse.bass_utils import run_bass_kernel_spmd

F32 = mybir.dt.float32
BF16 = mybir.dt.bfloat16
ALU = mybir.AluOpType
AF = mybir.ActivationFunctionType

D = 1024
T = 2048
DEPTH = 4
NH = 8
HD = 64
D_IN = 2328
D_FF = 2816
D_FFE = 3584
NE = 8
NCMP = 127
ALPHA = (2 * DEPTH) ** 0.25
LN_EPS = 1e-5
NEG = -30000.0
CHUNK_ELEMS = 4096 * 1024
ORDER = [0, 1, 4, 5, 2, 3, 6, 7]
EPOCH = 16000
SEG_CH = 28
SEG_ELEMS = SEG_CH * CHUNK_ELEMS


class Buf:
    __slots__ = ("name", "w", "wf", "r", "sem", "semcnt", "excl")

    def __init__(self, name, excl=False):
        self.name = name
        self.excl = excl
        self.w = {}
        self.wf = {}
        self.r = {}
        self.sem = None
        self.semcnt = 0


def _snap(fn):
    if fn is None or fn.__closure__ is None:
        return fn
    cells = []
    for c in fn.__closure__:
        try:
            cells.append(types.CellType(c.cell_contents))
        except ValueError:
            cells.append(c)
    g = types.FunctionType(fn.__code__, fn.__globals__, fn.__name__, fn.__defaults__, tuple(cells))
    g.__kwdefaults__ = fn.__kwdefaults__
    return g


class Prog:
    def __init__(self, nc):
        self.nc = nc
        self.stack = contextlib.ExitStack()
        self.engnames = ["pe", "act", "dve", "pool", "sp"]
        self.streams = {k: [] for k in self.engnames}
        self.count = {k: 0 for k in self.engnames}
        self.esems = {k: [] for k in self.engnames}
        self.seen = {k: {} for k in self.engnames}
        self.dmabufs = []
        self.nobarrier = set()
        self.nsem = 0

    def new_sem(self, name):
        self.nsem += 1
        return self.stack.enter_context(self.nc.semaphore(name))

    def _esem(self, k, epoch):
        while len(self.esems[k]) <= epoch:
            self.esems[k].append(self.new_sem(f"e_{k}_{len(self.esems[k])}"))
        return self.esems[k][epoch]

    def _need(self, k, wt, waits):
        if wt[0] == "e":
            _, e2, c = wt
            if e2 == "pe" and k == "pe":
                return
            key = e2
        else:
            _, b, c = wt
            key = ("d", id(b))
        if self.seen[k].get(key, 0) >= c:
            return
        self.seen[k][key] = c
        waits.append(wt)

    def _deps(self, k, reads, writes, pwrites):
        waits = []
        for b in reads:
            for wt in b.w.values():
                self._need(k, wt, waits)
            if b.excl:
                for kk, wt in b.r.items():
                    if kk != k:
                        self._need(k, wt, waits)
        for b in writes:
            for wt in b.w.values():
                self._need(k, wt, waits)
            for wt in b.r.values():
                self._need(k, wt, waits)
        for b in pwrites:
            for wt in b.r.values():
                self._need(k, wt, waits)
            for wt in b.wf.values():
                self._need(k, wt, waits)
        return waits

    def _record(self, key, me, reads, writes, pwrites):
        for b in reads:
            b.r[key] = me
        for b in writes:
            b.w = {key: me}
            b.wf = {key: me}
            b.r = {}
        for b in pwrites:
            if b.r:
                b.w = {key: me}
                b.wf = {}
                b.r = {}
            else:
                b.w[key] = me

    def op(self, k, fn, reads=(), writes=(), pwrites=(), extra=()):
        waits = self._deps(k, reads, writes, pwrites)
        for wt in extra:
            self._need(k, wt, waits)
        self.count[k] += 1
        me = ("e", k, self.count[k])
        self._record(k, me, reads, writes, pwrites)
        self.streams[k].append((waits, _snap(fn), me))

    def dma(self, q, out_ap, in_ap, reads=(), writes=(), pwrites=(), owner=None, extra=(), fn=None, inc=16, **kw):
        if owner is None:
            owner = (list(writes) + list(pwrites))[0]
        if owner.sem is None:
            owner.sem = self.new_sem("d_" + owner.name)
            self.dmabufs.append(owner)
        waits = self._deps(q, reads, writes, pwrites)
        for wt in extra:
            self._need(q, wt, waits)
        owner.semcnt += inc
        me = ("d", owner, owner.semcnt)
        self._record(("d", id(owner)), me, reads, writes, pwrites)
        if fn is None:
            fn = lambda eng, o=out_ap, i=in_ap, kw=kw: eng.dma_start(out=o, in_=i, **kw)
        self.streams[q].append((waits, _snap(fn), ("dinc", owner, inc)))

    def barrier(self):
        snap = dict(self.count)
        dsn = [(b, b.semcnt) for b in self.dmabufs if b.name not in self.nobarrier]
        for k in self.engnames:
            waits = []
            for e2 in self.engnames:
                if e2 != k and snap[e2] > 0:
                    self._need(k, ("e", e2, snap[e2]), waits)
            for b, c in dsn:
                if c > 0:
                    self._need(k, ("d", b, c), waits)
            self.streams[k].append((waits, None, None))

    def wait_all(self, k, bufs):
        waits = self._deps(k, bufs, (), ())
        self.streams[k].append((waits, None, None))

    def emit(self):
        nc = self.nc
        engattr = {"pe": "tensor", "act": "scalar", "dve": "vector", "pool": "gpsimd", "sp": "sync"}
        for k in self.engnames:
            self._esem(k, max(self.count[k] - 1, 0) // EPOCH)
        with nc.Block() as block:
            for k in self.engnames:
                stream = self.streams[k]

                def body(eng, k=k, stream=stream):
                    for waits, fn, inc in stream:
                        for wt in waits:
                            if wt[0] == "e":
                                ep, cc = divmod(wt[2] - 1, EPOCH)
                                eng.wait_ge(self.esems[wt[1]][ep], cc + 1)
                            else:
                                eng.wait_ge(wt[1].sem, wt[2])
                        if fn is None:
                            continue
                        ins = fn(eng)
                        if inc[0] == "e":
                            ep, cc = divmod(inc[2] - 1, EPOCH)
                            ins.then_inc(self.esems[k][ep], 1)
                        else:
                            ins.then_inc(inc[1].sem, inc[2])

                getattr(block, engattr[k])(body)
        self.stack.close()


def flat_layout(layers):
    off = 0
    lay = {}

    def add(name, n):
        nonlocal off
        if (off // SEG_ELEMS) != ((off + n - 1) // SEG_ELEMS):
            off = ((off + n - 1) // SEG_ELEMS) * SEG_ELEMS
        lay[name] = off
        off += n

    for l in layers:
        add(("ada", l), D * 6 * D)
        add(("w_in", l), D * D_IN)
        add(("cmp_w1", l), 2 * 2048 * 64)
        add(("w_out", l), D * D)
        if l % 2 == 0:
            add(("ffn1", l), D * 2 * D_FF)
            add(("ffn2", l), D_FF * D)
        else:
            for e in range(NE):
                add(("moe1", l, e), D * 2 * D_FFE)
                add(("moe2", l, e), D_FFE * D)
    nch = (off + CHUNK_ELEMS - 1) // CHUNK_ELEMS
    return lay, off, nch


def w_in_perm():
    cols = []
    for fo in range(4):
        cols += list(range(fo * 64, fo * 64 + 64)) + list(range((4 + fo) * 64, (4 + fo) * 64 + 64))
    q_end = 512
    kc, vc, ks, vs, kw, vw = [q_end + i * 128 for i in range(6)]
    gl = q_end + 6 * 128
    cv = gl + 24
    cols += list(range(kc, kc + 128)) + list(range(vc, vc + 128)) + list(range(ks, ks + 128)) + list(range(kw, kw + 128))
    cols += list(range(cv, cv + 1024))
    cols += list(range(vs, vs + 128)) + list(range(vw, vw + 128)) + list(range(gl, gl + 24))
    assert len(cols) == D_IN
    return np.asarray(cols)


def pack_flat(inp, layers):
    lay, total, nch = flat_layout(layers)
    flat = np.zeros(nch * CHUNK_ELEMS, np.float32)

    def put(key, arr):
        a = np.ascontiguousarray(arr, dtype=np.float32).reshape(-1)
        flat[lay[key]:lay[key] + a.size] = a

    perm = w_in_perm()
    for l in layers:
        put(("ada", l), inp["ada_w"][l])
        put(("w_in", l), np.asarray(inp["w_in"][l])[:, perm])
        put(("cmp_w1", l), inp["cmp_w1"][l])
        put(("w_out", l), inp["w_out"][l])
        if l % 2 == 0:
            put(("ffn1", l), inp["ffn_w1"][l // 2])
            put(("ffn2", l), inp["ffn_w2"][l // 2])
        else:
            for e in range(NE):
                put(("moe1", l, e), inp["moe_w1"][l // 2, e])
                put(("moe2", l, e), inp["moe_w2"][l // 2, e])
    return flat.reshape(nch, 8, 512, 1024), nch


def rel_bucket_np(dist):
    n = np.maximum(dist, 0)
    nf = np.maximum(n, 1).astype(np.float32)
    large = 16 + (np.log(nf / np.float32(16)) / np.float32(math.log(128 / 16)) * np.float32(16)).astype(np.int32)
    large = np.minimum(large, 31)
    return np.where(n < 16, n, large)


def host_consts():
    c = {}
    n = np.arange(NCMP)[:, None]
    q = np.arange(T)[None, :]
    dist = q - (16 * n + 31)
    c["bk_c"] = np.where(dist >= 0, rel_bucket_np(dist), 32).astype(np.float32)
    k = np.arange(128)[:, None]
    qq = np.arange(128)[None, :]
    d0 = qq - k
    c["bk_d0"] = np.where(d0 >= 0, rel_bucket_np(d0), 32).astype(np.float32)
    c["bk_d1"] = rel_bucket_np(128 + qq - k).astype(np.float32)
    c["edge"] = np.where(qq >= k, NEG, 0.0).astype(np.float32)
    j = np.arange(32)[:, None]
    kk = np.arange(T)[None, :]
    c["emat"] = (kk // 64 == j).astype(np.float32)
    tb = (np.arange(T) // 64)[:, None]
    jj = np.arange(32)[None, :]
    fm = np.where(jj > tb, -1e9, np.where((jj == 0) | (jj == tb) | (jj == tb - 1), 1e9, 0.0))
    c["fm"] = fm.astype(np.float32)
    starts = 16 * np.arange(NCMP)[:, None]
    bstart = 64 * np.arange(32)[None, :]
    c["ov"] = ((starts < bstart + 64) & (starts + 32 > bstart)).astype(np.float32)
    c["ident"] = np.eye(128, dtype=np.float32)
    return c
SB_BASE = 16512
SB_END = 229376


def build(cfg):
    NSEQ = cfg["nseq"]
    layers = cfg["layers"]
    gather = cfg["gather"]
    lay, total, NCH = flat_layout(layers)
    nc = bass.Bass("TRN2", target_bir_lowering=False)
    P = Prog(nc)
    NT = NSEQ * T

    def dram_in(name, shape, dt=F32):
        return nc.dram_tensor(name, list(shape), dt, kind="ExternalInput").ap()

    x_in = dram_in("x", [NT, D])
    c_in = dram_in("c", [NSEQ, D])
    if gather:
        wsh = dram_in("wsh", [NCH * 512, 1024])
    else:
        wfull = dram_in("wfull", [NCH * 4096, 1024])
    cmp_pos = dram_in("cmp_pos", [4, 2, 32, 64])
    cmp_w2 = dram_in("cmp_w2", [4, 2, 64, 64])
    conv_w = dram_in("conv_w", [4, 31, 512])
    conv_b = dram_in("conv_b", [4, 512])
    conv_g = dram_in("conv_ln_g", [4, 512])
    conv_be = dram_in("conv_ln_b", [4, 512])
    rel_bias = dram_in("rel_bias", [1, 256])
    ada_b = dram_in("ada_b", [4, 6144])
    ln_g = dram_in("ln_g", [4, 2, 1024])
    ln_b = dram_in("ln_b", [4, 2, 1024])
    router_w = dram_in("router_w", [2, 1024, 8])
    bk_c = dram_in("bk_c", [NCMP, T])
    bk_d0 = dram_in("bk_d0", [128, 128])
    bk_d1 = dram_in("bk_d1", [128, 128])
    edge_in = dram_in("edge", [128, 128])
    emat_in = dram_in("emat", [32, T])
    fm_in = dram_in("fm", [T, 32])
    ov_in = dram_in("ov", [NCMP, 32])
    ident_in = dram_in("ident", [128, 128])
    out = nc.dram_tensor("out", [NT, D], F32, kind="ExternalOutput").ap()

    NSEG = (NCH + SEG_CH - 1) // SEG_CH
    wflat_t = [nc.dram_tensor(f"wflat{i}", [min(SEG_CH, NCH - i * SEG_CH) * 4096, 1024], BF16).ap() for i in range(NSEG)]
    xa = nc.dram_tensor("xa", [NT, D], F32).ap()
    xb = nc.dram_tensor("xb", [NT, D], F32).ap()
    modrow = nc.dram_tensor("modrow", [len(layers) * NSEQ, 6144], F32).ap()
    bc_dram = nc.dram_tensor("bc_dram", [8 * 128, T], BF16).ap()
    b_xa, b_xb, b_out, b_modrow, b_bc = Buf("xa"), Buf("xb"), Buf("outd"), Buf("modrow"), Buf("bcd")

    b_wflat = Buf("wflat")
    b_cc = Buf("cc")
    chunk_ready = {}
    P.nobarrier.update(["cc", "ib0", "wflat"])
    if gather:
        ib = nc.dram_tensor("ib", [NCH * 512, 1024], BF16).ap()
        ob1 = nc.dram_tensor("ob1", [NCH * 2048, 1024], BF16).ap()
        NP_ = (NCH * 512 + 2047) // 2048
        b_ibp = [Buf(f"ib{i}") for i in range(NP_)]
        cast_done = set()

        def cast(pi):
            if pi in cast_done or pi >= NP_:
                return
            cast_done.add(pi)
            i = pi * 2048
            j = min(i + 2048, NCH * 512)
            P.dma("pool", ib[i:j, :], wsh[i:j, :], writes=[b_ibp[pi]], owner=b_ibp[0], max_dma_last_dim=4096)

        s1idx = {}

        def s1(ch):
            cast(ch // 4)
            fn = lambda eng, ch=ch: eng.collective_compute(
                "AllGather", ALU.bypass, replica_groups=[[0, 1, 2, 3], [4, 5, 6, 7]],
                ins=[ib[ch * 512:(ch + 1) * 512, :]], outs=[ob1[ch * 2048:(ch + 1) * 2048, :]])
            P.dma("pool", None, None, reads=[b_ibp[ch // 4]], writes=[], owner=b_cc, fn=fn, inc=1)
            s1idx[ch] = b_cc.semcnt

        def s2(ch, hf):
            fn = lambda eng, ch=ch, hf=hf: eng.collective_compute(
                "AllGather", ALU.bypass, replica_groups=[[0, 4], [1, 5], [2, 6], [3, 7]],
                ins=[ob1[ch * 2048 + hf * 1024:ch * 2048 + (hf + 1) * 1024, :]],
                outs=[wflat_t[ch // SEG_CH][(2 * (ch % SEG_CH) + hf) * 2048:(2 * (ch % SEG_CH) + hf + 1) * 2048, :]])
            P.dma("pool", None, None, reads=[], writes=[], owner=b_cc, fn=fn, inc=1,
                  extra=[("d", b_cc, s1idx[ch])])

        s1(0)
        if NCH > 1:
            s1(1)
        for ch in range(NCH):
            s2(ch, 0)
            s2(ch, 1)
            chunk_ready[ch] = b_cc.semcnt
            if ch + 2 < NCH:
                s1(ch + 2)
            cast((ch + 6) // 4)
    else:
        for i in range(0, NCH * 4096, 2048):
            sg, li_ = divmod(i, SEG_CH * 4096)
            P.dma("pool", wflat_t[sg][li_:li_ + 2048, :], wfull[i:i + 2048, :], pwrites=[b_wflat], max_dma_last_dim=4096)

    flat1 = [w_.rearrange("r c -> (r c)") for w_ in wflat_t]

    def wview(key, K, N, sub=0):
        sg, o = divmod(lay[key] + sub, SEG_ELEMS)
        return flat1[sg][o:o + K * N].rearrange("(k n) -> k n", n=N)

    def wdep(key, nelem):
        if gather:
            ch = (lay[key] + nelem - 1) // CHUNK_ELEMS
            return dict(extra=[("d", b_cc, chunk_ready[ch])])
        return dict(reads=[b_wflat])

    _cache = {}
    _bufs = {}

    def getbuf(name):
        if name not in _bufs:
            _bufs[name] = Buf(name)
        return _bufs[name]

    class Arena:
        def __init__(self, base, end):
            self.base, self.off, self.end = base, base, end

        def alloc(self, name, shape, dt):
            esz = 4 if dt == F32 else 2
            n = esz
            for s in shape[1:]:
                n *= s
            n = (n + 63) // 64 * 64
            assert self.off + n <= self.end, (name, self.off, n, self.end)
            if name in _cache:
                ap_, off_ = _cache[name]
                assert off_ == self.off, (name, off_, self.off)
            else:
                ap_ = nc.alloc_sbuf_tensor_at(name, list(shape), dt, offset=self.off).ap()
                _cache[name] = (ap_, self.off)
            self.off += n
            return ap_, getbuf(name)

    AP_ = Arena(SB_BASE, SB_END)
    al = AP_.alloc
    ident_f, b_identf = al("ident_f", [128, 128], F32)
    ident_b, b_identb = al("ident_b", [128, 128], BF16)
    onesm, b_onesm = al("onesm", [128, 128], F32)
    emat_b, b_emat = al("emat_b", [128, T], BF16)
    fm_sb, b_fm = al("fm_sb", [128, 16, 32], F32)
    edge_b, b_edge = al("edge_b", [128, 128], BF16)
    bt0, b_bt0 = al("bt0", [128, 8, 128], BF16)
    bt1, b_bt1 = al("bt1", [128, 8, 128], BF16)
    relb, b_relb = al("relb", [128, 32, 8], F32)
    relc, b_relc = al("relc", [128, 32, 8], F32)
    modcol, b_modcol = al("modcol", [128, NSEQ, 6, 8], F32)
    scT, b_scT = al("scT", [128, 8, NSEQ], BF16)
    vcmp, b_vcmp = al("vcmp", [128, 2, 97], BF16)
    convw, b_convw = al("convw", [128, 4, 31], F32)
    convp, b_convp = al("convp", [128, 3, 4], F32)
    w1sb, b_w1sb = al("w1sb", [128, 2, 32, 64], BF16)
    posT, b_posT = al("posT", [128, 2, 34], BF16)
    posTf, b_posTf = al("posTf", [128, 2, 32], F32)
    w2sb, b_w2sb = al("w2sb", [128, 2, 64], BF16)
    w2f, b_w2f = al("w2f", [128, 2, 64], F32)
    c1col, b_c1col = al("c1col", [128, 2], F32)
    rw_sb, b_rw = al("rw_sb", [128, 8, 8], F32)
    epsc, b_epsc = al("epsc", [128, 1], F32)
    stgT, b_stgT = al("stgT", [128, 128], F32)
    bcast = [al(f"bcast{i}", [128, 1024], F32) for i in range(3)]
    xt = [al(f"xt{i}", [128, 1024], F32) for i in range(2)]
    tt_ = [al(f"tt{i}", [128, 1024], F32) for i in range(2)]
    we_sb, b_we = al("we_sb", [128, 16, 8], F32)
    st_bn, b_stbn = al("st_bn", [128, 2, 6], F32)
    st_mv, b_stmv = al("st_mv", [128, 4], F32)
    sm8, b_sm8 = al("sm8", [128, 16], F32)
    PH = AP_.off

    banks = []
    for i in range(8):
        banks.append((nc.alloc_psum_tensor(f"bank{i}", [128, 512], F32).ap(), Buf(f"bank{i}", excl=True)))

    def A(k, f, reads=(), writes=(), pwrites=(), extra=()):
        P.op(k, f, reads=reads, writes=writes, pwrites=pwrites, extra=extra)

    def load_T(dst, b_dst, n, srcs, full=False):
        A("dve", lambda e: e.memset(stgT, 0.0), [], [b_stgT])
        for cs_, rs_, ap_ in srcs:
            P.dma("sp", stgT[rs_, cs_], ap_, pwrites=[b_stgT])
        n2 = (n + 1) // 2 * 2
        p4, bp4 = banks[4]
        A("pe", lambda e: e.transpose(out=p4[:, 0:n2], in_=stgT[0:n2, :], identity=ident_f[0:n2, 0:n2]), [b_stgT, b_identf], [bp4])
        if full:
            A("dve", lambda e: e.tensor_copy(out=dst, in_=p4[:, 0:n]), [bp4], [b_dst])
        else:
            A("dve", lambda e: e.tensor_copy(out=dst, in_=p4[:, 0:n]), [bp4], pwrites=[b_dst])

    MA = Arena(PH, SB_END)
    P.dma("sp", ident_f, ident_in, writes=[b_identf])
    A("dve", lambda e: e.tensor_copy(out=ident_b, in_=ident_f), [b_identf], [b_identb])
    A("dve", lambda e: e.memset(onesm, 1.0 / 512.0), [], [b_onesm])
    A("dve", lambda e: e.memset(epsc, LN_EPS), [], [b_epsc])
    stg, b_stg = MA.alloc("stg", [128, T], F32)
    P.dma("sp", stg[0:32, :], emat_in, writes=[b_stg])
    A("dve", lambda e: e.memset(emat_b, 0.0), [], [b_emat])
    A("dve", lambda e: e.tensor_copy(out=emat_b[0:32, :], in_=stg[0:32, :]), [b_stg], [b_emat])
    P.dma("sp", fm_sb, fm_in.rearrange("(t p) j -> p t j", p=128), writes=[b_fm])
    stg2, b_stg2 = MA.alloc("stg2", [128, 128], F32)
    P.dma("sp", stg2, edge_in, writes=[b_stg2])
    A("dve", lambda e: e.tensor_copy(out=edge_b, in_=stg2), [b_stg2], [b_edge])
    P.dma("sp", relb.rearrange("p b h -> p (b h)"), rel_bias.partition_broadcast(128).rearrange("p o n -> p (o n)"), writes=[b_relb])
    for b in range(32):
        A("dve", lambda e, b=b: e.tensor_tensor(out=relc[:, b, :], in0=relb[:, b, :], in1=relb[:, 31, :], op=ALU.subtract), [b_relb], [b_relc])
    stg3, b_stg3 = MA.alloc("stg3", [128, 32], F32)
    P.dma("sp", stg3[0:NCMP, :], ov_in, writes=[b_stg3])
    A("dve", lambda e: e.memset(vcmp, 1.0), [], [b_vcmp])
    for g in range(2):
        A("dve", lambda e, g=g: e.tensor_copy(out=vcmp[0:NCMP, g, 65:97], in_=stg3[0:NCMP, :]), [b_stg3], [b_vcmp])
    bkt, b_bkt = MA.alloc("bkt", [128, 2, 128], F32)
    P.dma("sp", bkt[:, 0, :], bk_d0, pwrites=[b_bkt])
    P.dma("sp", bkt[:, 1, :], bk_d1, pwrites=[b_bkt])
    acc01, b_acc01 = MA.alloc("acc01", [128, 2, 8, 128], F32)
    msk, b_msk = MA.alloc("msk", [128, 2, 128], F32)
    A("dve", lambda e: e.tensor_scalar(out=msk, in0=bkt, scalar1=32.0, scalar2=NEG, op0=ALU.is_equal, op1=ALU.mult), [b_bkt], [b_msk])
    for h in range(8):
        A("dve", lambda e, h=h: e.tensor_copy(out=acc01[:, :, h, :], in_=msk), [b_msk], [b_acc01])
    for b in range(31):
        A("dve", lambda e, b=b: e.tensor_scalar(out=msk, in0=bkt, scalar1=float(b), scalar2=None, op0=ALU.is_equal), [b_bkt], [b_msk])
        for h in range(8):
            A("dve", lambda e, b=b, h=h: e.scalar_tensor_tensor(out=acc01[:, :, h, :], in0=msk, scalar=relc[:, b, h:h + 1], in1=acc01[:, :, h, :],
                                                              op0=ALU.mult, op1=ALU.add), [b_msk, b_relc, b_acc01], [b_acc01])
    A("dve", lambda e: e.tensor_copy(out=bt0, in_=acc01[:, 0, :, :]), [b_acc01], [b_bt0])
    A("dve", lambda e: e.tensor_copy(out=bt1, in_=acc01[:, 1, :, :]), [b_acc01], [b_bt1])
    bkc, b_bkc = MA.alloc("bkc", [128, T], F32)
    P.dma("sp", bkc[0:NCMP, :], bk_c, writes=[b_bkc])
    mskc, b_mskc = MA.alloc("mskc", [128, T], F32)
    accc, b_accc = MA.alloc("accc", [128, 8, T], F32)
    A("dve", lambda e: e.tensor_scalar(out=mskc[0:NCMP], in0=bkc[0:NCMP], scalar1=32.0, scalar2=NEG, op0=ALU.is_equal, op1=ALU.mult), [b_bkc], [b_mskc])
    for h in range(8):
        A("dve", lambda e, h=h: e.tensor_copy(out=accc[0:NCMP, h, :], in_=mskc[0:NCMP]), [b_mskc], [b_accc])
    for b in range(31):
        A("dve", lambda e, b=b: e.tensor_scalar(out=mskc[0:NCMP], in0=bkc[0:NCMP], scalar1=float(b), scalar2=None, op0=ALU.is_equal), [b_bkc], [b_mskc])
        for h in range(8):
            A("dve", lambda e, b=b, h=h: e.scalar_tensor_tensor(out=accc[0:NCMP, h, :], in0=mskc[0:NCMP], scalar=relc[0:NCMP, b, h:h + 1], in1=accc[0:NCMP, h, :],
                                                              op0=ALU.mult, op1=ALU.add), [b_mskc, b_relc, b_accc], [b_accc])
    bcb, b_bcb = MA.alloc("bcb", [128, 8, T], BF16)
    A("act", lambda e: e.activation(out=bcb[0:NCMP], in_=accc[0:NCMP], func=AF.Copy), [b_accc], [b_bcb])
    for h in range(8):
        P.dma("sp", bc_dram[h * 128:h * 128 + NCMP, :], bcb[0:NCMP, h, :], reads=[b_bcb], pwrites=[b_bc], owner=b_bcb)
    csb, b_csb = MA.alloc("csb", [128, D], F32)
    A("dve", lambda e: e.memset(csb[0:32, :], 0.0), [], [b_csb])
    P.dma("sp", csb[0:NSEQ, :], c_in, writes=[b_csb])
    A("act", lambda e: e.activation(out=csb[0:32, :], in_=csb[0:32, :], func=AF.Silu), [b_csb], [b_csb])
    pb, b_pb = banks[4]
    for kc in range(8):
        A("pe", lambda e, kc=kc: e.transpose(out=pb[:, kc * 32:(kc + 1) * 32], in_=csb[0:32, kc * 128:(kc + 1) * 128], identity=ident_f[0:32, 0:32]),
          [b_csb, b_identf], [b_pb] if kc == 0 else (), pwrites=() if kc == 0 else [b_pb])
    A("dve", lambda e: e.tensor_copy(out=scT, in_=pb[:, 0:256].rearrange("p (k s) -> p k s", s=32)[:, :, 0:NSEQ]), [b_pb], [b_scT])
    P.barrier()
    def rowb(dst, b_dst, src_row, reads=()):
        P.dma("sp", dst, src_row.partition_broadcast(128).rearrange("p o n -> p (o n)"), reads=list(reads), writes=[b_dst])

    def layer_norm_tile(tt, b_tt, lng, b_lng, lnb, b_lnb):
        for hf in range(2):
            A("dve", lambda e, hf=hf: e.bn_stats(out=st_bn[:, hf, :], in_=tt[:, hf * 512:(hf + 1) * 512]), [b_tt], pwrites=[b_stbn])
        A("dve", lambda e: e.bn_aggr(out=st_mv[:, 0:2], in_=st_bn.rearrange("p a b -> p (a b)")), [b_stbn], [b_stmv])
        A("act", lambda e: e.activation(out=st_mv[:, 2:3], in_=st_mv[:, 1:2], func=AF.Sqrt, bias=epsc[:, 0:1]), [b_stmv, b_epsc], [b_stmv])
        A("dve", lambda e: e.reciprocal(out=st_mv[:, 3:4], in_=st_mv[:, 2:3]), [b_stmv], [b_stmv])
        A("dve", lambda e: e.tensor_scalar(out=tt, in0=tt, scalar1=st_mv[:, 0:1], scalar2=st_mv[:, 3:4], op0=ALU.subtract, op1=ALU.mult), [b_tt, b_stmv], [b_tt])
        A("dve", lambda e: e.tensor_tensor(out=tt, in0=tt, in1=lng, op=ALU.mult), [b_tt, b_lng], [b_tt])
        A("dve", lambda e: e.tensor_tensor(out=tt, in0=tt, in1=lnb, op=ALU.add), [b_tt, b_lnb], [b_tt])

    NS = 0.125

    def ada_phase(li, l):
        R = Arena(PH, SB_END)
        adab, b_adab = R.alloc("adab", [128, 6144], F32)
        mrow, b_mrow = R.alloc("mrow", [128, 6144], F32)
        wp = [R.alloc(f"adaw{i}", [128, 8, 512], BF16) for i in range(2)]
        P.dma("sp", adab[0:NSEQ, :], ada_b[l:l + 1, :].partition_broadcast(NSEQ).rearrange("p o n -> p (o n)"), writes=[b_adab])
        wv = wview(("ada", l), D, 6144)
        for j in range(12):
            t, bt = wp[j % 2]
            P.dma("sp", t, wv[:, j * 512:(j + 1) * 512].rearrange("(kc p) n -> p kc n", p=128), writes=[bt], **wdep(("ada", l), D * 6144))
            ps, bps = banks[j % 2]
            for kc in range(8):
                A("pe", lambda e, kc=kc, t=t, ps=ps: e.matmul(ps[0:NSEQ, :], lhsT=scT[:, kc, :], rhs=t[:, kc, :], start=(kc == 0), stop=(kc == 7)),
                  [b_scT, bt], [bps] if kc == 0 else (), pwrites=() if kc == 0 else [bps])
            A("dve", lambda e, j=j, ps=ps: e.tensor_tensor(out=mrow[0:NSEQ, j * 512:(j + 1) * 512], in0=ps[0:NSEQ, :], in1=adab[0:NSEQ, j * 512:(j + 1) * 512], op=ALU.add),
              [bps, b_adab], pwrites=[b_mrow])
        for seg in (1, 2, 4, 5):
            A("dve", lambda e, seg=seg: e.tensor_scalar(out=mrow[0:NSEQ, seg * 1024:(seg + 1) * 1024], in0=mrow[0:NSEQ, seg * 1024:(seg + 1) * 1024],
                                                        scalar1=1.0, scalar2=None, op0=ALU.add), [b_mrow], [b_mrow])
        P.dma("sp", modrow[li * NSEQ:(li + 1) * NSEQ, :], mrow[0:NSEQ, :], reads=[b_mrow], writes=[b_modrow], owner=b_mrow)
        P.barrier()
        for s in range(NSEQ):
            load_T(modcol[:, s, :, :].rearrange("p a b -> p (a b)"), b_modcol, 48,
                   [(slice(0, 128), slice(0, 48), modrow[li * NSEQ + s, :].rearrange("(r p) -> r p", p=128))])

    def mixer_phase(li, l, s, xsrc, b_xsrc, xdst, b_xdst):
        R1 = Arena(PH, SB_END)
        w_in_sb, b_win = R1.alloc("w_in_sb", [128, 8, D_IN], BF16)
        hT = [R1.alloc(f"hT{i}", [128, 8, 512], BF16) for i in range(2)]
        KcT, b_KcT = R1.alloc("KcT", [128, T], BF16)
        VcT, b_VcT = R1.alloc("VcT", [128, T], BF16)
        sig, b_sig = R1.alloc("sig", [128, 512], F32)
        ov_end = R1.off
        w_out_sb, b_wout = R1.alloc("w_out_sb", [128, 8, D], BF16)
        QT, b_QT = R1.alloc("QT", [128, 4, T], BF16)
        KsP = [R1.alloc(f"KsP{g}", [128, T], BF16) for g in range(2)]
        KwP = [R1.alloc(f"KwP{g}", [128, T], BF16) for g in range(2)]
        Vs_aug, b_Vs = R1.alloc("Vs_aug", [128, 16, 2, 65], BF16)
        Vw_aug, b_Vw = R1.alloc("Vw_aug", [128, 16, 2, 65], BF16)
        gsb, b_gsb = R1.alloc("gsb", [128, 16, 24], F32)
        yT, b_yT = R1.alloc("yT", [128, 4, 30 + T], BF16)
        convacc, b_cacc = R1.alloc("convacc", [128, 4, 512], F32)
        negselT, b_nsT = R1.alloc("negselT", [128, 2, 512], BF16)
        KcmpT, b_KcmpT = R1.alloc("KcmpT", [128, 2, 128], BF16)
        Gt, b_G = R1.alloc("Gt", [128, 128], BF16)
        dg = [R1.alloc(f"dg{i}", [128, 8, 128], BF16) for i in range(2)]
        R2 = Arena(PH, ov_end)
        o_nsaT, b_onT = R2.alloc("o_nsaT", [128, 4, T], BF16)
        o_convT, b_ocT = R2.alloc("o_convT", [128, 4, T], BF16)
        o_nsa, b_on = R2.alloc("o_nsa", [128, 4, 512], F32)
        Osb = [R2.alloc(f"Osb{i}", [128, 512], F32) for i in range(2)]
        PTb = [R2.alloc(f"PT{i}", [128, 512], BF16) for i in range(3)]
        Bcs = [R2.alloc(f"Bcs{i}", [128, 512], BF16) for i in range(2)]
        convsq = [R2.alloc(f"convsq{i}", [128, 512], F32) for i in range(2)]
        s_mean, b_smean = R2.alloc("s_mean", [128, 512], F32)
        s_var, b_svar = R2.alloc("s_var", [128, 512], F32)
        s_rstd, b_srstd = R2.alloc("s_rstd", [128, 512], F32)
        impacc, b_imp = R2.alloc("impacc", [128, 4, 2, 32], F32)
        score, b_score = R2.alloc("score", [128, 32], F32)
        negsel, b_negsel = R2.alloc("negsel", [128, 32], F32)
        rz, b_rz = R2.alloc("rz", [128, 4], F32)
        coef, b_coef = R2.alloc("coef", [128, 4], F32)
        print("mixer sbuf: R1 end", R1.off, "R2 end", R2.off, "ov_end", ov_end, "limit", SB_END)

        wv = wview(("w_in", l), D, D_IN)
        for kc in range(8):
            P.dma("sp", w_in_sb[:, kc, :], wv[kc * 128:(kc + 1) * 128, :], pwrites=[b_win], **wdep(("w_in", l), D * D_IN))
        wo = wview(("w_out", l), D, D)
        P.dma("sp", w_out_sb, wo.rearrange("(kc p) n -> p kc n", p=128), writes=[b_wout], **wdep(("w_out", l), D * D))
        cw = wview(("cmp_w1", l), 2 * 2048, 64).rearrange("(kv l d) o -> d kv l o", kv=2, l=32)
        for g in range(2):
            for kv in range(2):
                P.dma("sp", w1sb[g * 64:(g + 1) * 64, kv, :, :], cw[:, kv, :, :], pwrites=[b_w1sb], **wdep(("cmp_w1", l), 2 * 2048 * 64))
            P.dma("sp", w2f[g * 64:(g + 1) * 64, :, :], cmp_w2[l].rearrange("kv o p -> o kv p"), pwrites=[b_w2f])
        A("dve", lambda e: e.memset(posT, 0.0), [], [b_posT])
        for kv in range(2):
            load_T(posTf[:, kv, :], b_posTf, 32, [(slice(0, 64), slice(0, 32), cmp_pos[l, kv]), (slice(64, 128), slice(0, 32), cmp_pos[l, kv])])
        A("dve", lambda e: e.tensor_copy(out=posT[:, :, 0:32], in_=posTf), [b_posTf], [b_posT])
        A("dve", lambda e: e.tensor_copy(out=w2sb, in_=w2f), [b_w2f], [b_w2sb])
        for c4 in range(4):
            load_T(convw[:, c4, :], b_convw, 31, [(slice(0, 128), slice(0, 31), conv_w[l][:, c4 * 128:(c4 + 1) * 128])])
        load_T(convp.rearrange("p a b -> p (a b)"), b_convp, 12,
               [(slice(0, 128), slice(4 * i, 4 * i + 4), src[l, :].rearrange("(c p) -> c p", p=128)) for i, src in enumerate((conv_b, conv_g, conv_be))], full=True)
        A("dve", lambda e: e.memset(Vs_aug, 1.0), [], [b_Vs])
        A("dve", lambda e: e.memset(Vw_aug, 1.0), [], [b_Vw])
        A("dve", lambda e: e.memset(yT[:, :, 0:30], 0.0), [], [b_yT])
        for g in range(2):
            A("dve", lambda e, g=g: e.memset(KsP[g][0], 0.0), [], [KsP[g][1]])
            A("dve", lambda e, g=g: e.memset(KwP[g][0], 0.0), [], [KwP[g][1]])
        A("dve", lambda e: e.memset(KcmpT, 0.0), [], [b_KcmpT])
        A("dve", lambda e: e.memset(negselT, 0.0), [], [b_nsT])

        if cfg.get("stop") == "loads":
            P.barrier()
            return
        def p1_T(tc):
            hTt, b_hTt = hT[tc % 2]
            cs = slice(tc * 512, (tc + 1) * 512)
            for t4 in range(4):
                tile = tc * 4 + t4
                xtile, b_x = xt[tile % 2]
                r0 = s * T + tile * 128
                P.dma("sp", xtile, xsrc[r0:r0 + 128, :], reads=[b_xsrc], writes=[b_x])
                for half in range(2):
                    ps, bps = banks[half]
                    for k4 in range(4):
                        kc = half * 4 + k4
                        A("pe", lambda e, ps=ps, k4=k4, kc=kc, xtile=xtile: e.transpose(out=ps[:, k4 * 128:(k4 + 1) * 128], in_=xtile[:, kc * 128:(kc + 1) * 128], identity=ident_f),
                          [b_x, b_identf], [bps] if k4 == 0 else (), pwrites=() if k4 == 0 else [bps])
                    for k4 in range(4):
                        kc = half * 4 + k4
                        o_ap = hTt[:, kc, t4 * 128:(t4 + 1) * 128]
                        i_ap = ps[:, k4 * 128:(k4 + 1) * 128]
                        if half == 0:
                            A("dve", lambda e, o_ap=o_ap, i_ap=i_ap, kc=kc: e.tensor_scalar(out=o_ap, in0=i_ap, scalar1=modcol[:, s, 1, kc:kc + 1], scalar2=modcol[:, s, 0, kc:kc + 1],
                                                                                         op0=ALU.mult, op1=ALU.add), [bps, b_modcol], pwrites=[b_hTt])
                        else:
                            A("act", lambda e, o_ap=o_ap, i_ap=i_ap, kc=kc: e.activation(out=o_ap, in_=i_ap, func=AF.Identity, scale=modcol[:, s, 1, kc:kc + 1], bias=modcol[:, s, 0, kc:kc + 1]),
                              [bps, b_modcol], pwrites=[b_hTt])


        def p1_P(tc):
            hTt, b_hTt = hT[tc % 2]
            cs = slice(tc * 512, (tc + 1) * 512)
            def proj(fo, ps, bps):
                for kc in range(8):
                    A("pe", lambda e, kc=kc: e.matmul(ps, lhsT=w_in_sb[:, kc, fo * 128:(fo + 1) * 128], rhs=hTt[:, kc, :], start=(kc == 0), stop=(kc == 7)),
                      [b_win, b_hTt], [bps] if kc == 0 else (), pwrites=() if kc == 0 else [bps])

            for fo in range(8):
                ps, bps = banks[2 + fo % 2]
                proj(fo, ps, bps)
                if fo < 4:
                    A("act", lambda e, fo=fo, ps=ps: e.activation(out=QT[:, fo, cs], in_=ps, func=AF.Copy, scale=NS), [bps], pwrites=[b_QT])
                elif fo < 6:
                    dst, bd = ((KcT, b_KcT), (VcT, b_VcT))[fo - 4]
                    A("dve", lambda e, dst=dst, ps=ps: e.tensor_copy(out=dst[:, cs], in_=ps), [bps], pwrites=[bd])
                else:
                    KP = KsP if fo == 6 else KwP
                    for g in range(2):
                        A("dve", lambda e, g=g, KP=KP, ps=ps: e.tensor_copy(out=KP[g][0][g * 64:(g + 1) * 64, cs], in_=ps[g * 64:(g + 1) * 64, :]), [bps], pwrites=[KP[g][1]])
            for c in range(4):
                pa, bpa = banks[2 if c % 2 == 0 else 5]
                pg, bpg = banks[3 if c % 2 == 0 else 6]
                proj(8 + c, pa, bpa)
                proj(12 + c, pg, bpg)
                A("act", lambda e, pg=pg: e.activation(out=sig, in_=pg, func=AF.Sigmoid), [bpg], [b_sig])
                A("dve", lambda e, c=c, pa=pa: e.tensor_tensor(out=yT[:, c, 30 + tc * 512:30 + (tc + 1) * 512], in0=pa, in1=sig, op=ALU.mult), [bpa, b_sig], pwrites=[b_yT])
            for t4 in range(4):
                tile = tc * 4 + t4
                ps, bps = banks[7]
                for kc in range(8):
                    A("pe", lambda e, kc=kc, t4=t4, ps=ps: e.matmul(ps[:, 0:280], lhsT=hTt[:, kc, t4 * 128:(t4 + 1) * 128], rhs=w_in_sb[:, kc, 2048:2328], start=(kc == 0), stop=(kc == 7)),
                      [b_win, b_hTt], [bps] if kc == 0 else (), pwrites=() if kc == 0 else [bps])
                A("dve", lambda e, tile=tile, ps=ps: e.tensor_copy(out=Vs_aug[:, tile, :, 0:64], in_=ps[:, 0:128].rearrange("p (g d) -> p g d", g=2)), [bps], pwrites=[b_Vs])
                A("dve", lambda e, tile=tile, ps=ps: e.tensor_copy(out=Vw_aug[:, tile, :, 0:64], in_=ps[:, 128:256].rearrange("p (g d) -> p g d", g=2)), [bps], pwrites=[b_Vw])
                A("act", lambda e, tile=tile, ps=ps: e.activation(out=gsb[:, tile, :], in_=ps[:, 256:280], func=AF.Sigmoid), [bps], pwrites=[b_gsb])


        p1_T(0)
        for tc in range(4):
            if tc + 1 < 4:
                p1_T(tc + 1)
            p1_P(tc)
        if cfg.get("stop") == "p1":
            P.barrier()
            return
        for kv, (src, b_src) in enumerate(((KcT, b_KcT), (VcT, b_VcT))):
            for g in range(2):
                pbs = slice(g * 64, g * 64 + 64)
                psm, bpsm = banks[0 + 2 * g]
                psc, bpsc = banks[1 + 2 * g]
                for l_ in range(32):
                    A("pe", lambda e, l_=l_, pbs=pbs, psc=psc: e.matmul(psc[pbs, 0:2], lhsT=w1sb[pbs, kv, l_, :], rhs=posT[pbs, kv, l_:l_ + 2], start=(l_ == 0), stop=(l_ == 31)),
                      [b_w1sb, b_posT], [bpsc] if l_ == 0 else (), pwrites=() if l_ == 0 else [bpsc])
                for l_ in range(32):
                    A("pe", lambda e, l_=l_, pbs=pbs, psm=psm: e.matmul(psm[pbs, 0:NCMP], lhsT=w1sb[pbs, kv, l_, :], rhs=src[pbs, l_:l_ + 16 * 126 + 1:16], start=(l_ == 0), stop=(l_ == 31)),
                      [b_w1sb, b_src], [bpsm] if l_ == 0 else (), pwrites=() if l_ == 0 else [bpsm])
                A("dve", lambda e, pbs=pbs, psc=psc: e.tensor_copy(out=c1col[pbs, kv:kv + 1], in_=psc[pbs, 0:1]), [bpsc], pwrites=[b_c1col])
                A("act", lambda e, pbs=pbs, psm=psm: e.activation(out=Gt[pbs, 0:NCMP], in_=psm[pbs, 0:NCMP], func=AF.Gelu_apprx_tanh, bias=c1col[pbs, kv:kv + 1]), [bpsm, b_c1col], pwrites=[b_G])
            if kv == 0:
                for g in range(2):
                    pbs = slice(g * 64, g * 64 + 64)
                    ps2, bps2 = banks[4 + g]
                    A("pe", lambda e, pbs=pbs, ps2=ps2: e.matmul(ps2[pbs, 0:NCMP], lhsT=w2sb[pbs, 0, :], rhs=Gt[pbs, 0:NCMP], start=True, stop=True), [b_w2sb, b_G], [bps2])
                    A("dve", lambda e, pbs=pbs, ps2=ps2, g=g: e.tensor_copy(out=KcmpT[pbs, g, 0:NCMP], in_=ps2[pbs, 0:NCMP]), [bps2], pwrites=[b_KcmpT])
            else:
                for g in range(2):
                    pbs = slice(g * 64, g * 64 + 64)
                    ps3, bps3 = banks[6 + g]
                    A("pe", lambda e, pbs=pbs, ps3=ps3: e.matmul(ps3[0:NCMP, 0:64], lhsT=Gt[pbs, 0:NCMP], rhs=w2sb[pbs, 1, :], start=True, stop=True), [b_w2sb, b_G], [bps3])
                    A("dve", lambda e, g=g, ps3=ps3: e.tensor_copy(out=vcmp[0:NCMP, g, 0:64], in_=ps3[0:NCMP, 0:64]), [bps3], pwrites=[b_vcmp])
        P.barrier()
        if cfg.get("stop") == "p2":
            return

        def conv_ops(qc):
            ops = []
            base = qc * 512
            pc, bpc = banks[6]
            batches = [(0, 8), (8, 16), (16, 24), (24, 31)]

            def build(c, bi):
                j0, j1 = batches[bi]
                dgt, b_dg = dg[(c * 4 + bi) % 2]
                for j in range(j0, j1):
                    A("dve", lambda e: e.tensor_scalar(out=dgt[:, j - j0, :], in0=ident_b, scalar1=convw[:, c, j:j + 1], scalar2=None, op0=ALU.mult),
                      [b_identb, b_convw], [b_dg] if j == j0 else (), pwrites=() if j == j0 else [b_dg])

            def mm(c, bi):
                j0, j1 = batches[bi]
                dgt, b_dg = dg[(c * 4 + bi) % 2]
                for j in range(j0, j1):
                    A("pe", lambda e: e.matmul(pc, lhsT=dgt[:, j - j0, :], rhs=yT[:, c, base + j:base + j + 512], start=(j == 0), stop=(j == 30)),
                      [b_dg, b_yT], [bpc] if j == 0 else (), pwrites=() if j == 0 else [bpc])

            def evac(c):
                A("act", lambda e: e.activation(out=convacc[:, c, :], in_=pc, func=AF.Identity, bias=convp[:, 0, c:c + 1]), [bpc, b_convp], [b_cacc] if c == 0 else (), pwrites=() if c == 0 else [b_cacc])

            for c in range(4):
                ops.append(lambda c=c: build(c, 0))
                ops.append(lambda c=c: build(c, 1))
                ops.append(lambda c=c: mm(c, 0))
                ops.append(lambda c=c: build(c, 2))
                ops.append(lambda c=c: mm(c, 1))
                ops.append(lambda c=c: build(c, 3))
                ops.append(lambda c=c: mm(c, 2))
                ops.append(lambda c=c: (mm(c, 3), evac(c)))

            def stats():
                pm, bpm = banks[7]
                pe_, bpe = banks[6]
                for c in range(4):
                    sq, bsq = convsq[c % 2]
                    A("act", lambda e, c=c, sq=sq: e.activation(out=sq, in_=convacc[:, c, :], func=AF.Square), [b_cacc], [bsq])
                    A("pe", lambda e, c=c: e.matmul(pm, lhsT=onesm, rhs=convacc[:, c, :], start=(c == 0), stop=(c == 3)), [b_onesm, b_cacc], [bpm] if c == 0 else (), pwrites=() if c == 0 else [bpm])
                    A("pe", lambda e, c=c, sq=sq: e.matmul(pe_, lhsT=onesm, rhs=sq, start=(c == 0), stop=(c == 3)), [b_onesm, bsq], [bpe] if c == 0 else (), pwrites=() if c == 0 else [bpe])
                A("act", lambda e: e.activation(out=s_mean, in_=pm, func=AF.Copy), [bpm], [b_smean])
                A("dve", lambda e: e.tensor_tensor(out=s_var, in0=s_mean, in1=s_mean, op=ALU.mult), [b_smean], [b_svar])
                A("dve", lambda e: e.tensor_tensor(out=s_var, in0=pe_, in1=s_var, op=ALU.subtract), [bpe, b_svar], [b_svar])
                A("act", lambda e: e.activation(out=s_rstd, in_=s_var, func=AF.Sqrt, bias=epsc[:, 0:1]), [b_svar, b_epsc], [b_srstd])
                A("dve", lambda e: e.reciprocal(out=s_rstd, in_=s_rstd), [b_srstd], [b_srstd])
                for c in range(4):
                    A("dve", lambda e, c=c: e.tensor_tensor(out=convacc[:, c, :], in0=convacc[:, c, :], in1=s_mean, op=ALU.subtract), [b_cacc, b_smean], pwrites=[b_cacc])
                    A("dve", lambda e, c=c: e.tensor_tensor(out=convacc[:, c, :], in0=convacc[:, c, :], in1=s_rstd, op=ALU.mult), [b_cacc, b_srstd], pwrites=[b_cacc])
                    A("act", lambda e, c=c: e.activation(out=o_convT[:, c, base:base + 512], in_=convacc[:, c, :], func=AF.Silu, scale=convp[:, 1, c:c + 1], bias=convp[:, 2, c:c + 1]),
                      [b_cacc, b_convp], pwrites=[b_ocT])
            ops.append(stats)
            return ops

        cnt = {"o": 0, "s": 0}

        def finish_branch(h, br, W, qc, psO, bpsO):
            osb, b_osb = Osb[cnt["o"] % 2]
            cnt["o"] += 1
            W2 = W + 1
            A("act", lambda e: e.activation(out=osb[0:W, :], in_=psO[0:W, :], func=AF.Copy), [bpsO], [b_osb])
            pT, bpT = banks[4]
            for qt in range(4):
                A("pe", lambda e, qt=qt: e.transpose(out=pT[:, qt * 128:qt * 128 + W2], in_=osb[0:W2, qt * 128:(qt + 1) * 128], identity=ident_f[0:W2, 0:W2]),
                  [b_osb, b_identf], [bpT] if qt == 0 else (), pwrites=() if qt == 0 else [bpT])
            pT3 = pT.rearrange("p (q w) -> p q w", w=128)
            A("dve", lambda e: e.tensor_scalar(out=rz, in0=pT3[:, :, 64], scalar1=1e-30, scalar2=None, op0=ALU.max), [bpT], [b_rz])
            A("dve", lambda e: e.reciprocal(out=rz, in_=rz), [b_rz], [b_rz])
            A("dve", lambda e: e.tensor_tensor(out=coef, in0=rz, in1=gsb[:, qc * 4:(qc + 1) * 4, h * 3 + br], op=ALU.mult), [b_rz, b_gsb], [b_coef])
            for qt in range(4):
                o_ap = o_nsa[:, qt, h * 64:(h + 1) * 64]
                if br == 0:
                    A("dve", lambda e, qt=qt, o_ap=o_ap: e.tensor_scalar(out=o_ap, in0=pT3[:, qt, 0:64], scalar1=coef[:, qt:qt + 1], scalar2=None, op0=ALU.mult), [bpT, b_coef], pwrites=[b_on])
                else:
                    A("dve", lambda e, qt=qt, o_ap=o_ap: e.scalar_tensor_tensor(out=o_ap, in0=pT3[:, qt, 0:64], scalar=coef[:, qt:qt + 1], in1=o_ap, op0=ALU.mult, op1=ALU.add),
                      [bpT, b_coef], pwrites=[b_on])
                if br == 0:
                    g = h // 4
                    i_ap = impacc[:, qt, g, :]
                    if h % 4 == 0:
                        A("dve", lambda e, qt=qt, i_ap=i_ap: e.tensor_scalar(out=i_ap, in0=pT3[:, qt, 65:97], scalar1=rz[:, qt:qt + 1], scalar2=None, op0=ALU.mult), [bpT, b_rz], pwrites=[b_imp])
                    else:
                        A("dve", lambda e, qt=qt, i_ap=i_ap: e.scalar_tensor_tensor(out=i_ap, in0=pT3[:, qt, 65:97], scalar=rz[:, qt:qt + 1], in1=i_ap, op0=ALU.mult, op1=ALU.add),
                          [bpT, b_rz], pwrites=[b_imp])

        for qc in range(4):
            qs = slice(qc * 512, (qc + 1) * 512)
            cops = conv_ops(qc)
            per = (len(cops) + 23) // 24

            def drain(n):
                for _ in range(n):
                    if cops:
                        cops.pop(0)()

            if cfg.get("stop") == "att" and qc == 1:
                return
            pend = []

            def cmp_a(h):
                fo, g = h % 4, h // 4
                bcs, b_bcs = Bcs[h % 2]
                P.dma("sp", bcs[0:NCMP, :], bc_dram[h * 128:h * 128 + NCMP, qs], reads=[b_bc], writes=[b_bcs])
                psS, bpsS = banks[h % 2]
                A("pe", lambda e: e.matmul(psS[0:NCMP, :], lhsT=KcmpT[:, g, 0:NCMP], rhs=QT[:, fo, qs], start=True, stop=False, skip_group_check=True), [b_KcmpT, b_QT], [bpsS])
                A("pe", lambda e: e.matmul(psS[0:NCMP, :], lhsT=ident_b[0:NCMP, 0:NCMP], rhs=bcs[0:NCMP, :], start=False, stop=True, skip_group_check=True), [b_identb, b_bcs], pwrites=[bpsS])
                pt, b_pt = PTb[h % 2]
                A("act", lambda e: e.activation(out=pt[0:NCMP, :], in_=psS[0:NCMP, :], func=AF.Exp, bias=relb[0:NCMP, 31, h:h + 1]), [bpsS, b_relb], [b_pt])

            def cmp_b(h):
                g = h // 4
                pt, b_pt = PTb[h % 2]
                psO, bpsO = banks[2 + h % 2]
                A("pe", lambda e: e.matmul(psO[0:97, :], lhsT=vcmp[0:NCMP, g, :], rhs=pt[0:NCMP, :], start=True, stop=True), [b_vcmp, b_pt], [bpsO])
                pend.append(lambda: finish_branch(h, 0, 97, qc, psO, bpsO))

            cmp_a(0)
            for h in range(8):
                if h + 1 < 8:
                    cmp_a(h + 1)
                cmp_b(h)
                if len(pend) > 1:
                    pend.pop(0)()
                drain(per)
            while pend:
                pend.pop(0)()
            for g in range(2):
                p5, bp5 = banks[5]
                for qt in range(4):
                    A("dve", lambda e, qt=qt, g=g: e.tensor_tensor(out=score, in0=impacc[:, qt, g, :], in1=fm_sb[:, qc * 4 + qt, :], op=ALU.add), [b_imp, b_fm], [b_score])
                    A("dve", lambda e: e.max(out=sm8[:, 0:8], in_=score), [b_score], [b_sm8])
                    A("dve", lambda e: e.tensor_scalar(out=negsel, in0=score, scalar1=sm8[:, 7:8], scalar2=NEG, op0=ALU.is_lt, op1=ALU.mult), [b_score, b_sm8], [b_negsel])
                    A("pe", lambda e, qt=qt: e.transpose(out=p5[0:32, qt * 128:(qt + 1) * 128], in_=negsel, identity=ident_f), [b_negsel, b_identf], [bp5] if qt == 0 else (), pwrites=() if qt == 0 else [bp5])
                A("act", lambda e, g=g: e.activation(out=negselT[0:32, g, :], in_=p5[0:32, :], func=AF.Copy), [bp5], pwrites=[b_nsT])
            pend = []
            for br in (1, 2):
                for h in range(8):
                    fo, g = h % 4, h // 4
                    kts = list(range(0, 4 * qc + 4)) if br == 1 else list(range(max(0, 4 * qc - 4), 4 * qc + 4))
                    KT, b_KT = KsP[g] if br == 1 else KwP[g]
                    Va, b_Va = (Vs_aug, b_Vs) if br == 1 else (Vw_aug, b_Vw)
                    psO, bpsO = banks[2 + cnt["o2"] % 2] if "o2" in cnt else banks[2]
                    cnt["o2"] = cnt.get("o2", 0) + 1
                    steps = []

                    def qk(i):
                        kt = kts[i]
                        sidx = cnt["s"]
                        cnt["s"] += 1
                        psS, bpsS = banks[(0, 1, 5)[sidx % 3]]
                        pt, b_pt = PTb[sidx % 3]
                        qlo = max(kt, 4 * qc)
                        qhi = 4 * qc + 3 if br == 1 else min(kt + 4, 4 * qc + 3)
                        c0, c1 = (qlo - 4 * qc) * 128, (qhi - 4 * qc + 1) * 128
                        mm = [(psS[:, c0:c1], KT[:, kt * 128:(kt + 1) * 128], QT[:, fo, qc * 512 + c0:qc * 512 + c1], [b_KT, b_QT])]
                        if br == 1:
                            mm.append((psS[:, c0:c1], emat_b[:, kt * 128:(kt + 1) * 128], negselT[:, g, c0:c1], [b_emat, b_nsT]))
                        if kt >= 4 * qc:
                            j = kt - 4 * qc
                            mm.append((psS[:, j * 128:(j + 1) * 128], ident_b, bt0[:, h, :], [b_identb, b_bt0]))
                        j = kt + 1 - 4 * qc
                        if 0 <= j <= 3:
                            mm.append((psS[:, j * 128:(j + 1) * 128], ident_b, bt1[:, h, :], [b_identb, b_bt1]))
                        j = kt + 4 - 4 * qc
                        if br == 2 and 0 <= j <= 3:
                            mm.append((psS[:, j * 128:(j + 1) * 128], ident_b, edge_b, [b_identb, b_edge]))
                        n = len(mm)
                        for mi, (o_, l_, r_, rd) in enumerate(mm):
                            A("pe", lambda e: e.matmul(o_, lhsT=l_, rhs=r_, start=(mi == 0), stop=(mi == n - 1), skip_group_check=True),
                              rd, [bpsS] if mi == 0 else (), pwrites=() if mi == 0 else [bpsS])
                        A("act", lambda e: e.activation(out=pt[:, c0:c1], in_=psS[:, c0:c1], func=AF.Exp, bias=relb[:, 31, h:h + 1]), [bpsS, b_relb], [b_pt])
                        steps.append((kt, c0, c1, pt, b_pt))

                    def pv(i):
                        kt, c0, c1, pt, b_pt = steps[i]
                        n = len(kts)
                        A("pe", lambda e: e.matmul(psO[0:65, c0:c1], lhsT=Va[:, kt, g, :], rhs=pt[:, c0:c1], start=(i == 0), stop=(i == n - 1), skip_group_check=True),
                          [b_Va, b_pt], [bpsO] if i == 0 else (), pwrites=() if i == 0 else [bpsO])

                    qk(0)
                    if len(kts) > 1:
                        qk(1)
                    for i in range(len(kts)):
                        if i + 2 < len(kts):
                            qk(i + 2)
                        pv(i)
                        if i == min(1, len(kts) - 1) and pend:
                            pend.pop(0)()
                    pend.append(lambda h=h, br=br, psO=psO, bpsO=bpsO: finish_branch(h, br, 65, qc, psO, bpsO))
                    drain(per)
            while pend:
                pend.pop(0)()
            drain(len(cops))
            for qt in range(4):
                p5, bp5 = banks[5]
                for fc in range(4):
                    A("pe", lambda e, qt=qt, fc=fc: e.transpose(out=p5[:, fc * 128:(fc + 1) * 128], in_=o_nsa[:, qt, fc * 128:(fc + 1) * 128], identity=ident_f), [b_on, b_identf],
                      [bp5] if fc == 0 else (), pwrites=() if fc == 0 else [bp5])
                A("act", lambda e, qt=qt: e.activation(out=o_nsaT[:, :, qc * 512 + qt * 128:qc * 512 + (qt + 1) * 128], in_=p5.rearrange("p (f q) -> p f q", f=4), func=AF.Copy), [bp5], pwrites=[b_onT])

        (g1b, b_g1b), (lng, b_lng), (lnb, b_lnb) = bcast
        rowb(g1b, b_g1b, modrow[li * NSEQ + s:li * NSEQ + s + 1, 2 * 1024:3 * 1024], reads=[b_modrow])
        rowb(lng, b_lng, ln_g[l, 0:1, :])
        rowb(lnb, b_lnb, ln_b[l, 0:1, :])
        for tile in range(16):
            ts_ = slice(tile * 128, (tile + 1) * 128)
            xtile, b_x = xt[tile % 2]
            ttile, b_t = tt_[tile % 2]
            r0 = s * T + tile * 128
            P.dma("sp", xtile, xsrc[r0:r0 + 128, :], reads=[b_xsrc], writes=[b_x])
            for half in range(2):
                ps, bps = banks[(tile % 2) * 2 + half]
                for k in range(8):
                    lhsT = o_nsaT[:, k, ts_] if k < 4 else o_convT[:, k - 4, ts_]
                    A("pe", lambda e, ps=ps, lhsT=lhsT, k=k, half=half: e.matmul(ps, lhsT=lhsT, rhs=w_out_sb[:, k, half * 512:(half + 1) * 512], start=(k == 0), stop=(k == 7)),
                      [b_onT, b_ocT, b_wout], [bps] if k == 0 else (), pwrites=() if k == 0 else [bps])
                A("dve", lambda e, ps=ps, half=half, ttile=ttile: e.tensor_tensor(out=ttile[:, half * 512:(half + 1) * 512], in0=ps, in1=g1b[:, half * 512:(half + 1) * 512], op=ALU.mult),
                  [bps, b_g1b], [b_t] if half == 0 else (), pwrites=() if half == 0 else [b_t])
            A("dve", lambda e, xtile=xtile, ttile=ttile: e.scalar_tensor_tensor(out=ttile, in0=xtile, scalar=ALPHA, in1=ttile, op0=ALU.mult, op1=ALU.add), [b_x, b_t], [b_t])
            layer_norm_tile(ttile, b_t, lng, b_lng, lnb, b_lnb)
            P.dma("sp", xdst[r0:r0 + 128, :], ttile, reads=[b_t], pwrites=[b_xdst], owner=b_t)

        if cfg.get("dump") and s == 0:
            P.barrier()
            b_dmp = getbuf("dmp")
            items = [QT[:, 0, 0:1024], KsP[0][0][:, 0:1024], KwP[1][0][:, 0:1024], yT[:, 0, 30:1054], o_convT[:, 0, 0:1024], o_nsaT[:, 0, 0:1024],
                     gsb.rearrange("p a b -> p (a b)"), Vs_aug.rearrange("p a b c -> p (a b c)")[:, 0:1024], KcmpT.rearrange("p a b -> p (a b)"),
                     vcmp.rearrange("p a b -> p (a b)"), negselT.rearrange("p a b -> p (a b)"), impacc.rearrange("p a b c -> p (a b c)"),
                     o_nsaT[:, 0, 1024:2048], o_convT[:, 3, 1024:2048], QT[:, 3, 1024:2048]]
            for i, ap_ in enumerate(items):
                w_ = ap_.shape[1]
                P.dma("pool", out[T + i * 128:T + (i + 1) * 128, 0:w_], ap_, pwrites=[b_dmp], owner=b_dmp)
            P.barrier()
    def ffn_phase(li, l, s, xsrc, b_xsrc, xdst, b_xdst):
        moe = (l % 2 == 1)
        R = Arena(PH, SB_END)
        h2T, b_h2T = R.alloc("h2T", [128, 8, T], BF16)
        out_acc, _ = R.alloc("out_acc", [128, 16, D], F32)
        b_acc = [getbuf(f"acc{i}") for i in range(16)]
        w1s = [R.alloc(f"w1s{i}", [128, 8, 2, 256], BF16) for i in range(2)]
        w2s = [R.alloc(f"w2s{i}", [128, 2, D], BF16) for i in range(2)]
        actT = [R.alloc(f"actT{i}", [128, 2, 512], BF16) for i in range(2)]
        sgs = [R.alloc(f"sg{i}", [128, 512], F32) for i in range(2)]
        h2f = [R.alloc(f"h2f{i}", [128, 8, 128], F32) for i in range(2)]
        lg, b_lg = R.alloc("lg", [128, 8], F32)
        ex8, b_ex8 = R.alloc("ex8", [128, 8], F32)
        mk8, b_mk8 = R.alloc("mk8", [128, 8], F32)
        den, b_den = R.alloc("den", [128, 2], F32)
        (g2b, b_g2b), (lng, b_lng), (lnb, b_lnb) = bcast
        rowb(g2b, b_g2b, modrow[li * NSEQ + s:li * NSEQ + s + 1, 5 * 1024:6 * 1024], reads=[b_modrow])
        rowb(lng, b_lng, ln_g[l, 1:2, :])
        rowb(lnb, b_lnb, ln_b[l, 1:2, :])
        if moe:
            P.dma("sp", rw_sb, router_w[l // 2].rearrange("(kc p) e -> p kc e", p=128), writes=[b_rw])
        for tile in range(16):
            ts_ = slice(tile * 128, (tile + 1) * 128)
            xtile, b_x = xt[tile % 2]
            r0 = s * T + tile * 128
            P.dma("sp", xtile, xsrc[r0:r0 + 128, :], reads=[b_xsrc], writes=[b_x])
            hf_, b_hf = h2f[tile % 2]
            for half in range(2):
                ps, bps = banks[half]
                for k4 in range(4):
                    kc = half * 4 + k4
                    A("pe", lambda e, ps=ps, k4=k4, kc=kc, xtile=xtile: e.transpose(out=ps[:, k4 * 128:(k4 + 1) * 128], in_=xtile[:, kc * 128:(kc + 1) * 128], identity=ident_f),
                      [b_x, b_identf], [bps] if k4 == 0 else (), pwrites=() if k4 == 0 else [bps])
                for k4 in range(4):
                    kc = half * 4 + k4
                    i_ap = ps[:, k4 * 128:(k4 + 1) * 128]
                    o_ap = hf_[:, kc, :] if moe else h2T[:, kc, ts_]
                    bo = b_hf if moe else b_h2T
                    if k4 % 2 == 0:
                        A("dve", lambda e, o_ap=o_ap, i_ap=i_ap, kc=kc: e.tensor_scalar(out=o_ap, in0=i_ap, scalar1=modcol[:, s, 4, kc:kc + 1], scalar2=modcol[:, s, 3, kc:kc + 1],
                                                                                     op0=ALU.mult, op1=ALU.add), [bps, b_modcol], pwrites=[bo])
                    else:
                        A("act", lambda e, o_ap=o_ap, i_ap=i_ap, kc=kc: e.activation(out=o_ap, in_=i_ap, func=AF.Identity, scale=modcol[:, s, 4, kc:kc + 1], bias=modcol[:, s, 3, kc:kc + 1]),
                          [bps, b_modcol], pwrites=[bo])
            if moe:
                A("act", lambda e, hf_=hf_, ts_=ts_: e.activation(out=h2T[:, :, ts_], in_=hf_, func=AF.Copy), [b_hf], pwrites=[b_h2T])
                p4, bp4 = banks[4]
                for kc in range(8):
                    A("pe", lambda e, kc=kc, hf_=hf_: e.matmul(p4[:, 0:8], lhsT=hf_[:, kc, :], rhs=rw_sb[:, kc, :], start=(kc == 0), stop=(kc == 7)), [b_hf, b_rw],
                      [bp4] if kc == 0 else (), pwrites=() if kc == 0 else [bp4])
                A("dve", lambda e: e.tensor_copy(out=lg, in_=p4[:, 0:8]), [bp4], [b_lg])
                A("dve", lambda e: e.max(out=sm8[:, 0:8], in_=lg), [b_lg], [b_sm8])
                A("dve", lambda e: e.tensor_scalar(out=sm8[:, 8:9], in0=sm8[:, 0:1], scalar1=-1.0, scalar2=None, op0=ALU.mult), [b_sm8], [b_sm8])
                A("act", lambda e: e.activation(out=ex8, in_=lg, func=AF.Exp, bias=sm8[:, 8:9]), [b_lg, b_sm8], [b_ex8])
                A("dve", lambda e: e.tensor_scalar(out=mk8, in0=lg, scalar1=sm8[:, 1:2], scalar2=None, op0=ALU.is_ge), [b_lg, b_sm8], [b_mk8])
                A("dve", lambda e: e.tensor_tensor(out=mk8, in0=mk8, in1=ex8, op=ALU.mult), [b_mk8, b_ex8], [b_mk8])
                A("dve", lambda e: e.reduce_sum(out=den[:, 0:1], in_=mk8, axis=mybir.AxisListType.X), [b_mk8], [b_den])
                A("dve", lambda e: e.reciprocal(out=den[:, 1:2], in_=den[:, 0:1]), [b_den], [b_den])
                A("dve", lambda e, tile=tile: e.tensor_scalar(out=we_sb[:, tile, :], in0=mk8, scalar1=den[:, 1:2], scalar2=None, op0=ALU.mult), [b_mk8, b_den], pwrites=[b_we])
        if moe:
            specs = [(("moe1", l, e_), ("moe2", l, e_), D_FFE, e_) for e_ in range(NE)]
        else:
            specs = [(("ffn1", l), ("ffn2", l), D_FF, None)]
        slot = 0
        first = True
        for (k1, k2, F, ex) in specs:
            W1 = wview(k1, D, 2 * F)
            W2 = wview(k2, F, D)
            for f0 in range(0, F, 256):
                w1t, b_w1 = w1s[slot % 2]
                w2t, b_w2 = w2s[slot % 2]
                slot += 1
                P.dma("sp", w1t[:, :, 0, :], W1[:, f0:f0 + 256].rearrange("(kc p) n -> p kc n", p=128), pwrites=[b_w1], **wdep(k1, D * 2 * F))
                P.dma("sp", w1t[:, :, 1, :], W1[:, F + f0:F + f0 + 256].rearrange("(kc p) n -> p kc n", p=128), pwrites=[b_w1], **wdep(k1, D * 2 * F))
                P.dma("sp", w2t, W2[f0:f0 + 256, :].rearrange("(fc p) n -> p fc n", p=128), writes=[b_w2], **wdep(k2, F * D))
                for tc in range(4):
                    aT, b_aT = actT[tc % 2]
                    cs = slice(tc * 512, (tc + 1) * 512)
                    for fc in range(2):
                        psG, bpsG = banks[(fc % 2) * 2]
                        psU, bpsU = banks[(fc % 2) * 2 + 1]
                        sg, b_sg = sgs[fc % 2]
                        for gu, (ps, bps) in enumerate(((psG, bpsG), (psU, bpsU))):
                            for kc in range(8):
                                A("pe", lambda e, ps=ps, kc=kc, gu=gu, fc=fc, w1t=w1t, cs=cs: e.matmul(ps, lhsT=w1t[:, kc, gu, fc * 128:(fc + 1) * 128], rhs=h2T[:, kc, cs], start=(kc == 0), stop=(kc == 7)),
                                  [b_w1, b_h2T], [bps] if kc == 0 else (), pwrites=() if kc == 0 else [bps])
                        A("act", lambda e, sg=sg, psG=psG: e.activation(out=sg, in_=psG, func=AF.Silu), [bpsG], [b_sg])
                        A("dve", lambda e, aT=aT, fc=fc, psU=psU, sg=sg: e.tensor_tensor(out=aT[:, fc, :], in0=psU, in1=sg, op=ALU.mult), [bpsU, b_sg], [b_aT] if fc == 0 else (), pwrites=() if fc == 0 else [b_aT])
                    for t4 in range(4):
                        tile = tc * 4 + t4
                        for half in range(2):
                            psO, bpsO = banks[4 + (t4 * 2 + half) % 4]
                            for fc in range(2):
                                A("pe", lambda e, psO=psO, aT=aT, fc=fc, t4=t4, w2t=w2t, half=half: e.matmul(psO, lhsT=aT[:, fc, t4 * 128:(t4 + 1) * 128], rhs=w2t[:, fc, half * 512:(half + 1) * 512], start=(fc == 0), stop=(fc == 1)),
                                  [b_aT, b_w2], [bpsO] if fc == 0 else (), pwrites=() if fc == 0 else [bpsO])
                            o_ap = out_acc[:, tile, half * 512:(half + 1) * 512]
                            if moe:
                                wcol = we_sb[:, tile, ex:ex + 1]
                                if first:
                                    A("dve", lambda e, o_ap=o_ap, psO=psO, wcol=wcol: e.tensor_scalar(out=o_ap, in0=psO, scalar1=wcol, scalar2=None, op0=ALU.mult), [bpsO, b_we], pwrites=[b_acc[tile]])
                                else:
                                    A("dve", lambda e, o_ap=o_ap, psO=psO, wcol=wcol: e.scalar_tensor_tensor(out=o_ap, in0=psO, scalar=wcol, in1=o_ap, op0=ALU.mult, op1=ALU.add), [bpsO, b_we], pwrites=[b_acc[tile]])
                            else:
                                if first:
                                    A("dve", lambda e, o_ap=o_ap, psO=psO: e.tensor_copy(out=o_ap, in_=psO), [bpsO], pwrites=[b_acc[tile]])
                                else:
                                    A("dve", lambda e, o_ap=o_ap, psO=psO: e.tensor_tensor(out=o_ap, in0=psO, in1=o_ap, op=ALU.add), [bpsO], pwrites=[b_acc[tile]])
                first = False
        for tile in range(16):
            xtile, b_x = xt[tile % 2]
            ttile, b_t = tt_[tile % 2]
            r0 = s * T + tile * 128
            P.dma("sp", xtile, xsrc[r0:r0 + 128, :], reads=[b_xsrc], writes=[b_x])
            A("dve", lambda e, tile=tile, ttile=ttile: e.tensor_tensor(out=ttile, in0=out_acc[:, tile, :], in1=g2b, op=ALU.mult), [b_acc[tile], b_g2b], [b_t])
            A("dve", lambda e, xtile=xtile, ttile=ttile: e.scalar_tensor_tensor(out=ttile, in0=xtile, scalar=ALPHA, in1=ttile, op0=ALU.mult, op1=ALU.add), [b_x, b_t], [b_t])
            layer_norm_tile(ttile, b_t, lng, b_lng, lnb, b_lnb)
            P.dma("sp", xdst[r0:r0 + 128, :], ttile, reads=[b_t], pwrites=[b_xdst], owner=b_t)

    cur, b_cur = x_in, Buf("x_in")
    for li, l in enumerate(layers):
        if cfg.get("stop") == "setup":
            break
        ada_phase(li, l)
        P.barrier()
        if cfg.get("stop") == "ada":
            break
        last = (li == len(layers) - 1)
        dst, b_dst = (out, b_out) if last else (xb, b_xb)
        for s in range(NSEQ):
            if cfg.get("mixer_only"):
                if cfg.get("dump") and s > 0:
                    continue
                mixer_phase(li, l, s, cur, b_cur, dst, b_dst)
                P.barrier()
                continue
            if cfg.get("ffn_only"):
                ffn_phase(li, l, s, cur, b_cur, dst, b_dst)
                P.barrier()
                continue
            mixer_phase(li, l, s, cur, b_cur, xa, b_xa)
            P.barrier()
            ffn_phase(li, l, s, xa, b_xa, dst, b_dst)
            P.barrier()
        cur, b_cur = xb, b_xb
    if cfg.get("stop"):
        A("dve", lambda e: e.memset(tt_[0][0], 0.0), [], [tt_[0][1]])
        if cfg["stop"] == "ada":
            A("dve", lambda e: e.tensor_copy(out=tt_[0][0][:, 0:NSEQ * 48], in_=modcol.rearrange("p s a b -> p (s a b)")), [b_modcol], [tt_[0][1]])
        P.dma("sp", out[0:128, :], tt_[0][0], reads=[tt_[0][1]], pwrites=[b_out], owner=tt_[0][1])
        if cfg["stop"] == "ada":
            P.dma("sp", out[128:128 + NSEQ, :], modrow[0:NSEQ, 0:1024], reads=[b_modrow], pwrites=[b_out], owner=b_modrow)
    P.wait_all("sp", [b_out])
    P.emit()
    return nc, NCH


SMALL = ["cmp_pos", "cmp_w2", "conv_w", "conv_b", "conv_ln_g", "conv_ln_b", "ada_b", "ln_g", "ln_b", "router_w"]


def run(inp, cfg, n_cores, trace=False):
    layers = cfg["layers"]
    NSEQ = cfg["nseq"]
    nc, NCH = build(cfg)
    flat, nch = pack_flat(inp, layers)
    consts = host_consts()
    x = np.asarray(inp["x"], np.float32)
    c = np.asarray(inp["c"], np.float32)
    in_maps = []
    for core in range(n_cores):
        m = {"x": np.ascontiguousarray(x[core * NSEQ:(core + 1) * NSEQ].reshape(NSEQ * T, D)),
             "c": np.ascontiguousarray(c[core * NSEQ:(core + 1) * NSEQ])}
        if cfg["gather"]:
            p = ORDER.index(core)
            m["wsh"] = np.ascontiguousarray(flat[:, p].reshape(nch * 512, 1024))
        else:
            m["wfull"] = flat.reshape(nch * 4096, 1024)
        for k in SMALL:
            m[k] = np.ascontiguousarray(np.asarray(inp[k], np.float32))
        m["rel_bias"] = np.ascontiguousarray(np.asarray(inp["rel_bias"], np.float32).reshape(1, 256))
        m.update(consts)
        in_maps.append(m)
    res = run_bass_kernel_spmd(nc, in_maps, core_ids=list(range(n_cores)), trace=trace)
    outs = [r["out"].reshape(NSEQ, T, D) for r in res.results]
    return np.concatenate(outs, 0), res


def kernel(**inputs):
    cfg = dict(nseq=2, layers=[0, 1, 2, 3], gather=True)
    o, _ = run(inputs, cfg, 8)
    return o.astype(np.float32)
```

```python
import math
import contextlib
import types
import numpy as np
import concourse.bass as bass
import concourse.mybir as mybir
from concourse.bass_utils import run_bass_kernel_spmd

F32 = mybir.dt.float32
BF16 = mybir.dt.bfloat16
ALU = mybir.AluOpType
AF = mybir.ActivationFunctionType

D = 1024
T = 2048
DEPTH = 4
NH = 8
HD = 64
D_IN = 2328
D_FF = 2816
D_FFE = 3584
NE = 8
NCMP = 127
ALPHA = (2 * DEPTH) ** 0.25
LN_EPS = 1e-5
NEG = -30000.0
CHUNK_ELEMS = 4096 * 1024
ORDER = [0, 1, 4, 5, 2, 3, 6, 7]
EPOCH = 16000
SEG_CH = 28
SEG_ELEMS = SEG_CH * CHUNK_ELEMS


class Buf:
    __slots__ = ("name", "w", "wf", "r", "sem", "semcnt", "excl")

    def __init__(self, name, excl=False):
        self.name = name
        self.excl = excl
        self.w = {}
        self.wf = {}
        self.r = {}
        self.sem = None
        self.semcnt = 0


def _snap(fn):
    if fn is None or fn.__closure__ is None:
        return fn
    cells = []
    for c in fn.__closure__:
        try:
            cells.append(types.CellType(c.cell_contents))
        except ValueError:
            cells.append(c)
    g = types.FunctionType(fn.__code__, fn.__globals__, fn.__name__, fn.__defaults__, tuple(cells))
    g.__kwdefaults__ = fn.__kwdefaults__
    return g


class Prog:
    def __init__(self, nc):
        self.nc = nc
        self.stack = contextlib.ExitStack()
        self.engnames = ["pe", "act", "dve", "pool", "sp"]
        self.streams = {k: [] for k in self.engnames}
        self.count = {k: 0 for k in self.engnames}
        self.esems = {k: [] for k in self.engnames}
        self.seen = {k: {} for k in self.engnames}
        self.dmabufs = []
        self.nobarrier = set()
        self.nsem = 0

    def new_sem(self, name):
        self.nsem += 1
        return self.stack.enter_context(self.nc.semaphore(name))

    def _esem(self, k, epoch):
        while len(self.esems[k]) <= epoch:
            self.esems[k].append(self.new_sem(f"e_{k}_{len(self.esems[k])}"))
        return self.esems[k][epoch]

    def _need(self, k, wt, waits):
        if wt[0] == "e":
            _, e2, c = wt
            if e2 == "pe" and k == "pe":
                return
            key = e2
        else:
            _, b, c = wt
            key = ("d", id(b))
        if self.seen[k].get(key, 0) >= c:
            return
        self.seen[k][key] = c
        waits.append(wt)

    def _deps(self, k, reads, writes, pwrites):
        waits = []
        for b in reads:
            for wt in b.w.values():
                self._need(k, wt, waits)
            if b.excl:
                for kk, wt in b.r.items():
                    if kk != k:
                        self._need(k, wt, waits)
        for b in writes:
            for wt in b.w.values():
                self._need(k, wt, waits)
            for wt in b.r.values():
                self._need(k, wt, waits)
        for b in pwrites:
            for wt in b.r.values():
                self._need(k, wt, waits)
            for wt in b.wf.values():
                self._need(k, wt, waits)
        return waits

    def _record(self, key, me, reads, writes, pwrites):
        for b in reads:
            b.r[key] = me
        for b in writes:
            b.w = {key: me}
            b.wf = {key: me}
            b.r = {}
        for b in pwrites:
            if b.r:
                b.w = {key: me}
                b.wf = {}
                b.r = {}
            else:
                b.w[key] = me

    def op(self, k, fn, reads=(), writes=(), pwrites=(), extra=()):
        waits = self._deps(k, reads, writes, pwrites)
        for wt in extra:
            self._need(k, wt, waits)
        self.count[k] += 1
        me = ("e", k, self.count[k])
        self._record(k, me, reads, writes, pwrites)
        self.streams[k].append((waits, _snap(fn), me))

    def dma(self, q, out_ap, in_ap, reads=(), writes=(), pwrites=(), owner=None, extra=(), fn=None, inc=16, **kw):
        if owner is None:
            owner = (list(writes) + list(pwrites))[0]
        if owner.sem is None:
            owner.sem = self.new_sem("d_" + owner.name)
            self.dmabufs.append(owner)
        waits = self._deps(q, reads, writes, pwrites)
        for wt in extra:
            self._need(q, wt, waits)
        owner.semcnt += inc
        me = ("d", owner, owner.semcnt)
        self._record(("d", id(owner)), me, reads, writes, pwrites)
        if fn is None:
            fn = lambda eng, o=out_ap, i=in_ap, kw=kw: eng.dma_start(out=o, in_=i, **kw)
        self.streams[q].append((waits, _snap(fn), ("dinc", owner, inc)))

    def barrier(self):
        snap = dict(self.count)
        dsn = [(b, b.semcnt) for b in self.dmabufs if b.name not in self.nobarrier]
        for k in self.engnames:
            waits = []
            for e2 in self.engnames:
                if e2 != k and snap[e2] > 0:
                    self._need(k, ("e", e2, snap[e2]), waits)
            for b, c in dsn:
                if c > 0:
                    self._need(k, ("d", b, c), waits)
            self.streams[k].append((waits, None, None))

    def wait_all(self, k, bufs):
        waits = self._deps(k, bufs, (), ())
        self.streams[k].append((waits, None, None))

    def emit(self):
        nc = self.nc
        engattr = {"pe": "tensor", "act": "scalar", "dve": "vector", "pool": "gpsimd", "sp": "sync"}
        for k in self.engnames:
            self._esem(k, max(self.count[k] - 1, 0) // EPOCH)
        with nc.Block() as block:
            for k in self.engnames:
                stream = self.streams[k]

                def body(eng, k=k, stream=stream):
                    for waits, fn, inc in stream:
                        for wt in waits:
                            if wt[0] == "e":
                                ep, cc = divmod(wt[2] - 1, EPOCH)
                                eng.wait_ge(self.esems[wt[1]][ep], cc + 1)
                            else:
                                eng.wait_ge(wt[1].sem, wt[2])
                        if fn is None:
                            continue
                        ins = fn(eng)
                        if inc[0] == "e":
                            ep, cc = divmod(inc[2] - 1, EPOCH)
                            ins.then_inc(self.esems[k][ep], 1)
                        else:
                            ins.then_inc(inc[1].sem, inc[2])

                getattr(block, engattr[k])(body)
        self.stack.close()


def flat_layout(layers):
    off = 0
    lay = {}

    def add(name, n):
        nonlocal off
        if (off // SEG_ELEMS) != ((off + n - 1) // SEG_ELEMS):
            off = ((off + n - 1) // SEG_ELEMS) * SEG_ELEMS
        lay[name] = off
        off += n

    for l in layers:
        add(("ada", l), D * 6 * D)
        add(("w_in", l), D * D_IN)
        add(("cmp_w1", l), 2 * 2048 * 64)
        add(("w_out", l), D * D)
        if l % 2 == 0:
            add(("ffn1", l), D * 2 * D_FF)
            add(("ffn2", l), D_FF * D)
        else:
            for e in range(NE):
                add(("moe1", l, e), D * 2 * D_FFE)
                add(("moe2", l, e), D_FFE * D)
    nch = (off + CHUNK_ELEMS - 1) // CHUNK_ELEMS
    return lay, off, nch


def w_in_perm():
    cols = []
    for fo in range(4):
        cols += list(range(fo * 64, fo * 64 + 64)) + list(range((4 + fo) * 64, (4 + fo) * 64 + 64))
    q_end = 512
    kc, vc, ks, vs, kw, vw = [q_end + i * 128 for i in range(6)]
    gl = q_end + 6 * 128
    cv = gl + 24
    cols += list(range(kc, kc + 128)) + list(range(vc, vc + 128)) + list(range(ks, ks + 128)) + list(range(kw, kw + 128))
    cols += list(range(cv, cv + 1024))
    cols += list(range(vs, vs + 128)) + list(range(vw, vw + 128)) + list(range(gl, gl + 24))
    assert len(cols) == D_IN
    return np.asarray(cols)


def pack_flat(inp, layers):
    lay, total, nch = flat_layout(layers)
    flat = np.zeros(nch * CHUNK_ELEMS, np.float32)

    def put(key, arr):
        a = np.ascontiguousarray(arr, dtype=np.float32).reshape(-1)
        flat[lay[key]:lay[key] + a.size] = a

    perm = w_in_perm()
    for l in layers:
        put(("ada", l), inp["ada_w"][l])
        put(("w_in", l), np.asarray(inp["w_in"][l])[:, perm])
        put(("cmp_w1", l), inp["cmp_w1"][l])
        put(("w_out", l), inp["w_out"][l])
        if l % 2 == 0:
            put(("ffn1", l), inp["ffn_w1"][l // 2])
            put(("ffn2", l), inp["ffn_w2"][l // 2])
        else:
            for e in range(NE):
                put(("moe1", l, e), inp["moe_w1"][l // 2, e])
                put(("moe2", l, e), inp["moe_w2"][l // 2, e])
    return flat.reshape(nch, 8, 512, 1024), nch


def rel_bucket_np(dist):
    n = np.maximum(dist, 0)
    nf = np.maximum(n, 1).astype(np.float32)
    large = 16 + (np.log(nf / np.float32(16)) / np.float32(math.log(128 / 16)) * np.float32(16)).astype(np.int32)
    large = np.minimum(large, 31)
    return np.where(n < 16, n, large)


def host_consts():
    c = {}
    n = np.arange(NCMP)[:, None]
    q = np.arange(T)[None, :]
    dist = q - (16 * n + 31)
    c["bk_c"] = np.where(dist >= 0, rel_bucket_np(dist), 32).astype(np.float32)
    k = np.arange(128)[:, None]
    qq = np.arange(128)[None, :]
    d0 = qq - k
    c["bk_d0"] = np.where(d0 >= 0, rel_bucket_np(d0), 32).astype(np.float32)
    c["bk_d1"] = rel_bucket_np(128 + qq - k).astype(np.float32)
    c["edge"] = np.where(qq >= k, NEG, 0.0).astype(np.float32)
    j = np.arange(32)[:, None]
    kk = np.arange(T)[None, :]
    c["emat"] = (kk // 64 == j).astype(np.float32)
    tb = (np.arange(T) // 64)[:, None]
    jj = np.arange(32)[None, :]
    fm = np.where(jj > tb, -1e9, np.where((jj == 0) | (jj == tb) | (jj == tb - 1), 1e9, 0.0))
    c["fm"] = fm.astype(np.float32)
    starts = 16 * np.arange(NCMP)[:, None]
    bstart = 64 * np.arange(32)[None, :]
    c["ov"] = ((starts < bstart + 64) & (starts + 32 > bstart)).astype(np.float32)
    c["ident"] = np.eye(128, dtype=np.float32)
    return c
SB_BASE = 16512
SB_END = 229376


def build(cfg):
    NSEQ = cfg["nseq"]
    layers = cfg["layers"]
    gather = cfg["gather"]
    lay, total, NCH = flat_layout(layers)
    nc = bass.Bass("TRN2", target_bir_lowering=False)
    P = Prog(nc)
    NT = NSEQ * T

    def dram_in(name, shape, dt=F32):
        return nc.dram_tensor(name, list(shape), dt, kind="ExternalInput").ap()

    x_in = dram_in("x", [NT, D])
    c_in = dram_in("c", [NSEQ, D])
    if gather:
        wsh = dram_in("wsh", [NCH * 512, 1024])
    else:
        wfull = dram_in("wfull", [NCH * 4096, 1024])
    cmp_pos = dram_in("cmp_pos", [4, 2, 32, 64])
    cmp_w2 = dram_in("cmp_w2", [4, 2, 64, 64])
    conv_w = dram_in("conv_w", [4, 31, 512])
    conv_b = dram_in("conv_b", [4, 512])
    conv_g = dram_in("conv_ln_g", [4, 512])
    conv_be = dram_in("conv_ln_b", [4, 512])
    rel_bias = dram_in("rel_bias", [1, 256])
    ada_b = dram_in("ada_b", [4, 6144])
    ln_g = dram_in("ln_g", [4, 2, 1024])
    ln_b = dram_in("ln_b", [4, 2, 1024])
    router_w = dram_in("router_w", [2, 1024, 8])
    bk_c = dram_in("bk_c", [NCMP, T])
    bk_d0 = dram_in("bk_d0", [128, 128])
    bk_d1 = dram_in("bk_d1", [128, 128])
    edge_in = dram_in("edge", [128, 128])
    emat_in = dram_in("emat", [32, T])
    fm_in = dram_in("fm", [T, 32])
    ov_in = dram_in("ov", [NCMP, 32])
    ident_in = dram_in("ident", [128, 128])
    out = nc.dram_tensor("out", [NT, D], F32, kind="ExternalOutput").ap()

    NSEG = (NCH + SEG_CH - 1) // SEG_CH
    wflat_t = [nc.dram_tensor(f"wflat{i}", [min(SEG_CH, NCH - i * SEG_CH) * 4096, 1024], BF16).ap() for i in range(NSEG)]
    xa = nc.dram_tensor("xa", [NT, D], F32).ap()
    xb = nc.dram_tensor("xb", [NT, D], F32).ap()
    modrow = nc.dram_tensor("modrow", [len(layers) * NSEQ, 6144], F32).ap()
    bc_dram = nc.dram_tensor("bc_dram", [8 * 128, T], BF16).ap()
    b_xa, b_xb, b_out, b_modrow, b_bc = Buf("xa"), Buf("xb"), Buf("outd"), Buf("modrow"), Buf("bcd")

    b_wflat = Buf("wflat")
    b_cc = Buf("cc")
    chunk_ready = {}
    P.nobarrier.update(["cc", "ib0", "wflat"])
    if gather:
        ib = nc.dram_tensor("ib", [NCH * 512, 1024], BF16).ap()
        ob1 = nc.dram_tensor("ob1", [NCH * 2048, 1024], BF16).ap()
        NP_ = (NCH * 512 + 2047) // 2048
        b_ibp = [Buf(f"ib{i}") for i in range(NP_)]
        cast_done = set()

        def cast(pi):
            if pi in cast_done or pi >= NP_:
                return
            cast_done.add(pi)
            i = pi * 2048
            j = min(i + 2048, NCH * 512)
            P.dma("pool", ib[i:j, :], wsh[i:j, :], writes=[b_ibp[pi]], owner=b_ibp[0], max_dma_last_dim=4096)

        s1idx = {}

        def s1(ch):
            cast(ch // 4)
            fn = lambda eng, ch=ch: eng.collective_compute(
                "AllGather", ALU.bypass, replica_groups=[[0, 1, 2, 3], [4, 5, 6, 7]],
                ins=[ib[ch * 512:(ch + 1) * 512, :]], outs=[ob1[ch * 2048:(ch + 1) * 2048, :]])
            P.dma("pool", None, None, reads=[b_ibp[ch // 4]], writes=[], owner=b_cc, fn=fn, inc=1)
            s1idx[ch] = b_cc.semcnt

        def s2(ch, hf):
            fn = lambda eng, ch=ch, hf=hf: eng.collective_compute(
                "AllGather", ALU.bypass, replica_groups=[[0, 4], [1, 5], [2, 6], [3, 7]],
                ins=[ob1[ch * 2048 + hf * 1024:ch * 2048 + (hf + 1) * 1024, :]],
                outs=[wflat_t[ch // SEG_CH][(2 * (ch % SEG_CH) + hf) * 2048:(2 * (ch % SEG_CH) + hf + 1) * 2048, :]])
            P.dma("pool", None, None, reads=[], writes=[], owner=b_cc, fn=fn, inc=1,
                  extra=[("d", b_cc, s1idx[ch])])

        s1(0)
        if NCH > 1:
            s1(1)
        for ch in range(NCH):
            s2(ch, 0)
            s2(ch, 1)
            chunk_ready[ch] = b_cc.semcnt
            if ch + 2 < NCH:
                s1(ch + 2)
            cast((ch + 6) // 4)
    else:
        for i in range(0, NCH * 4096, 2048):
            sg, li_ = divmod(i, SEG_CH * 4096)
            P.dma("pool", wflat_t[sg][li_:li_ + 2048, :], wfull[i:i + 2048, :], pwrites=[b_wflat], max_dma_last_dim=4096)

    flat1 = [w_.rearrange("r c -> (r c)") for w_ in wflat_t]

    def wview(key, K, N, sub=0):
        sg, o = divmod(lay[key] + sub, SEG_ELEMS)
        return flat1[sg][o:o + K * N].rearrange("(k n) -> k n", n=N)

    def wdep(key, nelem):
        if gather:
            ch = (lay[key] + nelem - 1) // CHUNK_ELEMS
            return dict(extra=[("d", b_cc, chunk_ready[ch])])
        return dict(reads=[b_wflat])

    _cache = {}
    _bufs = {}

    def getbuf(name):
        if name not in _bufs:
            _bufs[name] = Buf(name)
        return _bufs[name]

    class Arena:
        def __init__(self, base, end):
            self.base, self.off, self.end = base, base, end

        def alloc(self, name, shape, dt):
            esz = 4 if dt == F32 else 2
            n = esz
            for s in shape[1:]:
                n *= s
            n = (n + 63) // 64 * 64
            assert self.off + n <= self.end, (name, self.off, n, self.end)
            if name in _cache:
                ap_, off_ = _cache[name]
                assert off_ == self.off, (name, off_, self.off)
            else:
                ap_ = nc.alloc_sbuf_tensor_at(name, list(shape), dt, offset=self.off).ap()
                _cache[name] = (ap_, self.off)
            self.off += n
            return ap_, getbuf(name)

    AP_ = Arena(SB_BASE, SB_END)
    al = AP_.alloc
    ident_f, b_identf = al("ident_f", [128, 128], F32)
    ident_b, b_identb = al("ident_b", [128, 128], BF16)
    onesm, b_onesm = al("onesm", [128, 128], F32)
    emat_b, b_emat = al("emat_b", [128, T], BF16)
    fm_sb, b_fm = al("fm_sb", [128, 16, 32], F32)
    edge_b, b_edge = al("edge_b", [128, 128], BF16)
    bt0, b_bt0 = al("bt0", [128, 8, 128], BF16)
    bt1, b_bt1 = al("bt1", [128, 8, 128], BF16)
    relb, b_relb = al("relb", [128, 32, 8], F32)
    relc, b_relc = al("relc", [128, 32, 8], F32)
    modcol, b_modcol = al("modcol", [128, NSEQ, 6, 8], F32)
    scT, b_scT = al("scT", [128, 8, NSEQ], BF16)
    vcmp, b_vcmp = al("vcmp", [128, 2, 97], BF16)
    convw, b_convw = al("convw", [128, 4, 31], F32)
    convp, b_convp = al("convp", [128, 3, 4], F32)
    w1sb, b_w1sb = al("w1sb", [128, 2, 32, 64], BF16)
    posT, b_posT = al("posT", [128, 2, 34], BF16)
    posTf, b_posTf = al("posTf", [128, 2, 32], F32)
    w2sb, b_w2sb = al("w2sb", [128, 2, 64], BF16)
    w2f, b_w2f = al("w2f", [128, 2, 64], F32)
    c1col, b_c1col = al("c1col", [128, 2], F32)
    rw_sb, b_rw = al("rw_sb", [128, 8, 8], F32)
    epsc, b_epsc = al("epsc", [128, 1], F32)
    stgT, b_stgT = al("stgT", [128, 128], F32)
    bcast = [al(f"bcast{i}", [128, 1024], F32) for i in range(3)]
    xt = [al(f"xt{i}", [128, 1024], F32) for i in range(2)]
    tt_ = [al(f"tt{i}", [128, 1024], F32) for i in range(2)]
    we_sb, b_we = al("we_sb", [128, 16, 8], F32)
    st_bn, b_stbn = al("st_bn", [128, 2, 6], F32)
    st_mv, b_stmv = al("st_mv", [128, 4], F32)
    sm8, b_sm8 = al("sm8", [128, 16], F32)
    PH = AP_.off

    banks = []
    for i in range(8):
        banks.append((nc.alloc_psum_tensor(f"bank{i}", [128, 512], F32).ap(), Buf(f"bank{i}", excl=True)))

    def A(k, f, reads=(), writes=(), pwrites=(), extra=()):
        P.op(k, f, reads=reads, writes=writes, pwrites=pwrites, extra=extra)

    def load_T(dst, b_dst, n, srcs, full=False):
        A("dve", lambda e: e.memset(stgT, 0.0), [], [b_stgT])
        for cs_, rs_, ap_ in srcs:
            P.dma("sp", stgT[rs_, cs_], ap_, pwrites=[b_stgT])
        n2 = (n + 1) // 2 * 2
        p4, bp4 = banks[4]
        A("pe", lambda e: e.transpose(out=p4[:, 0:n2], in_=stgT[0:n2, :], identity=ident_f[0:n2, 0:n2]), [b_stgT, b_identf], [bp4])
        if full:
            A("dve", lambda e: e.tensor_copy(out=dst, in_=p4[:, 0:n]), [bp4], [b_dst])
        else:
            A("dve", lambda e: e.tensor_copy(out=dst, in_=p4[:, 0:n]), [bp4], pwrites=[b_dst])

    MA = Arena(PH, SB_END)
    P.dma("sp", ident_f, ident_in, writes=[b_identf])
    A("dve", lambda e: e.tensor_copy(out=ident_b, in_=ident_f), [b_identf], [b_identb])
    A("dve", lambda e: e.memset(onesm, 1.0 / 512.0), [], [b_onesm])
    A("dve", lambda e: e.memset(epsc, LN_EPS), [], [b_epsc])
    stg, b_stg = MA.alloc("stg", [128, T], F32)
    P.dma("sp", stg[0:32, :], emat_in, writes=[b_stg])
    A("dve", lambda e: e.memset(emat_b, 0.0), [], [b_emat])
    A("dve", lambda e: e.tensor_copy(out=emat_b[0:32, :], in_=stg[0:32, :]), [b_stg], [b_emat])
    P.dma("sp", fm_sb, fm_in.rearrange("(t p) j -> p t j", p=128), writes=[b_fm])
    stg2, b_stg2 = MA.alloc("stg2", [128, 128], F32)
    P.dma("sp", stg2, edge_in, writes=[b_stg2])
    A("dve", lambda e: e.tensor_copy(out=edge_b, in_=stg2), [b_stg2], [b_edge])
    P.dma("sp", relb.rearrange("p b h -> p (b h)"), rel_bias.partition_broadcast(128).rearrange("p o n -> p (o n)"), writes=[b_relb])
    for b in range(32):
        A("dve", lambda e, b=b: e.tensor_tensor(out=relc[:, b, :], in0=relb[:, b, :], in1=relb[:, 31, :], op=ALU.subtract), [b_relb], [b_relc])
    stg3, b_stg3 = MA.alloc("stg3", [128, 32], F32)
    P.dma("sp", stg3[0:NCMP, :], ov_in, writes=[b_stg3])
    A("dve", lambda e: e.memset(vcmp, 1.0), [], [b_vcmp])
    for g in range(2):
        A("dve", lambda e, g=g: e.tensor_copy(out=vcmp[0:NCMP, g, 65:97], in_=stg3[0:NCMP, :]), [b_stg3], [b_vcmp])
    bkt, b_bkt = MA.alloc("bkt", [128, 2, 128], F32)
    P.dma("sp", bkt[:, 0, :], bk_d0, pwrites=[b_bkt])
    P.dma("sp", bkt[:, 1, :], bk_d1, pwrites=[b_bkt])
    acc01, b_acc01 = MA.alloc("acc01", [128, 2, 8, 128], F32)
    msk, b_msk = MA.alloc("msk", [128, 2, 128], F32)
    A("dve", lambda e: e.tensor_scalar(out=msk, in0=bkt, scalar1=32.0, scalar2=NEG, op0=ALU.is_equal, op1=ALU.mult), [b_bkt], [b_msk])
    for h in range(8):
        A("dve", lambda e, h=h: e.tensor_copy(out=acc01[:, :, h, :], in_=msk), [b_msk], [b_acc01])
    for b in range(31):
        A("dve", lambda e, b=b: e.tensor_scalar(out=msk, in0=bkt, scalar1=float(b), scalar2=None, op0=ALU.is_equal), [b_bkt], [b_msk])
        for h in range(8):
            A("dve", lambda e, b=b, h=h: e.scalar_tensor_tensor(out=acc01[:, :, h, :], in0=msk, scalar=relc[:, b, h:h + 1], in1=acc01[:, :, h, :],
                                                              op0=ALU.mult, op1=ALU.add), [b_msk, b_relc, b_acc01], [b_acc01])
    A("dve", lambda e: e.tensor_copy(out=bt0, in_=acc01[:, 0, :, :]), [b_acc01], [b_bt0])
    A("dve", lambda e: e.tensor_copy(out=bt1, in_=acc01[:, 1, :, :]), [b_acc01], [b_bt1])
    bkc, b_bkc = MA.alloc("bkc", [128, T], F32)
    P.dma("sp", bkc[0:NCMP, :], bk_c, writes=[b_bkc])
    mskc, b_mskc = MA.alloc("mskc", [128, T], F32)
    accc, b_accc = MA.alloc("accc", [128, 8, T], F32)
    A("dve", lambda e: e.tensor_scalar(out=mskc[0:NCMP], in0=bkc[0:NCMP], scalar1=32.0, scalar2=NEG, op0=ALU.is_equal, op1=ALU.mult), [b_bkc], [b_mskc])
    for h in range(8):
        A("dve", lambda e, h=h: e.tensor_copy(out=accc[0:NCMP, h, :], in_=mskc[0:NCMP]), [b_mskc], [b_accc])
    for b in range(31):
        A("dve", lambda e, b=b: e.tensor_scalar(out=mskc[0:NCMP], in0=bkc[0:NCMP], scalar1=float(b), scalar2=None, op0=ALU.is_equal), [b_bkc], [b_mskc])
        for h in range(8):
            A("dve", lambda e, b=b, h=h: e.scalar_tensor_tensor(out=accc[0:NCMP, h, :], in0=mskc[0:NCMP], scalar=relc[0:NCMP, b, h:h + 1], in1=accc[0:NCMP, h, :],
                                                              op0=ALU.mult, op1=ALU.add), [b_mskc, b_relc, b_accc], [b_accc])
    bcb, b_bcb = MA.alloc("bcb", [128, 8, T], BF16)
    A("act", lambda e: e.activation(out=bcb[0:NCMP], in_=accc[0:NCMP], func=AF.Copy), [b_accc], [b_bcb])
    for h in range(8):
        P.dma("sp", bc_dram[h * 128:h * 128 + NCMP, :], bcb[0:NCMP, h, :], reads=[b_bcb], pwrites=[b_bc], owner=b_bcb)
    csb, b_csb = MA.alloc("csb", [128, D], F32)
    A("dve", lambda e: e.memset(csb[0:32, :], 0.0), [], [b_csb])
    P.dma("sp", csb[0:NSEQ, :], c_in, writes=[b_csb])
    A("act", lambda e: e.activation(out=csb[0:32, :], in_=csb[0:32, :], func=AF.Silu), [b_csb], [b_csb])
    pb, b_pb = banks[4]
    for kc in range(8):
        A("pe", lambda e, kc=kc: e.transpose(out=pb[:, kc * 32:(kc + 1) * 32], in_=csb[0:32, kc * 128:(kc + 1) * 128], identity=ident_f[0:32, 0:32]),
          [b_csb, b_identf], [b_pb] if kc == 0 else (), pwrites=() if kc == 0 else [b_pb])
    A("dve", lambda e: e.tensor_copy(out=scT, in_=pb[:, 0:256].rearrange("p (k s) -> p k s", s=32)[:, :, 0:NSEQ]), [b_pb], [b_scT])
    P.barrier()
    def rowb(dst, b_dst, src_row, reads=()):
        P.dma("sp", dst, src_row.partition_broadcast(128).rearrange("p o n -> p (o n)"), reads=list(reads), writes=[b_dst])

    def layer_norm_tile(tt, b_tt, lng, b_lng, lnb, b_lnb):
        for hf in range(2):
            A("dve", lambda e, hf=hf: e.bn_stats(out=st_bn[:, hf, :], in_=tt[:, hf * 512:(hf + 1) * 512]), [b_tt], pwrites=[b_stbn])
        A("dve", lambda e: e.bn_aggr(out=st_mv[:, 0:2], in_=st_bn.rearrange("p a b -> p (a b)")), [b_stbn], [b_stmv])
        A("act", lambda e: e.activation(out=st_mv[:, 2:3], in_=st_mv[:, 1:2], func=AF.Sqrt, bias=epsc[:, 0:1]), [b_stmv, b_epsc], [b_stmv])
        A("dve", lambda e: e.reciprocal(out=st_mv[:, 3:4], in_=st_mv[:, 2:3]), [b_stmv], [b_stmv])
        A("dve", lambda e: e.tensor_scalar(out=tt, in0=tt, scalar1=st_mv[:, 0:1], scalar2=st_mv[:, 3:4], op0=ALU.subtract, op1=ALU.mult), [b_tt, b_stmv], [b_tt])
        A("dve", lambda e: e.tensor_tensor(out=tt, in0=tt, in1=lng, op=ALU.mult), [b_tt, b_lng], [b_tt])
        A("dve", lambda e: e.tensor_tensor(out=tt, in0=tt, in1=lnb, op=ALU.add), [b_tt, b_lnb], [b_tt])

    NS = 0.125

    def ada_phase(li, l):
        R = Arena(PH, SB_END)
        adab, b_adab = R.alloc("adab", [128, 6144], F32)
        mrow, b_mrow = R.alloc("mrow", [128, 6144], F32)
        wp = [R.alloc(f"adaw{i}", [128, 8, 512], BF16) for i in range(2)]
        P.dma("sp", adab[0:NSEQ, :], ada_b[l:l + 1, :].partition_broadcast(NSEQ).rearrange("p o n -> p (o n)"), writes=[b_adab])
        wv = wview(("ada", l), D, 6144)
        for j in range(12):
            t, bt = wp[j % 2]
            P.dma("sp", t, wv[:, j * 512:(j + 1) * 512].rearrange("(kc p) n -> p kc n", p=128), writes=[bt], **wdep(("ada", l), D * 6144))
            ps, bps = banks[j % 2]
            for kc in range(8):
                A("pe", lambda e, kc=kc, t=t, ps=ps: e.matmul(ps[0:NSEQ, :], lhsT=scT[:, kc, :], rhs=t[:, kc, :], start=(kc == 0), stop=(kc == 7)),
                  [b_scT, bt], [bps] if kc == 0 else (), pwrites=() if kc == 0 else [bps])
            A("dve", lambda e, j=j, ps=ps: e.tensor_tensor(out=mrow[0:NSEQ, j * 512:(j + 1) * 512], in0=ps[0:NSEQ, :], in1=adab[0:NSEQ, j * 512:(j + 1) * 512], op=ALU.add),
              [bps, b_adab], pwrites=[b_mrow])
        for seg in (1, 2, 4, 5):
            A("dve", lambda e, seg=seg: e.tensor_scalar(out=mrow[0:NSEQ, seg * 1024:(seg + 1) * 1024], in0=mrow[0:NSEQ, seg * 1024:(seg + 1) * 1024],
                                                        scalar1=1.0, scalar2=None, op0=ALU.add), [b_mrow], [b_mrow])
        P.dma("sp", modrow[li * NSEQ:(li + 1) * NSEQ, :], mrow[0:NSEQ, :], reads=[b_mrow], writes=[b_modrow], owner=b_mrow)
        P.barrier()
        for s in range(NSEQ):
            load_T(modcol[:, s, :, :].rearrange("p a b -> p (a b)"), b_modcol, 48,
                   [(slice(0, 128), slice(0, 48), modrow[li * NSEQ + s, :].rearrange("(r p) -> r p", p=128))])

    def mixer_phase(li, l, s, xsrc, b_xsrc, xdst, b_xdst):
        R1 = Arena(PH, SB_END)
        w_in_sb, b_win = R1.alloc("w_in_sb", [128, 8, D_IN], BF16)
        hT = [R1.alloc(f"hT{i}", [128, 8, 512], BF16) for i in range(2)]
        KcT, b_KcT = R1.alloc("KcT", [128, T], BF16)
        VcT, b_VcT = R1.alloc("VcT", [128, T], BF16)
        sig, b_sig = R1.alloc("sig", [128, 512], F32)
        ov_end = R1.off
        w_out_sb, b_wout = R1.alloc("w_out_sb", [128, 8, D], BF16)
        QT, b_QT = R1.alloc("QT", [128, 4, T], BF16)
        KsP = [R1.alloc(f"KsP{g}", [128, T], BF16) for g in range(2)]
        KwP = [R1.alloc(f"KwP{g}", [128, T], BF16) for g in range(2)]
        Vs_aug, b_Vs = R1.alloc("Vs_aug", [128, 16, 2, 65], BF16)
        Vw_aug, b_Vw = R1.alloc("Vw_aug", [128, 16, 2, 65], BF16)
        gsb, b_gsb = R1.alloc("gsb", [128, 16, 24], F32)
        yT, b_yT = R1.alloc("yT", [128, 4, 30 + T], BF16)
        convacc, b_cacc = R1.alloc("convacc", [128, 4, 512], F32)
        negselT, b_nsT = R1.alloc("negselT", [128, 2, 512], BF16)
        KcmpT, b_KcmpT = R1.alloc("KcmpT", [128, 2, 128], BF16)
        Gt, b_G = R1.alloc("Gt", [128, 128], BF16)
        dg = [R1.alloc(f"dg{i}", [128, 8, 128], BF16) for i in range(2)]
        R2 = Arena(PH, ov_end)
        o_nsaT, b_onT = R2.alloc("o_nsaT", [128, 4, T], BF16)
        o_convT, b_ocT = R2.alloc("o_convT", [128, 4, T], BF16)
        o_nsa, b_on = R2.alloc("o_nsa", [128, 4, 512], F32)
        Osb = [R2.alloc(f"Osb{i}", [128, 512], F32) for i in range(2)]
        PTb = [R2.alloc(f"PT{i}", [128, 512], BF16) for i in range(3)]
        Bcs = [R2.alloc(f"Bcs{i}", [128, 512], BF16) for i in range(2)]
        convsq = [R2.alloc(f"convsq{i}", [128, 512], F32) for i in range(2)]
        s_mean, b_smean = R2.alloc("s_mean", [128, 512], F32)
        s_var, b_svar = R2.alloc("s_var", [128, 512], F32)
        s_rstd, b_srstd = R2.alloc("s_rstd", [128, 512], F32)
        impacc, b_imp = R2.alloc("impacc", [128, 4, 2, 32], F32)
        score, b_score = R2.alloc("score", [128, 32], F32)
        negsel, b_negsel = R2.alloc("negsel", [128, 32], F32)
        rz, b_rz = R2.alloc("rz", [128, 4], F32)
        coef, b_coef = R2.alloc("coef", [128, 4], F32)
        print("mixer sbuf: R1 end", R1.off, "R2 end", R2.off, "ov_end", ov_end, "limit", SB_END)

        wv = wview(("w_in", l), D, D_IN)
        for kc in range(8):
            P.dma("sp", w_in_sb[:, kc, :], wv[kc * 128:(kc + 1) * 128, :], pwrites=[b_win], **wdep(("w_in", l), D * D_IN))
        wo = wview(("w_out", l), D, D)
        P.dma("sp", w_out_sb, wo.rearrange("(kc p) n -> p kc n", p=128), writes=[b_wout], **wdep(("w_out", l), D * D))
        cw = wview(("cmp_w1", l), 2 * 2048, 64).rearrange("(kv l d) o -> d kv l o", kv=2, l=32)
        for g in range(2):
            for kv in range(2):
                P.dma("sp", w1sb[g * 64:(g + 1) * 64, kv, :, :], cw[:, kv, :, :], pwrites=[b_w1sb], **wdep(("cmp_w1", l), 2 * 2048 * 64))
            P.dma("sp", w2f[g * 64:(g + 1) * 64, :, :], cmp_w2[l].rearrange("kv o p -> o kv p"), pwrites=[b_w2f])
        A("dve", lambda e: e.memset(posT, 0.0), [], [b_posT])
        for kv in range(2):
            load_T(posTf[:, kv, :], b_posTf, 32, [(slice(0, 64), slice(0, 32), cmp_pos[l, kv]), (slice(64, 128), slice(0, 32), cmp_pos[l, kv])])
        A("dve", lambda e: e.tensor_copy(out=posT[:, :, 0:32], in_=posTf), [b_posTf], [b_posT])
        A("dve", lambda e: e.tensor_copy(out=w2sb, in_=w2f), [b_w2f], [b_w2sb])
        for c4 in range(4):
            load_T(convw[:, c4, :], b_convw, 31, [(slice(0, 128), slice(0, 31), conv_w[l][:, c4 * 128:(c4 + 1) * 128])])
        load_T(convp.rearrange("p a b -> p (a b)"), b_convp, 12,
               [(slice(0, 128), slice(4 * i, 4 * i + 4), src[l, :].rearrange("(c p) -> c p", p=128)) for i, src in enumerate((conv_b, conv_g, conv_be))], full=True)
        A("dve", lambda e: e.memset(Vs_aug, 1.0), [], [b_Vs])
        A("dve", lambda e: e.memset(Vw_aug, 1.0), [], [b_Vw])
        A("dve", lambda e: e.memset(yT[:, :, 0:30], 0.0), [], [b_yT])
        for g in range(2):
            A("dve", lambda e, g=g: e.memset(KsP[g][0], 0.0), [], [KsP[g][1]])
            A("dve", lambda e, g=g: e.memset(KwP[g][0], 0.0), [], [KwP[g][1]])
        A("dve", lambda e: e.memset(KcmpT, 0.0), [], [b_KcmpT])
        A("dve", lambda e: e.memset(negselT, 0.0), [], [b_nsT])

        if cfg.get("stop") == "loads":
            P.barrier()
            return
        def p1_T(tc):
            hTt, b_hTt = hT[tc % 2]
            cs = slice(tc * 512, (tc + 1) * 512)
            for t4 in range(4):
                tile = tc * 4 + t4
                xtile, b_x = xt[tile % 2]
                r0 = s * T + tile * 128
                P.dma("sp", xtile, xsrc[r0:r0 + 128, :], reads=[b_xsrc], writes=[b_x])
                for half in range(2):
                    ps, bps = banks[half]
                    for k4 in range(4):
                        kc = half * 4 + k4
                        A("pe", lambda e, ps=ps, k4=k4, kc=kc, xtile=xtile: e.transpose(out=ps[:, k4 * 128:(k4 + 1) * 128], in_=xtile[:, kc * 128:(kc + 1) * 128], identity=ident_f),
                          [b_x, b_identf], [bps] if k4 == 0 else (), pwrites=() if k4 == 0 else [bps])
                    for k4 in range(4):
                        kc = half * 4 + k4
                        o_ap = hTt[:, kc, t4 * 128:(t4 + 1) * 128]
                        i_ap = ps[:, k4 * 128:(k4 + 1) * 128]
                        if half == 0:
                            A("dve", lambda e, o_ap=o_ap, i_ap=i_ap, kc=kc: e.tensor_scalar(out=o_ap, in0=i_ap, scalar1=modcol[:, s, 1, kc:kc + 1], scalar2=modcol[:, s, 0, kc:kc + 1],
                                                                                         op0=ALU.mult, op1=ALU.add), [bps, b_modcol], pwrites=[b_hTt])
                        else:
                            A("act", lambda e, o_ap=o_ap, i_ap=i_ap, kc=kc: e.activation(out=o_ap, in_=i_ap, func=AF.Identity, scale=modcol[:, s, 1, kc:kc + 1], bias=modcol[:, s, 0, kc:kc + 1]),
                              [bps, b_modcol], pwrites=[b_hTt])


        def p1_P(tc):
            hTt, b_hTt = hT[tc % 2]
            cs = slice(tc * 512, (tc + 1) * 512)
            def proj(fo, ps, bps):
                for kc in range(8):
                    A("pe", lambda e, kc=kc: e.matmul(ps, lhsT=w_in_sb[:, kc, fo * 128:(fo + 1) * 128], rhs=hTt[:, kc, :], start=(kc == 0), stop=(kc == 7)),
                      [b_win, b_hTt], [bps] if kc == 0 else (), pwrites=() if kc == 0 else [bps])

            for fo in range(8):
                ps, bps = banks[2 + fo % 2]
                proj(fo, ps, bps)
                if fo < 4:
                    A("act", lambda e, fo=fo, ps=ps: e.activation(out=QT[:, fo, cs], in_=ps, func=AF.Copy, scale=NS), [bps], pwrites=[b_QT])
                elif fo < 6:
                    dst, bd = ((KcT, b_KcT), (VcT, b_VcT))[fo - 4]
                    A("dve", lambda e, dst=dst, ps=ps: e.tensor_copy(out=dst[:, cs], in_=ps), [bps], pwrites=[bd])
                else:
                    KP = KsP if fo == 6 else KwP
                    for g in range(2):
                        A("dve", lambda e, g=g, KP=KP, ps=ps: e.tensor_copy(out=KP[g][0][g * 64:(g + 1) * 64, cs], in_=ps[g * 64:(g + 1) * 64, :]), [bps], pwrites=[KP[g][1]])
            for c in range(4):
                pa, bpa = banks[2 if c % 2 == 0 else 5]
                pg, bpg = banks[3 if c % 2 == 0 else 6]
                proj(8 + c, pa, bpa)
                proj(12 + c, pg, bpg)
                A("act", lambda e, pg=pg: e.activation(out=sig, in_=pg, func=AF.Sigmoid), [bpg], [b_sig])
                A("dve", lambda e, c=c, pa=pa: e.tensor_tensor(out=yT[:, c, 30 + tc * 512:30 + (tc + 1) * 512], in0=pa, in1=sig, op=ALU.mult), [bpa, b_sig], pwrites=[b_yT])
            for t4 in range(4):
                tile = tc * 4 + t4
                ps, bps = banks[7]
                for kc in range(8):
                    A("pe", lambda e, kc=kc, t4=t4, ps=ps: e.matmul(ps[:, 0:280], lhsT=hTt[:, kc, t4 * 128:(t4 + 1) * 128], rhs=w_in_sb[:, kc, 2048:2328], start=(kc == 0), stop=(kc == 7)),
                      [b_win, b_hTt], [bps] if kc == 0 else (), pwrites=() if kc == 0 else [bps])
                A("dve", lambda e, tile=tile, ps=ps: e.tensor_copy(out=Vs_aug[:, tile, :, 0:64], in_=ps[:, 0:128].rearrange("p (g d) -> p g d", g=2)), [bps], pwrites=[b_Vs])
                A("dve", lambda e, tile=tile, ps=ps: e.tensor_copy(out=Vw_aug[:, tile, :, 0:64], in_=ps[:, 128:256].rearrange("p (g d) -> p g d", g=2)), [bps], pwrites=[b_Vw])
                A("act", lambda e, tile=tile, ps=ps: e.activation(out=gsb[:, tile, :], in_=ps[:, 256:280], func=AF.Sigmoid), [bps], pwrites=[b_gsb])


        p1_T(0)
        for tc in range(4):
            if tc + 1 < 4:
                p1_T(tc + 1)
            p1_P(tc)
        if cfg.get("stop") == "p1":
            P.barrier()
            return
        for kv, (src, b_src) in enumerate(((KcT, b_KcT), (VcT, b_VcT))):
            for g in range(2):
                pbs = slice(g * 64, g * 64 + 64)
                psm, bpsm = banks[0 + 2 * g]
                psc, bpsc = banks[1 + 2 * g]
                for l_ in range(32):
                    A("pe", lambda e, l_=l_, pbs=pbs, psc=psc: e.matmul(psc[pbs, 0:2], lhsT=w1sb[pbs, kv, l_, :], rhs=posT[pbs, kv, l_:l_ + 2], start=(l_ == 0), stop=(l_ == 31)),
                      [b_w1sb, b_posT], [bpsc] if l_ == 0 else (), pwrites=() if l_ == 0 else [bpsc])
                for l_ in range(32):
                    A("pe", lambda e, l_=l_, pbs=pbs, psm=psm: e.matmul(psm[pbs, 0:NCMP], lhsT=w1sb[pbs, kv, l_, :], rhs=src[pbs, l_:l_ + 16 * 126 + 1:16], start=(l_ == 0), stop=(l_ == 31)),
                      [b_w1sb, b_src], [bpsm] if l_ == 0 else (), pwrites=() if l_ == 0 else [bpsm])
                A("dve", lambda e, pbs=pbs, psc=psc: e.tensor_copy(out=c1col[pbs, kv:kv + 1], in_=psc[pbs, 0:1]), [bpsc], pwrites=[b_c1col])
                A("act", lambda e, pbs=pbs, psm=psm: e.activation(out=Gt[pbs, 0:NCMP], in_=psm[pbs, 0:NCMP], func=AF.Gelu_apprx_tanh, bias=c1col[pbs, kv:kv + 1]), [bpsm, b_c1col], pwrites=[b_G])
            if kv == 0:
                for g in range(2):
                    pbs = slice(g * 64, g * 64 + 64)
                    ps2, bps2 = banks[4 + g]
                    A("pe", lambda e, pbs=pbs, ps2=ps2: e.matmul(ps2[pbs, 0:NCMP], lhsT=w2sb[pbs, 0, :], rhs=Gt[pbs, 0:NCMP], start=True, stop=True), [b_w2sb, b_G], [bps2])
                    A("dve", lambda e, pbs=pbs, ps2=ps2, g=g: e.tensor_copy(out=KcmpT[pbs, g, 0:NCMP], in_=ps2[pbs, 0:NCMP]), [bps2], pwrites=[b_KcmpT])
            else:
                for g in range(2):
                    pbs = slice(g * 64, g * 64 + 64)
                    ps3, bps3 = banks[6 + g]
                    A("pe", lambda e, pbs=pbs, ps3=ps3: e.matmul(ps3[0:NCMP, 0:64], lhsT=Gt[pbs, 0:NCMP], rhs=w2sb[pbs, 1, :], start=True, stop=True), [b_w2sb, b_G], [bps3])
                    A("dve", lambda e, g=g, ps3=ps3: e.tensor_copy(out=vcmp[0:NCMP, g, 0:64], in_=ps3[0:NCMP, 0:64]), [bps3], pwrites=[b_vcmp])
        P.barrier()
        if cfg.get("stop") == "p2":
            return

        def conv_ops(qc):
            ops = []
            base = qc * 512
            pc, bpc = banks[6]
            batches = [(0, 8), (8, 16), (16, 24), (24, 31)]

            def build(c, bi):
                j0, j1 = batches[bi]
                dgt, b_dg = dg[(c * 4 + bi) % 2]
                for j in range(j0, j1):
                    A("dve", lambda e: e.tensor_scalar(out=dgt[:, j - j0, :], in0=ident_b, scalar1=convw[:, c, j:j + 1], scalar2=None, op0=ALU.mult),
                      [b_identb, b_convw], [b_dg] if j == j0 else (), pwrites=() if j == j0 else [b_dg])

            def mm(c, bi):
                j0, j1 = batches[bi]
                dgt, b_dg = dg[(c * 4 + bi) % 2]
                for j in range(j0, j1):
                    A("pe", lambda e: e.matmul(pc, lhsT=dgt[:, j - j0, :], rhs=yT[:, c, base + j:base + j + 512], start=(j == 0), stop=(j == 30)),
                      [b_dg, b_yT], [bpc] if j == 0 else (), pwrites=() if j == 0 else [bpc])

            def evac(c):
                A("act", lambda e: e.activation(out=convacc[:, c, :], in_=pc, func=AF.Identity, bias=convp[:, 0, c:c + 1]), [bpc, b_convp], [b_cacc] if c == 0 else (), pwrites=() if c == 0 else [b_cacc])

            for c in range(4):
                ops.append(lambda c=c: build(c, 0))
                ops.append(lambda c=c: build(c, 1))
                ops.append(lambda c=c: mm(c, 0))
                ops.append(lambda c=c: build(c, 2))
                ops.append(lambda c=c: mm(c, 1))
                ops.append(lambda c=c: build(c, 3))
                ops.append(lambda c=c: mm(c, 2))
                ops.append(lambda c=c: (mm(c, 3), evac(c)))

            def stats():
                pm, bpm = banks[7]
                pe_, bpe = banks[6]
                for c in range(4):
                    sq, bsq = convsq[c % 2]
                    A("act", lambda e, c=c, sq=sq: e.activation(out=sq, in_=convacc[:, c, :], func=AF.Square), [b_cacc], [bsq])
                    A("pe", lambda e, c=c: e.matmul(pm, lhsT=onesm, rhs=convacc[:, c, :], start=(c == 0), stop=(c == 3)), [b_onesm, b_cacc], [bpm] if c == 0 else (), pwrites=() if c == 0 else [bpm])
                    A("pe", lambda e, c=c, sq=sq: e.matmul(pe_, lhsT=onesm, rhs=sq, start=(c == 0), stop=(c == 3)), [b_onesm, bsq], [bpe] if c == 0 else (), pwrites=() if c == 0 else [bpe])
                A("act", lambda e: e.activation(out=s_mean, in_=pm, func=AF.Copy), [bpm], [b_smean])
                A("dve", lambda e: e.tensor_tensor(out=s_var, in0=s_mean, in1=s_mean, op=ALU.mult), [b_smean], [b_svar])
                A("dve", lambda e: e.tensor_tensor(out=s_var, in0=pe_, in1=s_var, op=ALU.subtract), [bpe, b_svar], [b_svar])
                A("act", lambda e: e.activation(out=s_rstd, in_=s_var, func=AF.Sqrt, bias=epsc[:, 0:1]), [b_svar, b_epsc], [b_srstd])
                A("dve", lambda e: e.reciprocal(out=s_rstd, in_=s_rstd), [b_srstd], [b_srstd])
                for c in range(4):
                    A("dve", lambda e, c=c: e.tensor_tensor(out=convacc[:, c, :], in0=convacc[:, c, :], in1=s_mean, op=ALU.subtract), [b_cacc, b_smean], pwrites=[b_cacc])
                    A("dve", lambda e, c=c: e.tensor_tensor(out=convacc[:, c, :], in0=convacc[:, c, :], in1=s_rstd, op=ALU.mult), [b_cacc, b_srstd], pwrites=[b_cacc])
                    A("act", lambda e, c=c: e.activation(out=o_convT[:, c, base:base + 512], in_=convacc[:, c, :], func=AF.Silu, scale=convp[:, 1, c:c + 1], bias=convp[:, 2, c:c + 1]),
                      [b_cacc, b_convp], pwrites=[b_ocT])
            ops.append(stats)
            return ops

        cnt = {"o": 0, "s": 0}

        def finish_branch(h, br, W, qc, psO, bpsO):
            osb, b_osb = Osb[cnt["o"] % 2]
            cnt["o"] += 1
            W2 = W + 1
            A("act", lambda e: e.activation(out=osb[0:W, :], in_=psO[0:W, :], func=AF.Copy), [bpsO], [b_osb])
            pT, bpT = banks[4]
            for qt in range(4):
                A("pe", lambda e, qt=qt: e.transpose(out=pT[:, qt * 128:qt * 128 + W2], in_=osb[0:W2, qt * 128:(qt + 1) * 128], identity=ident_f[0:W2, 0:W2]),
                  [b_osb, b_identf], [bpT] if qt == 0 else (), pwrites=() if qt == 0 else [bpT])
            pT3 = pT.rearrange("p (q w) -> p q w", w=128)
            A("dve", lambda e: e.tensor_scalar(out=rz, in0=pT3[:, :, 64], scalar1=1e-30, scalar2=None, op0=ALU.max), [bpT], [b_rz])
            A("dve", lambda e: e.reciprocal(out=rz, in_=rz), [b_rz], [b_rz])
            A("dve", lambda e: e.tensor_tensor(out=coef, in0=rz, in1=gsb[:, qc * 4:(qc + 1) * 4, h * 3 + br], op=ALU.mult), [b_rz, b_gsb], [b_coef])
            for qt in range(4):
                o_ap = o_nsa[:, qt, h * 64:(h + 1) * 64]
                if br == 0:
                    A("dve", lambda e, qt=qt, o_ap=o_ap: e.tensor_scalar(out=o_ap, in0=pT3[:, qt, 0:64], scalar1=coef[:, qt:qt + 1], scalar2=None, op0=ALU.mult), [bpT, b_coef], pwrites=[b_on])
                else:
                    A("dve", lambda e, qt=qt, o_ap=o_ap: e.scalar_tensor_tensor(out=o_ap, in0=pT3[:, qt, 0:64], scalar=coef[:, qt:qt + 1], in1=o_ap, op0=ALU.mult, op1=ALU.add),
                      [bpT, b_coef], pwrites=[b_on])
                if br == 0:
                    g = h // 4
                    i_ap = impacc[:, qt, g, :]
                    if h % 4 == 0:
                        A("dve", lambda e, qt=qt, i_ap=i_ap: e.tensor_scalar(out=i_ap, in0=pT3[:, qt, 65:97], scalar1=rz[:, qt:qt + 1], scalar2=None, op0=ALU.mult), [bpT, b_rz], pwrites=[b_imp])
                    else:
                        A("dve", lambda e, qt=qt, i_ap=i_ap: e.scalar_tensor_tensor(out=i_ap, in0=pT3[:, qt, 65:97], scalar=rz[:, qt:qt + 1], in1=i_ap, op0=ALU.mult, op1=ALU.add),
                          [bpT, b_rz], pwrites=[b_imp])

        for qc in range(4):
            qs = slice(qc * 512, (qc + 1) * 512)
            cops = conv_ops(qc)
            per = (len(cops) + 23) // 24

            def drain(n):
                for _ in range(n):
                    if cops:
                        cops.pop(0)()

            if cfg.get("stop") == "att" and qc == 1:
                return
            pend = []

            def cmp_a(h):
                fo, g = h % 4, h // 4
                bcs, b_bcs = Bcs[h % 2]
                P.dma("sp", bcs[0:NCMP, :], bc_dram[h * 128:h * 128 + NCMP, qs], reads=[b_bc], writes=[b_bcs])
                psS, bpsS = banks[h % 2]
                A("pe", lambda e: e.matmul(psS[0:NCMP, :], lhsT=KcmpT[:, g, 0:NCMP], rhs=QT[:, fo, qs], start=True, stop=False, skip_group_check=True), [b_KcmpT, b_QT], [bpsS])
                A("pe", lambda e: e.matmul(psS[0:NCMP, :], lhsT=ident_b[0:NCMP, 0:NCMP], rhs=bcs[0:NCMP, :], start=False, stop=True, skip_group_check=True), [b_identb, b_bcs], pwrites=[bpsS])
                pt, b_pt = PTb[h % 2]
                A("act", lambda e: e.activation(out=pt[0:NCMP, :], in_=psS[0:NCMP, :], func=AF.Exp, bias=relb[0:NCMP, 31, h:h + 1]), [bpsS, b_relb], [b_pt])

            def cmp_b(h):
                g = h // 4
                pt, b_pt = PTb[h % 2]
                psO, bpsO = banks[2 + h % 2]
                A("pe", lambda e: e.matmul(psO[0:97, :], lhsT=vcmp[0:NCMP, g, :], rhs=pt[0:NCMP, :], start=True, stop=True), [b_vcmp, b_pt], [bpsO])
                pend.append(lambda: finish_branch(h, 0, 97, qc, psO, bpsO))

            cmp_a(0)
            for h in range(8):
                if h + 1 < 8:
                    cmp_a(h + 1)
                cmp_b(h)
                if len(pend) > 1:
                    pend.pop(0)()
                drain(per)
            while pend:
                pend.pop(0)()
            for g in range(2):
                p5, bp5 = banks[5]
                for qt in range(4):
                    A("dve", lambda e, qt=qt, g=g: e.tensor_tensor(out=score, in0=impacc[:, qt, g, :], in1=fm_sb[:, qc * 4 + qt, :], op=ALU.add), [b_imp, b_fm], [b_score])
                    A("dve", lambda e: e.max(out=sm8[:, 0:8], in_=score), [b_score], [b_sm8])
                    A("dve", lambda e: e.tensor_scalar(out=negsel, in0=score, scalar1=sm8[:, 7:8], scalar2=NEG, op0=ALU.is_lt, op1=ALU.mult), [b_score, b_sm8], [b_negsel])
                    A("pe", lambda e, qt=qt: e.transpose(out=p5[0:32, qt * 128:(qt + 1) * 128], in_=negsel, identity=ident_f), [b_negsel, b_identf], [bp5] if qt == 0 else (), pwrites=() if qt == 0 else [bp5])
                A("act", lambda e, g=g: e.activation(out=negselT[0:32, g, :], in_=p5[0:32, :], func=AF.Copy), [bp5], pwrites=[b_nsT])
            pend = []
            for br in (1, 2):
                for h in range(8):
                    fo, g = h % 4, h // 4
                    kts = list(range(0, 4 * qc + 4)) if br == 1 else list(range(max(0, 4 * qc - 4), 4 * qc + 4))
                    KT, b_KT = KsP[g] if br == 1 else KwP[g]
                    Va, b_Va = (Vs_aug, b_Vs) if br == 1 else (Vw_aug, b_Vw)
                    psO, bpsO = banks[2 + cnt["o2"] % 2] if "o2" in cnt else banks[2]
                    cnt["o2"] = cnt.get("o2", 0) + 1
                    steps = []

                    def qk(i):
                        kt = kts[i]
                        sidx = cnt["s"]
                        cnt["s"] += 1
                        psS, bpsS = banks[(0, 1, 5)[sidx % 3]]
                        pt, b_pt = PTb[sidx % 3]
                        qlo = max(kt, 4 * qc)
                        qhi = 4 * qc + 3 if br == 1 else min(kt + 4, 4 * qc + 3)
                        c0, c1 = (qlo - 4 * qc) * 128, (qhi - 4 * qc + 1) * 128
                        mm = [(psS[:, c0:c1], KT[:, kt * 128:(kt + 1) * 128], QT[:, fo, qc * 512 + c0:qc * 512 + c1], [b_KT, b_QT])]
                        if br == 1:
                            mm.append((psS[:, c0:c1], emat_b[:, kt * 128:(kt + 1) * 128], negselT[:, g, c0:c1], [b_emat, b_nsT]))
                        if kt >= 4 * qc:
                            j = kt - 4 * qc
                            mm.append((psS[:, j * 128:(j + 1) * 128], ident_b, bt0[:, h, :], [b_identb, b_bt0]))
                        j = kt + 1 - 4 * qc
                        if 0 <= j <= 3:
                            mm.append((psS[:, j * 128:(j + 1) * 128], ident_b, bt1[:, h, :], [b_identb, b_bt1]))
                        j = kt + 4 - 4 * qc
                        if br == 2 and 0 <= j <= 3:
                            mm.append((psS[:, j * 128:(j + 1) * 128], ident_b, edge_b, [b_identb, b_edge]))
                        n = len(mm)
                        for mi, (o_, l_, r_, rd) in enumerate(mm):
                            A("pe", lambda e: e.matmul(o_, lhsT=l_, rhs=r_, start=(mi == 0), stop=(mi == n - 1), skip_group_check=True),
                              rd, [bpsS] if mi == 0 else (), pwrites=() if mi == 0 else [bpsS])
                        A("act", lambda e: e.activation(out=pt[:, c0:c1], in_=psS[:, c0:c1], func=AF.Exp, bias=relb[:, 31, h:h + 1]), [bpsS, b_relb], [b_pt])
                        steps.append((kt, c0, c1, pt, b_pt))

                    def pv(i):
                        kt, c0, c1, pt, b_pt = steps[i]
                        n = len(kts)
                        A("pe", lambda e: e.matmul(psO[0:65, c0:c1], lhsT=Va[:, kt, g, :], rhs=pt[:, c0:c1], start=(i == 0), stop=(i == n - 1), skip_group_check=True),
                          [b_Va, b_pt], [bpsO] if i == 0 else (), pwrites=() if i == 0 else [bpsO])

                    qk(0)
                    if len(kts) > 1:
                        qk(1)
                    for i in range(len(kts)):
                        if i + 2 < len(kts):
                            qk(i + 2)
                        pv(i)
                        if i == min(1, len(kts) - 1) and pend:
                            pend.pop(0)()
                    pend.append(lambda h=h, br=br, psO=psO, bpsO=bpsO: finish_branch(h, br, 65, qc, psO, bpsO))
                    drain(per)
            while pend:
                pend.pop(0)()
            drain(len(cops))
            for qt in range(4):
                p5, bp5 = banks[5]
                for fc in range(4):
                    A("pe", lambda e, qt=qt, fc=fc: e.transpose(out=p5[:, fc * 128:(fc + 1) * 128], in_=o_nsa[:, qt, fc * 128:(fc + 1) * 128], identity=ident_f), [b_on, b_identf],
                      [bp5] if fc == 0 else (), pwrites=() if fc == 0 else [bp5])
                A("act", lambda e, qt=qt: e.activation(out=o_nsaT[:, :, qc * 512 + qt * 128:qc * 512 + (qt + 1) * 128], in_=p5.rearrange("p (f q) -> p f q", f=4), func=AF.Copy), [bp5], pwrites=[b_onT])

        (g1b, b_g1b), (lng, b_lng), (lnb, b_lnb) = bcast
        rowb(g1b, b_g1b, modrow[li * NSEQ + s:li * NSEQ + s + 1, 2 * 1024:3 * 1024], reads=[b_modrow])
        rowb(lng, b_lng, ln_g[l, 0:1, :])
        rowb(lnb, b_lnb, ln_b[l, 0:1, :])
        for tile in range(16):
            ts_ = slice(tile * 128, (tile + 1) * 128)
            xtile, b_x = xt[tile % 2]
            ttile, b_t = tt_[tile % 2]
            r0 = s * T + tile * 128
            P.dma("sp", xtile, xsrc[r0:r0 + 128, :], reads=[b_xsrc], writes=[b_x])
            for half in range(2):
                ps, bps = banks[(tile % 2) * 2 + half]
                for k in range(8):
                    lhsT = o_nsaT[:, k, ts_] if k < 4 else o_convT[:, k - 4, ts_]
                    A("pe", lambda e, ps=ps, lhsT=lhsT, k=k, half=half: e.matmul(ps, lhsT=lhsT, rhs=w_out_sb[:, k, half * 512:(half + 1) * 512], start=(k == 0), stop=(k == 7)),
                      [b_onT, b_ocT, b_wout], [bps] if k == 0 else (), pwrites=() if k == 0 else [bps])
                A("dve", lambda e, ps=ps, half=half, ttile=ttile: e.tensor_tensor(out=ttile[:, half * 512:(half + 1) * 512], in0=ps, in1=g1b[:, half * 512:(half + 1) * 512], op=ALU.mult),
                  [bps, b_g1b], [b_t] if half == 0 else (), pwrites=() if half == 0 else [b_t])
            A("dve", lambda e, xtile=xtile, ttile=ttile: e.scalar_tensor_tensor(out=ttile, in0=xtile, scalar=ALPHA, in1=ttile, op0=ALU.mult, op1=ALU.add), [b_x, b_t], [b_t])
            layer_norm_tile(ttile, b_t, lng, b_lng, lnb, b_lnb)
            P.dma("sp", xdst[r0:r0 + 128, :], ttile, reads=[b_t], pwrites=[b_xdst], owner=b_t)

        if cfg.get("dump") and s == 0:
            P.barrier()
            b_dmp = getbuf("dmp")
            items = [QT[:, 0, 0:1024], KsP[0][0][:, 0:1024], KwP[1][0][:, 0:1024], yT[:, 0, 30:1054], o_convT[:, 0, 0:1024], o_nsaT[:, 0, 0:1024],
                     gsb.rearrange("p a b -> p (a b)"), Vs_aug.rearrange("p a b c -> p (a b c)")[:, 0:1024], KcmpT.rearrange("p a b -> p (a b)"),
                     vcmp.rearrange("p a b -> p (a b)"), negselT.rearrange("p a b -> p (a b)"), impacc.rearrange("p a b c -> p (a b c)"),
                     o_nsaT[:, 0, 1024:2048], o_convT[:, 3, 1024:2048], QT[:, 3, 1024:2048]]
            for i, ap_ in enumerate(items):
                w_ = ap_.shape[1]
                P.dma("pool", out[T + i * 128:T + (i + 1) * 128, 0:w_], ap_, pwrites=[b_dmp], owner=b_dmp)
            P.barrier()
    def ffn_phase(li, l, s, xsrc, b_xsrc, xdst, b_xdst):
        moe = (l % 2 == 1)
        R = Arena(PH, SB_END)
        h2T, b_h2T = R.alloc("h2T", [128, 8, T], BF16)
        out_acc, _ = R.alloc("out_acc", [128, 16, D], F32)
        b_acc = [getbuf(f"acc{i}") for i in range(16)]
        w1s = [R.alloc(f"w1s{i}", [128, 8, 2, 256], BF16) for i in range(2)]
        w2s = [R.alloc(f"w2s{i}", [128, 2, D], BF16) for i in range(2)]
        actT = [R.alloc(f"actT{i}", [128, 2, 512], BF16) for i in range(2)]
        sgs = [R.alloc(f"sg{i}", [128, 512], F32) for i in range(2)]
        h2f = [R.alloc(f"h2f{i}", [128, 8, 128], F32) for i in range(2)]
        lg, b_lg = R.alloc("lg", [128, 8], F32)
        ex8, b_ex8 = R.alloc("ex8", [128, 8], F32)
        mk8, b_mk8 = R.alloc("mk8", [128, 8], F32)
        den, b_den = R.alloc("den", [128, 2], F32)
        (g2b, b_g2b), (lng, b_lng), (lnb, b_lnb) = bcast
        rowb(g2b, b_g2b, modrow[li * NSEQ + s:li * NSEQ + s + 1, 5 * 1024:6 * 1024], reads=[b_modrow])
        rowb(lng, b_lng, ln_g[l, 1:2, :])
        rowb(lnb, b_lnb, ln_b[l, 1:2, :])
        if moe:
            P.dma("sp", rw_sb, router_w[l // 2].rearrange("(kc p) e -> p kc e", p=128), writes=[b_rw])
        for tile in range(16):
            ts_ = slice(tile * 128, (tile + 1) * 128)
            xtile, b_x = xt[tile % 2]
            r0 = s * T + tile * 128
            P.dma("sp", xtile, xsrc[r0:r0 + 128, :], reads=[b_xsrc], writes=[b_x])
            hf_, b_hf = h2f[tile % 2]
            for half in range(2):
                ps, bps = banks[half]
                for k4 in range(4):
                    kc = half * 4 + k4
                    A("pe", lambda e, ps=ps, k4=k4, kc=kc, xtile=xtile: e.transpose(out=ps[:, k4 * 128:(k4 + 1) * 128], in_=xtile[:, kc * 128:(kc + 1) * 128], identity=ident_f),
                      [b_x, b_identf], [bps] if k4 == 0 else (), pwrites=() if k4 == 0 else [bps])
                for k4 in range(4):
                    kc = half * 4 + k4
                    i_ap = ps[:, k4 * 128:(k4 + 1) * 128]
                    o_ap = hf_[:, kc, :] if moe else h2T[:, kc, ts_]
                    bo = b_hf if moe else b_h2T
                    if k4 % 2 == 0:
                        A("dve", lambda e, o_ap=o_ap, i_ap=i_ap, kc=kc: e.tensor_scalar(out=o_ap, in0=i_ap, scalar1=modcol[:, s, 4, kc:kc + 1], scalar2=modcol[:, s, 3, kc:kc + 1],
                                                                                     op0=ALU.mult, op1=ALU.add), [bps, b_modcol], pwrites=[bo])
                    else:
                        A("act", lambda e, o_ap=o_ap, i_ap=i_ap, kc=kc: e.activation(out=o_ap, in_=i_ap, func=AF.Identity, scale=modcol[:, s, 4, kc:kc + 1], bias=modcol[:, s, 3, kc:kc + 1]),
                          [bps, b_modcol], pwrites=[bo])
            if moe:
                A("act", lambda e, hf_=hf_, ts_=ts_: e.activation(out=h2T[:, :, ts_], in_=hf_, func=AF.Copy), [b_hf], pwrites=[b_h2T])
                p4, bp4 = banks[4]
                for kc in range(8):
                    A("pe", lambda e, kc=kc, hf_=hf_: e.matmul(p4[:, 0:8], lhsT=hf_[:, kc, :], rhs=rw_sb[:, kc, :], start=(kc == 0), stop=(kc == 7)), [b_hf, b_rw],
                      [bp4] if kc == 0 else (), pwrites=() if kc == 0 else [bp4])
                A("dve", lambda e: e.tensor_copy(out=lg, in_=p4[:, 0:8]), [bp4], [b_lg])
                A("dve", lambda e: e.max(out=sm8[:, 0:8], in_=lg), [b_lg], [b_sm8])
                A("dve", lambda e: e.tensor_scalar(out=sm8[:, 8:9], in0=sm8[:, 0:1], scalar1=-1.0, scalar2=None, op0=ALU.mult), [b_sm8], [b_sm8])
                A("act", lambda e: e.activation(out=ex8, in_=lg, func=AF.Exp, bias=sm8[:, 8:9]), [b_lg, b_sm8], [b_ex8])
                A("dve", lambda e: e.tensor_scalar(out=mk8, in0=lg, scalar1=sm8[:, 1:2], scalar2=None, op0=ALU.is_ge), [b_lg, b_sm8], [b_mk8])
                A("dve", lambda e: e.tensor_tensor(out=mk8, in0=mk8, in1=ex8, op=ALU.mult), [b_mk8, b_ex8], [b_mk8])
                A("dve", lambda e: e.reduce_sum(out=den[:, 0:1], in_=mk8, axis=mybir.AxisListType.X), [b_mk8], [b_den])
                A("dve", lambda e: e.reciprocal(out=den[:, 1:2], in_=den[:, 0:1]), [b_den], [b_den])
                A("dve", lambda e, tile=tile: e.tensor_scalar(out=we_sb[:, tile, :], in0=mk8, scalar1=den[:, 1:2], scalar2=None, op0=ALU.mult), [b_mk8, b_den], pwrites=[b_we])
        if moe:
            specs = [(("moe1", l, e_), ("moe2", l, e_), D_FFE, e_) for e_ in range(NE)]
        else:
            specs = [(("ffn1", l), ("ffn2", l), D_FF, None)]
        slices = []
        for (k1, k2, F, ex) in specs:
            for f0 in range(0, F, 256):
                slices.append((k1, k2, F, ex, f0))
        loaded = {}

        def load(si):
            if si in loaded or si >= len(slices):
                return
            k1, k2, F, ex, f0 = slices[si]
            W1 = wview(k1, D, 2 * F)
            W2 = wview(k2, F, D)
            w1t, b_w1 = w1s[si % 2]
            w2t, b_w2 = w2s[si % 2]
            P.dma("sp", w1t[:, :, 0, :], W1[:, f0:f0 + 256].rearrange("(kc p) n -> p kc n", p=128), pwrites=[b_w1], **wdep(k1, D * 2 * F))
            P.dma("sp", w1t[:, :, 1, :], W1[:, F + f0:F + f0 + 256].rearrange("(kc p) n -> p kc n", p=128), pwrites=[b_w1], **wdep(k1, D * 2 * F))
            P.dma("sp", w2t, W2[f0:f0 + 256, :].rearrange("(fc p) n -> p fc n", p=128), writes=[b_w2], **wdep(k2, F * D))
            loaded[si] = True

        def gu(si, tc, fc):
            load(si)
            w1t, b_w1 = w1s[si % 2]
            aT, b_aT = actT[(si * 4 + tc) % 2]
            cs = slice(tc * 512, (tc + 1) * 512)
            psG, bpsG = banks[(fc % 2) * 2]
            psU, bpsU = banks[(fc % 2) * 2 + 1]
            sg, b_sg = sgs[fc % 2]
            for gu_, (ps, bps) in enumerate(((psG, bpsG), (psU, bpsU))):
                for kc in range(8):
                    A("pe", lambda e: e.matmul(ps, lhsT=w1t[:, kc, gu_, fc * 128:(fc + 1) * 128], rhs=h2T[:, kc, cs], start=(kc == 0), stop=(kc == 7)),
                      [b_w1, b_h2T], [bps] if kc == 0 else (), pwrites=() if kc == 0 else [bps])
            A("act", lambda e: e.activation(out=sg, in_=psG, func=AF.Silu), [bpsG], [b_sg])
            A("dve", lambda e: e.tensor_tensor(out=aT[:, fc, :], in0=psU, in1=sg, op=ALU.mult), [bpsU, b_sg], [b_aT] if fc == 0 else (), pwrites=() if fc == 0 else [b_aT])

        def outp(si, tc, hq):
            k1, k2, F, ex, f0 = slices[si]
            w2t, b_w2 = w2s[si % 2]
            aT, b_aT = actT[(si * 4 + tc) % 2]
            first = (si == 0)
            for t4 in (2 * hq, 2 * hq + 1):
                tile = tc * 4 + t4
                for half in range(2):
                    psO, bpsO = banks[4 + (t4 * 2 + half) % 4]
                    for fc in range(2):
                        A("pe", lambda e: e.matmul(psO, lhsT=aT[:, fc, t4 * 128:(t4 + 1) * 128], rhs=w2t[:, fc, half * 512:(half + 1) * 512], start=(fc == 0), stop=(fc == 1)),
                          [b_aT, b_w2], [bpsO] if fc == 0 else (), pwrites=() if fc == 0 else [bpsO])
                    o_ap = out_acc[:, tile, half * 512:(half + 1) * 512]
                    if moe:
                        wcol = we_sb[:, tile, ex:ex + 1]
                        if first:
                            A("dve", lambda e: e.tensor_scalar(out=o_ap, in0=psO, scalar1=wcol, scalar2=None, op0=ALU.mult), [bpsO, b_we], pwrites=[b_acc[tile]])
                        else:
                            A("dve", lambda e: e.scalar_tensor_tensor(out=o_ap, in0=psO, scalar=wcol, in1=o_ap, op0=ALU.mult, op1=ALU.add), [bpsO, b_we], pwrites=[b_acc[tile]])
                    else:
                        if first:
                            A("dve", lambda e: e.tensor_copy(out=o_ap, in_=psO), [bpsO], pwrites=[b_acc[tile]])
                        else:
                            A("dve", lambda e: e.tensor_tensor(out=o_ap, in0=psO, in1=o_ap, op=ALU.add), [bpsO], pwrites=[b_acc[tile]])

        units = [(si, tc) for si in range(len(slices)) for tc in range(4)]
        gu(units[0][0], units[0][1], 0)
        gu(units[0][0], units[0][1], 1)
        for ui, (si, tc) in enumerate(units):
            if ui + 1 < len(units):
                nsi, ntc = units[ui + 1]
                gu(nsi, ntc, 0)
                outp(si, tc, 0)
                gu(nsi, ntc, 1)
                outp(si, tc, 1)
            else:
                outp(si, tc, 0)
                outp(si, tc, 1)
        for tile in range(16):
            xtile, b_x = xt[tile % 2]
            ttile, b_t = tt_[tile % 2]
            r0 = s * T + tile * 128
            P.dma("sp", xtile, xsrc[r0:r0 + 128, :], reads=[b_xsrc], writes=[b_x])
            A("dve", lambda e, tile=tile, ttile=ttile: e.tensor_tensor(out=ttile, in0=out_acc[:, tile, :], in1=g2b, op=ALU.mult), [b_acc[tile], b_g2b], [b_t])
            A("dve", lambda e, xtile=xtile, ttile=ttile: e.scalar_tensor_tensor(out=ttile, in0=xtile, scalar=ALPHA, in1=ttile, op0=ALU.mult, op1=ALU.add), [b_x, b_t], [b_t])
            layer_norm_tile(ttile, b_t, lng, b_lng, lnb, b_lnb)
            P.dma("sp", xdst[r0:r0 + 128, :], ttile, reads=[b_t], pwrites=[b_xdst], owner=b_t)

    cur, b_cur = x_in, Buf("x_in")
    for li, l in enumerate(layers):
        if cfg.get("stop") == "setup":
            break
        ada_phase(li, l)
        P.barrier()
        if cfg.get("stop") == "ada":
            break
        last = (li == len(layers) - 1)
        dst, b_dst = (out, b_out) if last else (xb, b_xb)
        for s in range(NSEQ):
            if cfg.get("mixer_only"):
                if cfg.get("dump") and s > 0:
                    continue
                mixer_phase(li, l, s, cur, b_cur, dst, b_dst)
                P.barrier()
                continue
            if cfg.get("ffn_only"):
                ffn_phase(li, l, s, cur, b_cur, dst, b_dst)
                P.barrier()
                continue
            mixer_phase(li, l, s, cur, b_cur, xa, b_xa)
            P.barrier()
            ffn_phase(li, l, s, xa, b_xa, dst, b_dst)
            P.barrier()
        cur, b_cur = xb, b_xb
    if cfg.get("stop"):
        A("dve", lambda e: e.memset(tt_[0][0], 0.0), [], [tt_[0][1]])
        if cfg["stop"] == "ada":
            A("dve", lambda e: e.tensor_copy(out=tt_[0][0][:, 0:NSEQ * 48], in_=modcol.rearrange("p s a b -> p (s a b)")), [b_modcol], [tt_[0][1]])
        P.dma("sp", out[0:128, :], tt_[0][0], reads=[tt_[0][1]], pwrites=[b_out], owner=tt_[0][1])
        if cfg["stop"] == "ada":
            P.dma("sp", out[128:128 + NSEQ, :], modrow[0:NSEQ, 0:1024], reads=[b_modrow], pwrites=[b_out], owner=b_modrow)
    P.wait_all("sp", [b_out])
    P.emit()
    return nc, NCH


SMALL = ["cmp_pos", "cmp_w2", "conv_w", "conv_b", "conv_ln_g", "conv_ln_b", "ada_b", "ln_g", "ln_b", "router_w"]


def run(inp, cfg, n_cores, trace=False):
    layers = cfg["layers"]
    NSEQ = cfg["nseq"]
    nc, NCH = build(cfg)
    flat, nch = pack_flat(inp, layers)
    consts = host_consts()
    x = np.asarray(inp["x"], np.float32)
    c = np.asarray(inp["c"], np.float32)
    in_maps = []
    for core in range(n_cores):
        m = {"x": np.ascontiguousarray(x[core * NSEQ:(core + 1) * NSEQ].reshape(NSEQ * T, D)),
             "c": np.ascontiguousarray(c[core * NSEQ:(core + 1) * NSEQ])}
        if cfg["gather"]:
            p = ORDER.index(core)
            m["wsh"] = np.ascontiguousarray(flat[:, p].reshape(nch * 512, 1024))
        else:
            m["wfull"] = flat.reshape(nch * 4096, 1024)
        for k in SMALL:
            m[k] = np.ascontiguousarray(np.asarray(inp[k], np.float32))
        m["rel_bias"] = np.ascontiguousarray(np.asarray(inp["rel_bias"], np.float32).reshape(1, 256))
        m.update(consts)
        in_maps.append(m)
    res = run_bass_kernel_spmd(nc, in_maps, core_ids=list(range(n_cores)), trace=trace)
    outs = [r["out"].reshape(NSEQ, T, D) for r in res.results]
    return np.concatenate(outs, 0), res


def kernel(**inputs):
    cfg = dict(nseq=2, layers=[0, 1, 2, 3], gather=True)
    o, _ = run(inputs, cfg, 8)
    return o.astype(np.float32)
```

```python
import math
import contextlib
import types
import numpy as np
import concourse.bass as bass
import concourse.mybir as mybir
from concourse.bass_utils import run_bass_kernel_spmd

F32 = mybir.dt.float32
BF16 = mybir.dt.bfloat16
ALU = mybir.AluOpType
AF = mybir.ActivationFunctionType

D = 1024
T = 2048
DEPTH = 4
NH = 8
HD = 64
D_IN = 2328
D_FF = 2816
D_FFE = 3584
NE = 8
NCMP = 127
ALPHA = (2 * DEPTH) ** 0.25
LN_EPS = 1e-5
NEG = -30000.0
CHUNK_ELEMS = 4096 * 1024
ORDER = [0, 1, 4, 5, 2, 3, 6, 7]
EPOCH = 16000
SEG_CH = 28
SEG_ELEMS = SEG_CH * CHUNK_ELEMS


class Buf:
    __slots__ = ("name", "w", "wf", "r", "sem", "semcnt", "excl")

    def __init__(self, name, excl=False):
        self.name = name
        self.excl = excl
        self.w = {}
        self.wf = {}
        self.r = {}
        self.sem = None
        self.semcnt = 0


def _snap(fn):
    if fn is None or fn.__closure__ is None:
        return fn
    cells = []
    for c in fn.__closure__:
        try:
            cells.append(types.CellType(c.cell_contents))
        except ValueError:
            cells.append(c)
    g = types.FunctionType(fn.__code__, fn.__globals__, fn.__name__, fn.__defaults__, tuple(cells))
    g.__kwdefaults__ = fn.__kwdefaults__
    return g


class Prog:
    def __init__(self, nc):
        self.nc = nc
        self.stack = contextlib.ExitStack()
        self.engnames = ["pe", "act", "dve", "pool", "sp"]
        self.streams = {k: [] for k in self.engnames}
        self.count = {k: 0 for k in self.engnames}
        self.esems = {k: [] for k in self.engnames}
        self.seen = {k: {} for k in self.engnames}
        self.dmabufs = []
        self.nobarrier = set()
        self.nsem = 0

    def new_sem(self, name):
        self.nsem += 1
        return self.stack.enter_context(self.nc.semaphore(name))

    def _esem(self, k, epoch):
        while len(self.esems[k]) <= epoch:
            self.esems[k].append(self.new_sem(f"e_{k}_{len(self.esems[k])}"))
        return self.esems[k][epoch]

    def _need(self, k, wt, waits):
        if wt[0] == "e":
            _, e2, c = wt
            if e2 == "pe" and k == "pe":
                return
            key = e2
        else:
            _, b, c = wt
            key = ("d", id(b))
        if self.seen[k].get(key, 0) >= c:
            return
        self.seen[k][key] = c
        waits.append(wt)

    def _deps(self, k, reads, writes, pwrites):
        waits = []
        for b in reads:
            for wt in b.w.values():
                self._need(k, wt, waits)
            if b.excl:
                for kk, wt in b.r.items():
                    if kk != k:
                        self._need(k, wt, waits)
        for b in writes:
            for wt in b.w.values():
                self._need(k, wt, waits)
            for wt in b.r.values():
                self._need(k, wt, waits)
        for b in pwrites:
            for wt in b.r.values():
                self._need(k, wt, waits)
            for wt in b.wf.values():
                self._need(k, wt, waits)
        return waits

    def _record(self, key, me, reads, writes, pwrites):
        for b in reads:
            b.r[key] = me
        for b in writes:
            b.w = {key: me}
            b.wf = {key: me}
            b.r = {}
        for b in pwrites:
            if b.r:
                b.w = {key: me}
                b.wf = {}
                b.r = {}
            else:
                b.w[key] = me

    def op(self, k, fn, reads=(), writes=(), pwrites=(), extra=()):
        waits = self._deps(k, reads, writes, pwrites)
        for wt in extra:
            self._need(k, wt, waits)
        self.count[k] += 1
        me = ("e", k, self.count[k])
        self._record(k, me, reads, writes, pwrites)
        self.streams[k].append((waits, _snap(fn), me))

    def dma(self, q, out_ap, in_ap, reads=(), writes=(), pwrites=(), owner=None, extra=(), fn=None, inc=16, **kw):
        if owner is None:
            owner = (list(writes) + list(pwrites))[0]
        if owner.sem is None:
            owner.sem = self.new_sem("d_" + owner.name)
            self.dmabufs.append(owner)
        waits = self._deps(q, reads, writes, pwrites)
        for wt in extra:
            self._need(q, wt, waits)
        owner.semcnt += inc
        me = ("d", owner, owner.semcnt)
        self._record(("d", id(owner)), me, reads, writes, pwrites)
        if fn is None:
            fn = lambda eng, o=out_ap, i=in_ap, kw=kw: eng.dma_start(out=o, in_=i, **kw)
        self.streams[q].append((waits, _snap(fn), ("dinc", owner, inc)))

    def barrier(self):
        snap = dict(self.count)
        dsn = [(b, b.semcnt) for b in self.dmabufs if b.name not in self.nobarrier]
        for k in self.engnames:
            waits = []
            for e2 in self.engnames:
                if e2 != k and snap[e2] > 0:
                    self._need(k, ("e", e2, snap[e2]), waits)
            for b, c in dsn:
                if c > 0:
                    self._need(k, ("d", b, c), waits)
            self.streams[k].append((waits, None, None))

    def wait_all(self, k, bufs):
        waits = self._deps(k, bufs, (), ())
        self.streams[k].append((waits, None, None))

    def emit(self):
        nc = self.nc
        engattr = {"pe": "tensor", "act": "scalar", "dve": "vector", "pool": "gpsimd", "sp": "sync"}
        for k in self.engnames:
            self._esem(k, max(self.count[k] - 1, 0) // EPOCH)
        with nc.Block() as block:
            for k in self.engnames:
                stream = self.streams[k]

                def body(eng, k=k, stream=stream):
                    for waits, fn, inc in stream:
                        for wt in waits:
                            if wt[0] == "e":
                                ep, cc = divmod(wt[2] - 1, EPOCH)
                                eng.wait_ge(self.esems[wt[1]][ep], cc + 1)
                            else:
                                eng.wait_ge(wt[1].sem, wt[2])
                        if fn is None:
                            continue
                        ins = fn(eng)
                        if inc[0] == "e":
                            ep, cc = divmod(inc[2] - 1, EPOCH)
                            ins.then_inc(self.esems[k][ep], 1)
                        else:
                            ins.then_inc(inc[1].sem, inc[2])

                getattr(block, engattr[k])(body)
        self.stack.close()


def flat_layout(layers):
    off = 0
    lay = {}

    def add(name, n):
        nonlocal off
        if (off // SEG_ELEMS) != ((off + n - 1) // SEG_ELEMS):
            off = ((off + n - 1) // SEG_ELEMS) * SEG_ELEMS
        lay[name] = off
        off += n

    for l in layers:
        add(("ada", l), D * 6 * D)
        add(("w_in", l), D * D_IN)
        add(("cmp_w1", l), 2 * 2048 * 64)
        add(("w_out", l), D * D)
        if l % 2 == 0:
            add(("ffn1", l), D * 2 * D_FF)
            add(("ffn2", l), D_FF * D)
        else:
            for e in range(NE):
                add(("moe1", l, e), D * 2 * D_FFE)
                add(("moe2", l, e), D_FFE * D)
    nch = (off + CHUNK_ELEMS - 1) // CHUNK_ELEMS
    return lay, off, nch


def w_in_perm():
    cols = []
    for fo in range(4):
        cols += list(range(fo * 64, fo * 64 + 64)) + list(range((4 + fo) * 64, (4 + fo) * 64 + 64))
    q_end = 512
    kc, vc, ks, vs, kw, vw = [q_end + i * 128 for i in range(6)]
    gl = q_end + 6 * 128
    cv = gl + 24
    cols += list(range(kc, kc + 128)) + list(range(vc, vc + 128)) + list(range(ks, ks + 128)) + list(range(kw, kw + 128))
    cols += list(range(cv, cv + 1024))
    cols += list(range(vs, vs + 128)) + list(range(vw, vw + 128)) + list(range(gl, gl + 24))
    assert len(cols) == D_IN
    return np.asarray(cols)


def pack_flat(inp, layers):
    lay, total, nch = flat_layout(layers)
    flat = np.zeros(nch * CHUNK_ELEMS, np.float32)

    def put(key, arr):
        a = np.ascontiguousarray(arr, dtype=np.float32).reshape(-1)
        flat[lay[key]:lay[key] + a.size] = a

    perm = w_in_perm()
    for l in layers:
        put(("ada", l), inp["ada_w"][l])
        put(("w_in", l), np.asarray(inp["w_in"][l])[:, perm])
        put(("cmp_w1", l), inp["cmp_w1"][l])
        put(("w_out", l), inp["w_out"][l])
        if l % 2 == 0:
            put(("ffn1", l), inp["ffn_w1"][l // 2])
            put(("ffn2", l), inp["ffn_w2"][l // 2])
        else:
            for e in range(NE):
                put(("moe1", l, e), inp["moe_w1"][l // 2, e])
                put(("moe2", l, e), inp["moe_w2"][l // 2, e])
    return flat.reshape(nch, 8, 512, 1024), nch


def rel_bucket_np(dist):
    n = np.maximum(dist, 0)
    nf = np.maximum(n, 1).astype(np.float32)
    large = 16 + (np.log(nf / np.float32(16)) / np.float32(math.log(128 / 16)) * np.float32(16)).astype(np.int32)
    large = np.minimum(large, 31)
    return np.where(n < 16, n, large)


def host_consts():
    c = {}
    n = np.arange(NCMP)[:, None]
    q = np.arange(T)[None, :]
    dist = q - (16 * n + 31)
    c["bk_c"] = np.where(dist >= 0, rel_bucket_np(dist), 32).astype(np.float32)
    k = np.arange(128)[:, None]
    qq = np.arange(128)[None, :]
    d0 = qq - k
    c["bk_d0"] = np.where(d0 >= 0, rel_bucket_np(d0), 32).astype(np.float32)
    c["bk_d1"] = rel_bucket_np(128 + qq - k).astype(np.float32)
    c["edge"] = np.where(qq >= k, NEG, 0.0).astype(np.float32)
    j = np.arange(32)[:, None]
    kk = np.arange(T)[None, :]
    c["emat"] = (kk // 64 == j).astype(np.float32)
    tb = (np.arange(T) // 64)[:, None]
    jj = np.arange(32)[None, :]
    fm = np.where(jj > tb, -1e9, np.where((jj == 0) | (jj == tb) | (jj == tb - 1), 1e9, 0.0))
    c["fm"] = fm.astype(np.float32)
    starts = 16 * np.arange(NCMP)[:, None]
    bstart = 64 * np.arange(32)[None, :]
    c["ov"] = ((starts < bstart + 64) & (starts + 32 > bstart)).astype(np.float32)
    c["ident"] = np.eye(128, dtype=np.float32)
    return c
SB_BASE = 16512
SB_END = 229376


def build(cfg):
    NSEQ = cfg["nseq"]
    layers = cfg["layers"]
    gather = cfg["gather"]
    lay, total, NCH = flat_layout(layers)
    nc = bass.Bass("TRN2", target_bir_lowering=False)
    P = Prog(nc)
    NT = NSEQ * T

    def dram_in(name, shape, dt=F32):
        return nc.dram_tensor(name, list(shape), dt, kind="ExternalInput").ap()

    x_in = dram_in("x", [NT, D])
    c_in = dram_in("c", [NSEQ, D])
    if gather:
        wsh = dram_in("wsh", [NCH * 512, 1024])
    else:
        wfull = dram_in("wfull", [NCH * 4096, 1024])
    cmp_pos = dram_in("cmp_pos", [4, 2, 32, 64])
    cmp_w2 = dram_in("cmp_w2", [4, 2, 64, 64])
    conv_w = dram_in("conv_w", [4, 31, 512])
    conv_b = dram_in("conv_b", [4, 512])
    conv_g = dram_in("conv_ln_g", [4, 512])
    conv_be = dram_in("conv_ln_b", [4, 512])
    rel_bias = dram_in("rel_bias", [1, 256])
    ada_b = dram_in("ada_b", [4, 6144])
    ln_g = dram_in("ln_g", [4, 2, 1024])
    ln_b = dram_in("ln_b", [4, 2, 1024])
    router_w = dram_in("router_w", [2, 1024, 8])
    bk_c = dram_in("bk_c", [NCMP, T])
    bk_d0 = dram_in("bk_d0", [128, 128])
    bk_d1 = dram_in("bk_d1", [128, 128])
    edge_in = dram_in("edge", [128, 128])
    emat_in = dram_in("emat", [32, T])
    fm_in = dram_in("fm", [T, 32])
    ov_in = dram_in("ov", [NCMP, 32])
    ident_in = dram_in("ident", [128, 128])
    out = nc.dram_tensor("out", [NT, D], F32, kind="ExternalOutput").ap()

    NSEG = (NCH + SEG_CH - 1) // SEG_CH
    wflat_t = [nc.dram_tensor(f"wflat{i}", [min(SEG_CH, NCH - i * SEG_CH) * 4096, 1024], BF16).ap() for i in range(NSEG)]
    xa = nc.dram_tensor("xa", [NT, D], F32).ap()
    xb = nc.dram_tensor("xb", [NT, D], F32).ap()
    modrow = nc.dram_tensor("modrow", [len(layers) * NSEQ, 6144], F32).ap()
    bc_dram = nc.dram_tensor("bc_dram", [8 * 128, T], BF16).ap()
    b_xa, b_xb, b_out, b_modrow, b_bc = Buf("xa"), Buf("xb"), Buf("outd"), Buf("modrow"), Buf("bcd")

    b_wflat = Buf("wflat")
    b_cc = Buf("cc")
    chunk_ready = {}
    P.nobarrier.update(["cc", "ib0", "wflat"])
    if gather:
        ib = nc.dram_tensor("ib", [NCH * 512, 1024], BF16).ap()
        ob1 = nc.dram_tensor("ob1", [NCH * 2048, 1024], BF16).ap()
        NP_ = (NCH * 512 + 2047) // 2048
        b_ibp = [Buf(f"ib{i}") for i in range(NP_)]
        cast_done = set()

        def cast(pi):
            if pi in cast_done or pi >= NP_:
                return
            cast_done.add(pi)
            i = pi * 2048
            j = min(i + 2048, NCH * 512)
            P.dma("pool", ib[i:j, :], wsh[i:j, :], writes=[b_ibp[pi]], owner=b_ibp[0], max_dma_last_dim=4096)

        s1idx = {}

        def s1(ch):
            cast(ch // 4)
            fn = lambda eng, ch=ch: eng.collective_compute(
                "AllGather", ALU.bypass, replica_groups=[[0, 1, 2, 3], [4, 5, 6, 7]],
                ins=[ib[ch * 512:(ch + 1) * 512, :]], outs=[ob1[ch * 2048:(ch + 1) * 2048, :]])
            P.dma("pool", None, None, reads=[b_ibp[ch // 4]], writes=[], owner=b_cc, fn=fn, inc=1)
            s1idx[ch] = b_cc.semcnt

        def s2(ch, hf):
            fn = lambda eng, ch=ch, hf=hf: eng.collective_compute(
                "AllGather", ALU.bypass, replica_groups=[[0, 4], [1, 5], [2, 6], [3, 7]],
                ins=[ob1[ch * 2048 + hf * 1024:ch * 2048 + (hf + 1) * 1024, :]],
                outs=[wflat_t[ch // SEG_CH][(2 * (ch % SEG_CH) + hf) * 2048:(2 * (ch % SEG_CH) + hf + 1) * 2048, :]])
            P.dma("pool", None, None, reads=[], writes=[], owner=b_cc, fn=fn, inc=1,
                  extra=[("d", b_cc, s1idx[ch])])

        s1(0)
        if NCH > 1:
            s1(1)
        for ch in range(NCH):
            s2(ch, 0)
            s2(ch, 1)
            chunk_ready[ch] = b_cc.semcnt
            if ch + 2 < NCH:
                s1(ch + 2)
            cast((ch + 6) // 4)
    else:
        for i in range(0, NCH * 4096, 2048):
            sg, li_ = divmod(i, SEG_CH * 4096)
            P.dma("pool", wflat_t[sg][li_:li_ + 2048, :], wfull[i:i + 2048, :], pwrites=[b_wflat], max_dma_last_dim=4096)

    flat1 = [w_.rearrange("r c -> (r c)") for w_ in wflat_t]

    def wview(key, K, N, sub=0):
        sg, o = divmod(lay[key] + sub, SEG_ELEMS)
        return flat1[sg][o:o + K * N].rearrange("(k n) -> k n", n=N)

    def wdep(key, nelem):
        if gather:
            ch = (lay[key] + nelem - 1) // CHUNK_ELEMS
            return dict(extra=[("d", b_cc, chunk_ready[ch])])
        return dict(reads=[b_wflat])

    _cache = {}
    _bufs = {}

    def getbuf(name):
        if name not in _bufs:
            _bufs[name] = Buf(name)
        return _bufs[name]

    class Arena:
        def __init__(self, base, end):
            self.base, self.off, self.end = base, base, end

        def alloc(self, name, shape, dt):
            esz = 4 if dt == F32 else 2
            n = esz
            for s in shape[1:]:
                n *= s
            n = (n + 63) // 64 * 64
            assert self.off + n <= self.end, (name, self.off, n, self.end)
            if name in _cache:
                ap_, off_ = _cache[name]
                assert off_ == self.off, (name, off_, self.off)
            else:
                ap_ = nc.alloc_sbuf_tensor_at(name, list(shape), dt, offset=self.off).ap()
                _cache[name] = (ap_, self.off)
            self.off += n
            return ap_, getbuf(name)

    AP_ = Arena(SB_BASE, SB_END)
    al = AP_.alloc
    ident_f, b_identf = al("ident_f", [128, 128], F32)
    ident_b, b_identb = al("ident_b", [128, 128], BF16)
    onesm, b_onesm = al("onesm", [128, 128], F32)
    emat_b, b_emat = al("emat_b", [128, T], BF16)
    fm_sb, b_fm = al("fm_sb", [128, 16, 32], F32)
    edge_b, b_edge = al("edge_b", [128, 128], BF16)
    bt0, b_bt0 = al("bt0", [128, 8, 128], BF16)
    bt1, b_bt1 = al("bt1", [128, 8, 128], BF16)
    relb, b_relb = al("relb", [128, 32, 8], F32)
    relc, b_relc = al("relc", [128, 32, 8], F32)
    modcol, b_modcol = al("modcol", [128, NSEQ, 6, 8], F32)
    scT, b_scT = al("scT", [128, 8, NSEQ], BF16)
    vcmp, b_vcmp = al("vcmp", [128, 2, 97], BF16)
    convw, b_convw = al("convw", [128, 4, 31], F32)
    convp, b_convp = al("convp", [128, 3, 4], F32)
    w1sb, b_w1sb = al("w1sb", [128, 2, 32, 64], BF16)
    posT, b_posT = al("posT", [128, 2, 34], BF16)
    posTf, b_posTf = al("posTf", [128, 2, 32], F32)
    w2sb, b_w2sb = al("w2sb", [128, 2, 64], BF16)
    w2f, b_w2f = al("w2f", [128, 2, 64], F32)
    c1col, b_c1col = al("c1col", [128, 2], F32)
    rw_sb, b_rw = al("rw_sb", [128, 8, 8], F32)
    epsc, b_epsc = al("epsc", [128, 1], F32)
    stgT, b_stgT = al("stgT", [128, 128], F32)
    bcast = [al(f"bcast{i}", [128, 1024], F32) for i in range(3)]
    xt = [al(f"xt{i}", [128, 1024], F32) for i in range(2)]
    tt_ = [al(f"tt{i}", [128, 1024], F32) for i in range(2)]
    we_sb, b_we = al("we_sb", [128, 16, 8], F32)
    st_bn, b_stbn = al("st_bn", [128, 2, 6], F32)
    st_mv, b_stmv = al("st_mv", [128, 4], F32)
    sm8, b_sm8 = al("sm8", [128, 16], F32)
    PH = AP_.off

    banks = []
    for i in range(8):
        banks.append((nc.alloc_psum_tensor(f"bank{i}", [128, 512], F32).ap(), Buf(f"bank{i}", excl=True)))

    def A(k, f, reads=(), writes=(), pwrites=(), extra=()):
        P.op(k, f, reads=reads, writes=writes, pwrites=pwrites, extra=extra)

    def load_T(dst, b_dst, n, srcs, full=False):
        A("dve", lambda e: e.memset(stgT, 0.0), [], [b_stgT])
        for cs_, rs_, ap_ in srcs:
            P.dma("sp", stgT[rs_, cs_], ap_, pwrites=[b_stgT])
        n2 = (n + 1) // 2 * 2
        p4, bp4 = banks[4]
        A("pe", lambda e: e.transpose(out=p4[:, 0:n2], in_=stgT[0:n2, :], identity=ident_f[0:n2, 0:n2]), [b_stgT, b_identf], [bp4])
        if full:
            A("dve", lambda e: e.tensor_copy(out=dst, in_=p4[:, 0:n]), [bp4], [b_dst])
        else:
            A("dve", lambda e: e.tensor_copy(out=dst, in_=p4[:, 0:n]), [bp4], pwrites=[b_dst])

    MA = Arena(PH, SB_END)
    P.dma("sp", ident_f, ident_in, writes=[b_identf])
    A("dve", lambda e: e.tensor_copy(out=ident_b, in_=ident_f), [b_identf], [b_identb])
    A("dve", lambda e: e.memset(onesm, 1.0 / 512.0), [], [b_onesm])
    A("dve", lambda e: e.memset(epsc, LN_EPS), [], [b_epsc])
    stg, b_stg = MA.alloc("stg", [128, T], F32)
    P.dma("sp", stg[0:32, :], emat_in, writes=[b_stg])
    A("dve", lambda e: e.memset(emat_b, 0.0), [], [b_emat])
    A("dve", lambda e: e.tensor_copy(out=emat_b[0:32, :], in_=stg[0:32, :]), [b_stg], [b_emat])
    P.dma("sp", fm_sb, fm_in.rearrange("(t p) j -> p t j", p=128), writes=[b_fm])
    stg2, b_stg2 = MA.alloc("stg2", [128, 128], F32)
    P.dma("sp", stg2, edge_in, writes=[b_stg2])
    A("dve", lambda e: e.tensor_copy(out=edge_b, in_=stg2), [b_stg2], [b_edge])
    P.dma("sp", relb.rearrange("p b h -> p (b h)"), rel_bias.partition_broadcast(128).rearrange("p o n -> p (o n)"), writes=[b_relb])
    for b in range(32):
        A("dve", lambda e, b=b: e.tensor_tensor(out=relc[:, b, :], in0=relb[:, b, :], in1=relb[:, 31, :], op=ALU.subtract), [b_relb], [b_relc])
    stg3, b_stg3 = MA.alloc("stg3", [128, 32], F32)
    P.dma("sp", stg3[0:NCMP, :], ov_in, writes=[b_stg3])
    A("dve", lambda e: e.memset(vcmp, 1.0), [], [b_vcmp])
    for g in range(2):
        A("dve", lambda e, g=g: e.tensor_copy(out=vcmp[0:NCMP, g, 65:97], in_=stg3[0:NCMP, :]), [b_stg3], [b_vcmp])
    bkt, b_bkt = MA.alloc("bkt", [128, 2, 128], F32)
    P.dma("sp", bkt[:, 0, :], bk_d0, pwrites=[b_bkt])
    P.dma("sp", bkt[:, 1, :], bk_d1, pwrites=[b_bkt])
    acc01, b_acc01 = MA.alloc("acc01", [128, 2, 8, 128], F32)
    msk, b_msk = MA.alloc("msk", [128, 2, 128], F32)
    A("dve", lambda e: e.tensor_scalar(out=msk, in0=bkt, scalar1=32.0, scalar2=NEG, op0=ALU.is_equal, op1=ALU.mult), [b_bkt], [b_msk])
    for h in range(8):
        A("dve", lambda e, h=h: e.tensor_copy(out=acc01[:, :, h, :], in_=msk), [b_msk], [b_acc01])
    for b in range(31):
        A("dve", lambda e, b=b: e.tensor_scalar(out=msk, in0=bkt, scalar1=float(b), scalar2=None, op0=ALU.is_equal), [b_bkt], [b_msk])
        for h in range(8):
            A("dve", lambda e, b=b, h=h: e.scalar_tensor_tensor(out=acc01[:, :, h, :], in0=msk, scalar=relc[:, b, h:h + 1], in1=acc01[:, :, h, :],
                                                              op0=ALU.mult, op1=ALU.add), [b_msk, b_relc, b_acc01], [b_acc01])
    A("dve", lambda e: e.tensor_copy(out=bt0, in_=acc01[:, 0, :, :]), [b_acc01], [b_bt0])
    A("dve", lambda e: e.tensor_copy(out=bt1, in_=acc01[:, 1, :, :]), [b_acc01], [b_bt1])
    bkc, b_bkc = MA.alloc("bkc", [128, T], F32)
    P.dma("sp", bkc[0:NCMP, :], bk_c, writes=[b_bkc])
    mskc, b_mskc = MA.alloc("mskc", [128, T], F32)
    accc, b_accc = MA.alloc("accc", [128, 8, T], F32)
    A("dve", lambda e: e.tensor_scalar(out=mskc[0:NCMP], in0=bkc[0:NCMP], scalar1=32.0, scalar2=NEG, op0=ALU.is_equal, op1=ALU.mult), [b_bkc], [b_mskc])
    for h in range(8):
        A("dve", lambda e, h=h: e.tensor_copy(out=accc[0:NCMP, h, :], in_=mskc[0:NCMP]), [b_mskc], [b_accc])
    for b in range(31):
        A("dve", lambda e, b=b: e.tensor_scalar(out=mskc[0:NCMP], in0=bkc[0:NCMP], scalar1=float(b), scalar2=None, op0=ALU.is_equal), [b_bkc], [b_mskc])
        for h in range(8):
            A("dve", lambda e, b=b, h=h: e.scalar_tensor_tensor(out=accc[0:NCMP, h, :], in0=mskc[0:NCMP], scalar=relc[0:NCMP, b, h:h + 1], in1=accc[0:NCMP, h, :],
                                                              op0=ALU.mult, op1=ALU.add), [b_mskc, b_relc, b_accc], [b_accc])
    bcb, b_bcb = MA.alloc("bcb", [128, 8, T], BF16)
    A("act", lambda e: e.activation(out=bcb[0:NCMP], in_=accc[0:NCMP], func=AF.Copy), [b_accc], [b_bcb])
    for h in range(8):
        P.dma("sp", bc_dram[h * 128:h * 128 + NCMP, :], bcb[0:NCMP, h, :], reads=[b_bcb], pwrites=[b_bc], owner=b_bcb)
    csb, b_csb = MA.alloc("csb", [128, D], F32)
    A("dve", lambda e: e.memset(csb[0:32, :], 0.0), [], [b_csb])
    P.dma("sp", csb[0:NSEQ, :], c_in, writes=[b_csb])
    A("act", lambda e: e.activation(out=csb[0:32, :], in_=csb[0:32, :], func=AF.Silu), [b_csb], [b_csb])
    pb, b_pb = banks[4]
    for kc in range(8):
        A("pe", lambda e, kc=kc: e.transpose(out=pb[:, kc * 32:(kc + 1) * 32], in_=csb[0:32, kc * 128:(kc + 1) * 128], identity=ident_f[0:32, 0:32]),
          [b_csb, b_identf], [b_pb] if kc == 0 else (), pwrites=() if kc == 0 else [b_pb])
    A("dve", lambda e: e.tensor_copy(out=scT, in_=pb[:, 0:256].rearrange("p (k s) -> p k s", s=32)[:, :, 0:NSEQ]), [b_pb], [b_scT])
    P.barrier()
    def rowb(dst, b_dst, src_row, reads=()):
        P.dma("sp", dst, src_row.partition_broadcast(128).rearrange("p o n -> p (o n)"), reads=list(reads), writes=[b_dst])

    def layer_norm_tile(tt, b_tt, lng, b_lng, lnb, b_lnb):
        for hf in range(2):
            A("dve", lambda e, hf=hf: e.bn_stats(out=st_bn[:, hf, :], in_=tt[:, hf * 512:(hf + 1) * 512]), [b_tt], pwrites=[b_stbn])
        A("dve", lambda e: e.bn_aggr(out=st_mv[:, 0:2], in_=st_bn.rearrange("p a b -> p (a b)")), [b_stbn], [b_stmv])
        A("act", lambda e: e.activation(out=st_mv[:, 2:3], in_=st_mv[:, 1:2], func=AF.Sqrt, bias=epsc[:, 0:1]), [b_stmv, b_epsc], [b_stmv])
        A("dve", lambda e: e.reciprocal(out=st_mv[:, 3:4], in_=st_mv[:, 2:3]), [b_stmv], [b_stmv])
        A("dve", lambda e: e.tensor_scalar(out=tt, in0=tt, scalar1=st_mv[:, 0:1], scalar2=st_mv[:, 3:4], op0=ALU.subtract, op1=ALU.mult), [b_tt, b_stmv], [b_tt])
        A("dve", lambda e: e.tensor_tensor(out=tt, in0=tt, in1=lng, op=ALU.mult), [b_tt, b_lng], [b_tt])
        A("dve", lambda e: e.tensor_tensor(out=tt, in0=tt, in1=lnb, op=ALU.add), [b_tt, b_lnb], [b_tt])

    NS = 0.125

    def ada_phase(li, l):
        R = Arena(PH, SB_END)
        adab, b_adab = R.alloc("adab", [128, 6144], F32)
        mrow, b_mrow = R.alloc("mrow", [128, 6144], F32)
        wp = [R.alloc(f"adaw{i}", [128, 8, 512], BF16) for i in range(2)]
        P.dma("sp", adab[0:NSEQ, :], ada_b[l:l + 1, :].partition_broadcast(NSEQ).rearrange("p o n -> p (o n)"), writes=[b_adab])
        wv = wview(("ada", l), D, 6144)
        for j in range(12):
            t, bt = wp[j % 2]
            P.dma("sp", t, wv[:, j * 512:(j + 1) * 512].rearrange("(kc p) n -> p kc n", p=128), writes=[bt], **wdep(("ada", l), D * 6144))
            ps, bps = banks[j % 2]
            for kc in range(8):
                A("pe", lambda e, kc=kc, t=t, ps=ps: e.matmul(ps[0:NSEQ, :], lhsT=scT[:, kc, :], rhs=t[:, kc, :], start=(kc == 0), stop=(kc == 7)),
                  [b_scT, bt], [bps] if kc == 0 else (), pwrites=() if kc == 0 else [bps])
            A("dve", lambda e, j=j, ps=ps: e.tensor_tensor(out=mrow[0:NSEQ, j * 512:(j + 1) * 512], in0=ps[0:NSEQ, :], in1=adab[0:NSEQ, j * 512:(j + 1) * 512], op=ALU.add),
              [bps, b_adab], pwrites=[b_mrow])
        for seg in (1, 2, 4, 5):
            A("dve", lambda e, seg=seg: e.tensor_scalar(out=mrow[0:NSEQ, seg * 1024:(seg + 1) * 1024], in0=mrow[0:NSEQ, seg * 1024:(seg + 1) * 1024],
                                                        scalar1=1.0, scalar2=None, op0=ALU.add), [b_mrow], [b_mrow])
        P.dma("sp", modrow[li * NSEQ:(li + 1) * NSEQ, :], mrow[0:NSEQ, :], reads=[b_mrow], writes=[b_modrow], owner=b_mrow)
        P.barrier()
        for s in range(NSEQ):
            load_T(modcol[:, s, :, :].rearrange("p a b -> p (a b)"), b_modcol, 48,
                   [(slice(0, 128), slice(0, 48), modrow[li * NSEQ + s, :].rearrange("(r p) -> r p", p=128))])

    def mixer_phase(li, l, s, xsrc, b_xsrc, xdst, b_xdst):
        R1 = Arena(PH, SB_END)
        w_in_sb, b_win = R1.alloc("w_in_sb", [128, 8, D_IN], BF16)
        hT = [R1.alloc(f"hT{i}", [128, 8, 512], BF16) for i in range(2)]
        KcT, b_KcT = R1.alloc("KcT", [128, T], BF16)
        VcT, b_VcT = R1.alloc("VcT", [128, T], BF16)
        sig, b_sig = R1.alloc("sig", [128, 512], F32)
        ov_end = R1.off
        w_out_sb, b_wout = R1.alloc("w_out_sb", [128, 8, D], BF16)
        QT, b_QT = R1.alloc("QT", [128, 4, T], BF16)
        KsP = [R1.alloc(f"KsP{g}", [128, T], BF16) for g in range(2)]
        KwP = [R1.alloc(f"KwP{g}", [128, T], BF16) for g in range(2)]
        Vs_aug, b_Vs = R1.alloc("Vs_aug", [128, 16, 2, 65], BF16)
        Vw_aug, b_Vw = R1.alloc("Vw_aug", [128, 16, 2, 65], BF16)
        gsb, b_gsb = R1.alloc("gsb", [128, 16, 24], F32)
        yT, b_yT = R1.alloc("yT", [128, 4, 30 + T], BF16)
        convacc, b_cacc = R1.alloc("convacc", [128, 4, 512], F32)
        negselT, b_nsT = R1.alloc("negselT", [128, 2, 512], BF16)
        KcmpT, b_KcmpT = R1.alloc("KcmpT", [128, 2, 128], BF16)
        Gt, b_G = R1.alloc("Gt", [128, 128], BF16)
        dg = [R1.alloc(f"dg{i}", [128, 8, 128], BF16) for i in range(2)]
        R2 = Arena(PH, ov_end)
        o_nsaT, b_onT = R2.alloc("o_nsaT", [128, 4, T], BF16)
        o_convT, b_ocT = R2.alloc("o_convT", [128, 4, T], BF16)
        o_nsa, b_on = R2.alloc("o_nsa", [128, 4, 512], F32)
        Osb = [R2.alloc(f"Osb{i}", [128, 512], F32) for i in range(2)]
        PTb = [R2.alloc(f"PT{i}", [128, 512], BF16) for i in range(3)]
        Bcs = [R2.alloc(f"Bcs{i}", [128, 512], BF16) for i in range(2)]
        convsq = [R2.alloc(f"convsq{i}", [128, 512], F32) for i in range(2)]
        s_mean, b_smean = R2.alloc("s_mean", [128, 512], F32)
        s_var, b_svar = R2.alloc("s_var", [128, 512], F32)
        s_rstd, b_srstd = R2.alloc("s_rstd", [128, 512], F32)
        impacc, b_imp = R2.alloc("impacc", [128, 4, 2, 32], F32)
        score, b_score = R2.alloc("score", [128, 32], F32)
        negsel, b_negsel = R2.alloc("negsel", [128, 32], F32)
        rz, b_rz = R2.alloc("rz", [128, 4], F32)
        coef, b_coef = R2.alloc("coef", [128, 4], F32)
        print("mixer sbuf: R1 end", R1.off, "R2 end", R2.off, "ov_end", ov_end, "limit", SB_END)

        wv = wview(("w_in", l), D, D_IN)
        for kc in range(8):
            P.dma("sp", w_in_sb[:, kc, :], wv[kc * 128:(kc + 1) * 128, :], pwrites=[b_win], **wdep(("w_in", l), D * D_IN))
        wo = wview(("w_out", l), D, D)
        P.dma("sp", w_out_sb, wo.rearrange("(kc p) n -> p kc n", p=128), writes=[b_wout], **wdep(("w_out", l), D * D))
        cw = wview(("cmp_w1", l), 2 * 2048, 64).rearrange("(kv l d) o -> d kv l o", kv=2, l=32)
        for g in range(2):
            for kv in range(2):
                P.dma("sp", w1sb[g * 64:(g + 1) * 64, kv, :, :], cw[:, kv, :, :], pwrites=[b_w1sb], **wdep(("cmp_w1", l), 2 * 2048 * 64))
            P.dma("sp", w2f[g * 64:(g + 1) * 64, :, :], cmp_w2[l].rearrange("kv o p -> o kv p"), pwrites=[b_w2f])
        A("dve", lambda e: e.memset(posT, 0.0), [], [b_posT])
        for kv in range(2):
            load_T(posTf[:, kv, :], b_posTf, 32, [(slice(0, 64), slice(0, 32), cmp_pos[l, kv]), (slice(64, 128), slice(0, 32), cmp_pos[l, kv])])
        A("dve", lambda e: e.tensor_copy(out=posT[:, :, 0:32], in_=posTf), [b_posTf], [b_posT])
        A("dve", lambda e: e.tensor_copy(out=w2sb, in_=w2f), [b_w2f], [b_w2sb])
        for c4 in range(4):
            load_T(convw[:, c4, :], b_convw, 31, [(slice(0, 128), slice(0, 31), conv_w[l][:, c4 * 128:(c4 + 1) * 128])])
        load_T(convp.rearrange("p a b -> p (a b)"), b_convp, 12,
               [(slice(0, 128), slice(4 * i, 4 * i + 4), src[l, :].rearrange("(c p) -> c p", p=128)) for i, src in enumerate((conv_b, conv_g, conv_be))], full=True)
        A("dve", lambda e: e.memset(Vs_aug, 1.0), [], [b_Vs])
        A("dve", lambda e: e.memset(Vw_aug, 1.0), [], [b_Vw])
        A("dve", lambda e: e.memset(yT[:, :, 0:30], 0.0), [], [b_yT])
        for g in range(2):
            A("dve", lambda e, g=g: e.memset(KsP[g][0], 0.0), [], [KsP[g][1]])
            A("dve", lambda e, g=g: e.memset(KwP[g][0], 0.0), [], [KwP[g][1]])
        A("dve", lambda e: e.memset(KcmpT, 0.0), [], [b_KcmpT])
        A("dve", lambda e: e.memset(negselT, 0.0), [], [b_nsT])

        if cfg.get("stop") == "loads":
            P.barrier()
            return
        def p1_T(tc):
            hTt, b_hTt = hT[tc % 2]
            cs = slice(tc * 512, (tc + 1) * 512)
            for t4 in range(4):
                tile = tc * 4 + t4
                xtile, b_x = xt[tile % 2]
                r0 = s * T + tile * 128
                P.dma("sp", xtile, xsrc[r0:r0 + 128, :], reads=[b_xsrc], writes=[b_x])
                for half in range(2):
                    ps, bps = banks[half]
                    for k4 in range(4):
                        kc = half * 4 + k4
                        A("pe", lambda e, ps=ps, k4=k4, kc=kc, xtile=xtile: e.transpose(out=ps[:, k4 * 128:(k4 + 1) * 128], in_=xtile[:, kc * 128:(kc + 1) * 128], identity=ident_f),
                          [b_x, b_identf], [bps] if k4 == 0 else (), pwrites=() if k4 == 0 else [bps])
                    for k4 in range(4):
                        kc = half * 4 + k4
                        o_ap = hTt[:, kc, t4 * 128:(t4 + 1) * 128]
                        i_ap = ps[:, k4 * 128:(k4 + 1) * 128]
                        if half == 0:
                            A("dve", lambda e, o_ap=o_ap, i_ap=i_ap, kc=kc: e.tensor_scalar(out=o_ap, in0=i_ap, scalar1=modcol[:, s, 1, kc:kc + 1], scalar2=modcol[:, s, 0, kc:kc + 1],
                                                                                         op0=ALU.mult, op1=ALU.add), [bps, b_modcol], pwrites=[b_hTt])
                        else:
                            A("act", lambda e, o_ap=o_ap, i_ap=i_ap, kc=kc: e.activation(out=o_ap, in_=i_ap, func=AF.Identity, scale=modcol[:, s, 1, kc:kc + 1], bias=modcol[:, s, 0, kc:kc + 1]),
                              [bps, b_modcol], pwrites=[b_hTt])


        def p1_P(tc):
            hTt, b_hTt = hT[tc % 2]
            cs = slice(tc * 512, (tc + 1) * 512)
            def proj(fo, ps, bps):
                for kc in range(8):
                    A("pe", lambda e, kc=kc: e.matmul(ps, lhsT=w_in_sb[:, kc, fo * 128:(fo + 1) * 128], rhs=hTt[:, kc, :], start=(kc == 0), stop=(kc == 7)),
                      [b_win, b_hTt], [bps] if kc == 0 else (), pwrites=() if kc == 0 else [bps])

            for fo in range(8):
                ps, bps = banks[2 + fo % 2]
                proj(fo, ps, bps)
                if fo < 4:
                    A("act", lambda e, fo=fo, ps=ps: e.activation(out=QT[:, fo, cs], in_=ps, func=AF.Copy, scale=NS), [bps], pwrites=[b_QT])
                elif fo < 6:
                    dst, bd = ((KcT, b_KcT), (VcT, b_VcT))[fo - 4]
                    A("dve", lambda e, dst=dst, ps=ps: e.tensor_copy(out=dst[:, cs], in_=ps), [bps], pwrites=[bd])
                else:
                    KP = KsP if fo == 6 else KwP
                    for g in range(2):
                        A("dve", lambda e, g=g, KP=KP, ps=ps: e.tensor_copy(out=KP[g][0][g * 64:(g + 1) * 64, cs], in_=ps[g * 64:(g + 1) * 64, :]), [bps], pwrites=[KP[g][1]])
            for c in range(4):
                pa, bpa = banks[2 if c % 2 == 0 else 5]
                pg, bpg = banks[3 if c % 2 == 0 else 6]
                proj(8 + c, pa, bpa)
                proj(12 + c, pg, bpg)
                A("act", lambda e, pg=pg: e.activation(out=sig, in_=pg, func=AF.Sigmoid), [bpg], [b_sig])
                A("dve", lambda e, c=c, pa=pa: e.tensor_tensor(out=yT[:, c, 30 + tc * 512:30 + (tc + 1) * 512], in0=pa, in1=sig, op=ALU.mult), [bpa, b_sig], pwrites=[b_yT])
            for t4 in range(4):
                tile = tc * 4 + t4
                ps, bps = banks[7]
                for kc in range(8):
                    A("pe", lambda e, kc=kc, t4=t4, ps=ps: e.matmul(ps[:, 0:280], lhsT=hTt[:, kc, t4 * 128:(t4 + 1) * 128], rhs=w_in_sb[:, kc, 2048:2328], start=(kc == 0), stop=(kc == 7)),
                      [b_win, b_hTt], [bps] if kc == 0 else (), pwrites=() if kc == 0 else [bps])
                A("dve", lambda e, tile=tile, ps=ps: e.tensor_copy(out=Vs_aug[:, tile, :, 0:64], in_=ps[:, 0:128].rearrange("p (g d) -> p g d", g=2)), [bps], pwrites=[b_Vs])
                A("dve", lambda e, tile=tile, ps=ps: e.tensor_copy(out=Vw_aug[:, tile, :, 0:64], in_=ps[:, 128:256].rearrange("p (g d) -> p g d", g=2)), [bps], pwrites=[b_Vw])
                A("act", lambda e, tile=tile, ps=ps: e.activation(out=gsb[:, tile, :], in_=ps[:, 256:280], func=AF.Sigmoid), [bps], pwrites=[b_gsb])


        p1_T(0)
        for tc in range(4):
            if tc + 1 < 4:
                p1_T(tc + 1)
            p1_P(tc)
        if cfg.get("stop") == "p1":
            P.barrier()
            return
        for kv, (src, b_src) in enumerate(((KcT, b_KcT), (VcT, b_VcT))):
            for g in range(2):
                pbs = slice(g * 64, g * 64 + 64)
                psm, bpsm = banks[0 + 2 * g]
                psc, bpsc = banks[1 + 2 * g]
                for l_ in range(32):
                    A("pe", lambda e, l_=l_, pbs=pbs, psc=psc: e.matmul(psc[pbs, 0:2], lhsT=w1sb[pbs, kv, l_, :], rhs=posT[pbs, kv, l_:l_ + 2], start=(l_ == 0), stop=(l_ == 31)),
                      [b_w1sb, b_posT], [bpsc] if l_ == 0 else (), pwrites=() if l_ == 0 else [bpsc])
                for l_ in range(32):
                    A("pe", lambda e, l_=l_, pbs=pbs, psm=psm: e.matmul(psm[pbs, 0:NCMP], lhsT=w1sb[pbs, kv, l_, :], rhs=src[pbs, l_:l_ + 16 * 126 + 1:16], start=(l_ == 0), stop=(l_ == 31)),
                      [b_w1sb, b_src], [bpsm] if l_ == 0 else (), pwrites=() if l_ == 0 else [bpsm])
                A("dve", lambda e, pbs=pbs, psc=psc: e.tensor_copy(out=c1col[pbs, kv:kv + 1], in_=psc[pbs, 0:1]), [bpsc], pwrites=[b_c1col])
                A("act", lambda e, pbs=pbs, psm=psm: e.activation(out=Gt[pbs, 0:NCMP], in_=psm[pbs, 0:NCMP], func=AF.Gelu_apprx_tanh, bias=c1col[pbs, kv:kv + 1]), [bpsm, b_c1col], pwrites=[b_G])
            if kv == 0:
                for g in range(2):
                    pbs = slice(g * 64, g * 64 + 64)
                    ps2, bps2 = banks[4 + g]
                    A("pe", lambda e, pbs=pbs, ps2=ps2: e.matmul(ps2[pbs, 0:NCMP], lhsT=w2sb[pbs, 0, :], rhs=Gt[pbs, 0:NCMP], start=True, stop=True), [b_w2sb, b_G], [bps2])
                    A("dve", lambda e, pbs=pbs, ps2=ps2, g=g: e.tensor_copy(out=KcmpT[pbs, g, 0:NCMP], in_=ps2[pbs, 0:NCMP]), [bps2], pwrites=[b_KcmpT])
            else:
                for g in range(2):
                    pbs = slice(g * 64, g * 64 + 64)
                    ps3, bps3 = banks[6 + g]
                    A("pe", lambda e, pbs=pbs, ps3=ps3: e.matmul(ps3[0:NCMP, 0:64], lhsT=Gt[pbs, 0:NCMP], rhs=w2sb[pbs, 1, :], start=True, stop=True), [b_w2sb, b_G], [bps3])
                    A("dve", lambda e, g=g, ps3=ps3: e.tensor_copy(out=vcmp[0:NCMP, g, 0:64], in_=ps3[0:NCMP, 0:64]), [bps3], pwrites=[b_vcmp])
        P.barrier()
        if cfg.get("stop") == "p2":
            return

        def conv_ops(qc):
            ops = []
            base = qc * 512
            pc, bpc = banks[6]
            batches = [(0, 8), (8, 16), (16, 24), (24, 31)]

            def build(c, bi):
                j0, j1 = batches[bi]
                dgt, b_dg = dg[(c * 4 + bi) % 2]
                for j in range(j0, j1):
                    A("dve", lambda e: e.tensor_scalar(out=dgt[:, j - j0, :], in0=ident_b, scalar1=convw[:, c, j:j + 1], scalar2=None, op0=ALU.mult),
                      [b_identb, b_convw], [b_dg] if j == j0 else (), pwrites=() if j == j0 else [b_dg])

            def mm(c, bi):
                j0, j1 = batches[bi]
                dgt, b_dg = dg[(c * 4 + bi) % 2]
                for j in range(j0, j1):
                    A("pe", lambda e: e.matmul(pc, lhsT=dgt[:, j - j0, :], rhs=yT[:, c, base + j:base + j + 512], start=(j == 0), stop=(j == 30)),
                      [b_dg, b_yT], [bpc] if j == 0 else (), pwrites=() if j == 0 else [bpc])

            def evac(c):
                A("act", lambda e: e.activation(out=convacc[:, c, :], in_=pc, func=AF.Identity, bias=convp[:, 0, c:c + 1]), [bpc, b_convp], [b_cacc] if c == 0 else (), pwrites=() if c == 0 else [b_cacc])

            for c in range(4):
                ops.append(lambda c=c: build(c, 0))
                ops.append(lambda c=c: build(c, 1))
                ops.append(lambda c=c: mm(c, 0))
                ops.append(lambda c=c: build(c, 2))
                ops.append(lambda c=c: mm(c, 1))
                ops.append(lambda c=c: build(c, 3))
                ops.append(lambda c=c: mm(c, 2))
                ops.append(lambda c=c: (mm(c, 3), evac(c)))

            def stats():
                pm, bpm = banks[7]
                pe_, bpe = banks[6]
                for c in range(4):
                    sq, bsq = convsq[c % 2]
                    A("act", lambda e, c=c, sq=sq: e.activation(out=sq, in_=convacc[:, c, :], func=AF.Square), [b_cacc], [bsq])
                    A("pe", lambda e, c=c: e.matmul(pm, lhsT=onesm, rhs=convacc[:, c, :], start=(c == 0), stop=(c == 3)), [b_onesm, b_cacc], [bpm] if c == 0 else (), pwrites=() if c == 0 else [bpm])
                    A("pe", lambda e, c=c, sq=sq: e.matmul(pe_, lhsT=onesm, rhs=sq, start=(c == 0), stop=(c == 3)), [b_onesm, bsq], [bpe] if c == 0 else (), pwrites=() if c == 0 else [bpe])
                A("act", lambda e: e.activation(out=s_mean, in_=pm, func=AF.Copy), [bpm], [b_smean])
                A("dve", lambda e: e.tensor_tensor(out=s_var, in0=s_mean, in1=s_mean, op=ALU.mult), [b_smean], [b_svar])
                A("dve", lambda e: e.tensor_tensor(out=s_var, in0=pe_, in1=s_var, op=ALU.subtract), [bpe, b_svar], [b_svar])
                A("act", lambda e: e.activation(out=s_rstd, in_=s_var, func=AF.Sqrt, bias=epsc[:, 0:1]), [b_svar, b_epsc], [b_srstd])
                A("dve", lambda e: e.reciprocal(out=s_rstd, in_=s_rstd), [b_srstd], [b_srstd])
                for c in range(4):
                    A("dve", lambda e, c=c: e.tensor_tensor(out=convacc[:, c, :], in0=convacc[:, c, :], in1=s_mean, op=ALU.subtract), [b_cacc, b_smean], pwrites=[b_cacc])
                    A("dve", lambda e, c=c: e.tensor_tensor(out=convacc[:, c, :], in0=convacc[:, c, :], in1=s_rstd, op=ALU.mult), [b_cacc, b_srstd], pwrites=[b_cacc])
                    A("act", lambda e, c=c: e.activation(out=o_convT[:, c, base:base + 512], in_=convacc[:, c, :], func=AF.Silu, scale=convp[:, 1, c:c + 1], bias=convp[:, 2, c:c + 1]),
                      [b_cacc, b_convp], pwrites=[b_ocT])
            ops.append(stats)
            return ops

        cnt = {"o": 0, "s": 0}

        def finish_branch(h, br, W, qc, psO, bpsO):
            osb, b_osb = Osb[cnt["o"] % 2]
            cnt["o"] += 1
            W2 = W + 1
            A("act", lambda e: e.activation(out=osb[0:W, :], in_=psO[0:W, :], func=AF.Copy), [bpsO], [b_osb])
            pT, bpT = banks[4]
            for qt in range(4):
                A("pe", lambda e, qt=qt: e.transpose(out=pT[:, qt * 128:qt * 128 + W2], in_=osb[0:W2, qt * 128:(qt + 1) * 128], identity=ident_f[0:W2, 0:W2]),
                  [b_osb, b_identf], [bpT] if qt == 0 else (), pwrites=() if qt == 0 else [bpT])
            pT3 = pT.rearrange("p (q w) -> p q w", w=128)
            A("dve", lambda e: e.tensor_scalar(out=rz, in0=pT3[:, :, 64], scalar1=1e-30, scalar2=None, op0=ALU.max), [bpT], [b_rz])
            A("dve", lambda e: e.reciprocal(out=rz, in_=rz), [b_rz], [b_rz])
            A("dve", lambda e: e.tensor_tensor(out=coef, in0=rz, in1=gsb[:, qc * 4:(qc + 1) * 4, h * 3 + br], op=ALU.mult), [b_rz, b_gsb], [b_coef])
            for qt in range(4):
                o_ap = o_nsa[:, qt, h * 64:(h + 1) * 64]
                if br == 0:
                    A("dve", lambda e, qt=qt, o_ap=o_ap: e.tensor_scalar(out=o_ap, in0=pT3[:, qt, 0:64], scalar1=coef[:, qt:qt + 1], scalar2=None, op0=ALU.mult), [bpT, b_coef], pwrites=[b_on])
                else:
                    A("dve", lambda e, qt=qt, o_ap=o_ap: e.scalar_tensor_tensor(out=o_ap, in0=pT3[:, qt, 0:64], scalar=coef[:, qt:qt + 1], in1=o_ap, op0=ALU.mult, op1=ALU.add),
                      [bpT, b_coef], pwrites=[b_on])
                if br == 0:
                    g = h // 4
                    i_ap = impacc[:, qt, g, :]
                    if h % 4 == 0:
                        A("dve", lambda e, qt=qt, i_ap=i_ap: e.tensor_scalar(out=i_ap, in0=pT3[:, qt, 65:97], scalar1=rz[:, qt:qt + 1], scalar2=None, op0=ALU.mult), [bpT, b_rz], pwrites=[b_imp])
                    else:
                        A("dve", lambda e, qt=qt, i_ap=i_ap: e.scalar_tensor_tensor(out=i_ap, in0=pT3[:, qt, 65:97], scalar=rz[:, qt:qt + 1], in1=i_ap, op0=ALU.mult, op1=ALU.add),
                          [bpT, b_rz], pwrites=[b_imp])

        for qc in range(4):
            qs = slice(qc * 512, (qc + 1) * 512)
            cops = conv_ops(qc)
            per = (len(cops) + 23) // 24

            def drain(n):
                for _ in range(n):
                    if cops:
                        cops.pop(0)()

            if cfg.get("stop") == "att" and qc == 1:
                return
            pend = []

            def cmp_a(h):
                fo, g = h % 4, h // 4
                bcs, b_bcs = Bcs[h % 2]
                P.dma("sp", bcs[0:NCMP, :], bc_dram[h * 128:h * 128 + NCMP, qs], reads=[b_bc], writes=[b_bcs])
                psS, bpsS = banks[h % 2]
                A("pe", lambda e: e.matmul(psS[0:NCMP, :], lhsT=KcmpT[:, g, 0:NCMP], rhs=QT[:, fo, qs], start=True, stop=False, skip_group_check=True), [b_KcmpT, b_QT], [bpsS])
                A("pe", lambda e: e.matmul(psS[0:NCMP, :], lhsT=ident_b[0:NCMP, 0:NCMP], rhs=bcs[0:NCMP, :], start=False, stop=True, skip_group_check=True), [b_identb, b_bcs], pwrites=[bpsS])
                pt, b_pt = PTb[h % 2]
                A("act", lambda e: e.activation(out=pt[0:NCMP, :], in_=psS[0:NCMP, :], func=AF.Exp, bias=relb[0:NCMP, 31, h:h + 1]), [bpsS, b_relb], [b_pt])

            def cmp_b(h):
                g = h // 4
                pt, b_pt = PTb[h % 2]
                psO, bpsO = banks[2 + h % 2]
                A("pe", lambda e: e.matmul(psO[0:97, :], lhsT=vcmp[0:NCMP, g, :], rhs=pt[0:NCMP, :], start=True, stop=True), [b_vcmp, b_pt], [bpsO])
                pend.append(lambda: finish_branch(h, 0, 97, qc, psO, bpsO))

            cmp_a(0)
            for h in range(8):
                if h + 1 < 8:
                    cmp_a(h + 1)
                cmp_b(h)
                if len(pend) > 1:
                    pend.pop(0)()
                drain(per)
            while pend:
                pend.pop(0)()
            for g in range(2):
                p5, bp5 = banks[5]
                for qt in range(4):
                    A("dve", lambda e, qt=qt, g=g: e.tensor_tensor(out=score, in0=impacc[:, qt, g, :], in1=fm_sb[:, qc * 4 + qt, :], op=ALU.add), [b_imp, b_fm], [b_score])
                    A("dve", lambda e: e.max(out=sm8[:, 0:8], in_=score), [b_score], [b_sm8])
                    A("dve", lambda e: e.tensor_scalar(out=negsel, in0=score, scalar1=sm8[:, 7:8], scalar2=NEG, op0=ALU.is_lt, op1=ALU.mult), [b_score, b_sm8], [b_negsel])
                    A("pe", lambda e, qt=qt: e.transpose(out=p5[0:32, qt * 128:(qt + 1) * 128], in_=negsel, identity=ident_f), [b_negsel, b_identf], [bp5] if qt == 0 else (), pwrites=() if qt == 0 else [bp5])
                A("act", lambda e, g=g: e.activation(out=negselT[0:32, g, :], in_=p5[0:32, :], func=AF.Copy), [bp5], pwrites=[b_nsT])
            pend = []
            for br in (1, 2):
                for h in range(8):
                    fo, g = h % 4, h // 4
                    kts = list(range(0, 4 * qc + 4)) if br == 1 else list(range(max(0, 4 * qc - 4), 4 * qc + 4))
                    KT, b_KT = KsP[g] if br == 1 else KwP[g]
                    Va, b_Va = (Vs_aug, b_Vs) if br == 1 else (Vw_aug, b_Vw)
                    psO, bpsO = banks[2 + cnt["o2"] % 2] if "o2" in cnt else banks[2]
                    cnt["o2"] = cnt.get("o2", 0) + 1
                    steps = []

                    def qk(i):
                        kt = kts[i]
                        sidx = cnt["s"]
                        cnt["s"] += 1
                        psS, bpsS = banks[(0, 1, 5)[sidx % 3]]
                        pt, b_pt = PTb[sidx % 3]
                        qlo = max(kt, 4 * qc)
                        qhi = 4 * qc + 3 if br == 1 else min(kt + 4, 4 * qc + 3)
                        c0, c1 = (qlo - 4 * qc) * 128, (qhi - 4 * qc + 1) * 128
                        mm = [(psS[:, c0:c1], KT[:, kt * 128:(kt + 1) * 128], QT[:, fo, qc * 512 + c0:qc * 512 + c1], [b_KT, b_QT])]
                        if br == 1:
                            mm.append((psS[:, c0:c1], emat_b[:, kt * 128:(kt + 1) * 128], negselT[:, g, c0:c1], [b_emat, b_nsT]))
                        if kt >= 4 * qc:
                            j = kt - 4 * qc
                            mm.append((psS[:, j * 128:(j + 1) * 128], ident_b, bt0[:, h, :], [b_identb, b_bt0]))
                        j = kt + 1 - 4 * qc
                        if 0 <= j <= 3:
                            mm.append((psS[:, j * 128:(j + 1) * 128], ident_b, bt1[:, h, :], [b_identb, b_bt1]))
                        j = kt + 4 - 4 * qc
                        if br == 2 and 0 <= j <= 3:
                            mm.append((psS[:, j * 128:(j + 1) * 128], ident_b, edge_b, [b_identb, b_edge]))
                        n = len(mm)
                        for mi, (o_, l_, r_, rd) in enumerate(mm):
                            A("pe", lambda e: e.matmul(o_, lhsT=l_, rhs=r_, start=(mi == 0), stop=(mi == n - 1), skip_group_check=True),
                              rd, [bpsS] if mi == 0 else (), pwrites=() if mi == 0 else [bpsS])
                        A("act", lambda e: e.activation(out=pt[:, c0:c1], in_=psS[:, c0:c1], func=AF.Exp, bias=relb[:, 31, h:h + 1]), [bpsS, b_relb], [b_pt])
                        steps.append((kt, c0, c1, pt, b_pt))

                    def pv(i):
                        kt, c0, c1, pt, b_pt = steps[i]
                        n = len(kts)
                        A("pe", lambda e: e.matmul(psO[0:65, c0:c1], lhsT=Va[:, kt, g, :], rhs=pt[:, c0:c1], start=(i == 0), stop=(i == n - 1), skip_group_check=True),
                          [b_Va, b_pt], [bpsO] if i == 0 else (), pwrites=() if i == 0 else [bpsO])

                    qk(0)
                    if len(kts) > 1:
                        qk(1)
                    for i in range(len(kts)):
                        if i + 2 < len(kts):
                            qk(i + 2)
                        pv(i)
                        if i == min(1, len(kts) - 1) and pend:
                            pend.pop(0)()
                    pend.append(lambda h=h, br=br, psO=psO, bpsO=bpsO: finish_branch(h, br, 65, qc, psO, bpsO))
                    drain(per)
            while pend:
                pend.pop(0)()
            drain(len(cops))
            for qt in range(4):
                p5, bp5 = banks[5]
                for fc in range(4):
                    A("pe", lambda e, qt=qt, fc=fc: e.transpose(out=p5[:, fc * 128:(fc + 1) * 128], in_=o_nsa[:, qt, fc * 128:(fc + 1) * 128], identity=ident_f), [b_on, b_identf],
                      [bp5] if fc == 0 else (), pwrites=() if fc == 0 else [bp5])
                A("act", lambda e, qt=qt: e.activation(out=o_nsaT[:, :, qc * 512 + qt * 128:qc * 512 + (qt + 1) * 128], in_=p5.rearrange("p (f q) -> p f q", f=4), func=AF.Copy), [bp5], pwrites=[b_onT])

        (g1b, b_g1b), (lng, b_lng), (lnb, b_lnb) = bcast
        rowb(g1b, b_g1b, modrow[li * NSEQ + s:li * NSEQ + s + 1, 2 * 1024:3 * 1024], reads=[b_modrow])
        rowb(lng, b_lng, ln_g[l, 0:1, :])
        rowb(lnb, b_lnb, ln_b[l, 0:1, :])
        for tile in range(16):
            ts_ = slice(tile * 128, (tile + 1) * 128)
            xtile, b_x = xt[tile % 2]
            ttile, b_t = tt_[tile % 2]
            r0 = s * T + tile * 128
            P.dma("sp", xtile, xsrc[r0:r0 + 128, :], reads=[b_xsrc], writes=[b_x])
            for half in range(2):
                ps, bps = banks[(tile % 2) * 2 + half]
                for k in range(8):
                    lhsT = o_nsaT[:, k, ts_] if k < 4 else o_convT[:, k - 4, ts_]
                    A("pe", lambda e, ps=ps, lhsT=lhsT, k=k, half=half: e.matmul(ps, lhsT=lhsT, rhs=w_out_sb[:, k, half * 512:(half + 1) * 512], start=(k == 0), stop=(k == 7)),
                      [b_onT, b_ocT, b_wout], [bps] if k == 0 else (), pwrites=() if k == 0 else [bps])
                A("dve", lambda e, ps=ps, half=half, ttile=ttile: e.tensor_tensor(out=ttile[:, half * 512:(half + 1) * 512], in0=ps, in1=g1b[:, half * 512:(half + 1) * 512], op=ALU.mult),
                  [bps, b_g1b], [b_t] if half == 0 else (), pwrites=() if half == 0 else [b_t])
            A("dve", lambda e, xtile=xtile, ttile=ttile: e.scalar_tensor_tensor(out=ttile, in0=xtile, scalar=ALPHA, in1=ttile, op0=ALU.mult, op1=ALU.add), [b_x, b_t], [b_t])
            layer_norm_tile(ttile, b_t, lng, b_lng, lnb, b_lnb)
            P.dma("sp", xdst[r0:r0 + 128, :], ttile, reads=[b_t], pwrites=[b_xdst], owner=b_t)

        if cfg.get("dump") and s == 0:
            P.barrier()
            b_dmp = getbuf("dmp")
            items = [QT[:, 0, 0:1024], KsP[0][0][:, 0:1024], KwP[1][0][:, 0:1024], yT[:, 0, 30:1054], o_convT[:, 0, 0:1024], o_nsaT[:, 0, 0:1024],
                     gsb.rearrange("p a b -> p (a b)"), Vs_aug.rearrange("p a b c -> p (a b c)")[:, 0:1024], KcmpT.rearrange("p a b -> p (a b)"),
                     vcmp.rearrange("p a b -> p (a b)"), negselT.rearrange("p a b -> p (a b)"), impacc.rearrange("p a b c -> p (a b c)"),
                     o_nsaT[:, 0, 1024:2048], o_convT[:, 3, 1024:2048], QT[:, 3, 1024:2048]]
            for i, ap_ in enumerate(items):
                w_ = ap_.shape[1]
                P.dma("pool", out[T + i * 128:T + (i + 1) * 128, 0:w_], ap_, pwrites=[b_dmp], owner=b_dmp)
            P.barrier()
    def ffn_phase(li, l, s, xsrc, b_xsrc, xdst, b_xdst):
        moe = (l % 2 == 1)
        R = Arena(PH, SB_END)
        h2T, b_h2T = R.alloc("h2T", [128, 8, T], BF16)
        out_acc, _ = R.alloc("out_acc", [128, 16, D], F32)
        b_acc = [getbuf(f"acc{i}") for i in range(16)]
        w1s = [R.alloc(f"w1s{i}", [128, 8, 2, 256], BF16) for i in range(2)]
        w2s = [R.alloc(f"w2s{i}", [128, 2, D], BF16) for i in range(2)]
        actT = [R.alloc(f"actT{i}", [128, 2, 512], BF16) for i in range(2)]
        sgs = [R.alloc(f"sg{i}", [128, 512], F32) for i in range(2)]
        h2f = [R.alloc(f"h2f{i}", [128, 8, 128], F32) for i in range(2)]
        lg, b_lg = R.alloc("lg", [128, 8], F32)
        ex8, b_ex8 = R.alloc("ex8", [128, 8], F32)
        mk8, b_mk8 = R.alloc("mk8", [128, 8], F32)
        den, b_den = R.alloc("den", [128, 2], F32)
        (g2b, b_g2b), (lng, b_lng), (lnb, b_lnb) = bcast
        rowb(g2b, b_g2b, modrow[li * NSEQ + s:li * NSEQ + s + 1, 5 * 1024:6 * 1024], reads=[b_modrow])
        rowb(lng, b_lng, ln_g[l, 1:2, :])
        rowb(lnb, b_lnb, ln_b[l, 1:2, :])
        if moe:
            P.dma("sp", rw_sb, router_w[l // 2].rearrange("(kc p) e -> p kc e", p=128), writes=[b_rw])
        b_h2Tc = [getbuf(f"h2Tc{i}") for i in range(4)]

        def h2_tiles(tc_):
          for tile in range(tc_ * 4, tc_ * 4 + 4):
              ts_ = slice(tile * 128, (tile + 1) * 128)
              xtile, b_x = xt[tile % 2]
              r0 = s * T + tile * 128
              P.dma("sp", xtile, xsrc[r0:r0 + 128, :], reads=[b_xsrc], writes=[b_x])
              hf_, b_hf = h2f[tile % 2]
              for half in range(2):
                  ps, bps = banks[half]
                  for k4 in range(4):
                      kc = half * 4 + k4
                      A("pe", lambda e, ps=ps, k4=k4, kc=kc, xtile=xtile: e.transpose(out=ps[:, k4 * 128:(k4 + 1) * 128], in_=xtile[:, kc * 128:(kc + 1) * 128], identity=ident_f),
                        [b_x, b_identf], [bps] if k4 == 0 else (), pwrites=() if k4 == 0 else [bps])
                  for k4 in range(4):
                      kc = half * 4 + k4
                      i_ap = ps[:, k4 * 128:(k4 + 1) * 128]
                      o_ap = hf_[:, kc, :] if moe else h2T[:, kc, ts_]
                      bo = b_hf if moe else b_h2Tc[tile // 4]
                      if k4 % 2 == 0:
                          A("dve", lambda e, o_ap=o_ap, i_ap=i_ap, kc=kc: e.tensor_scalar(out=o_ap, in0=i_ap, scalar1=modcol[:, s, 4, kc:kc + 1], scalar2=modcol[:, s, 3, kc:kc + 1],
                                                                                       op0=ALU.mult, op1=ALU.add), [bps, b_modcol], pwrites=[bo])
                      else:
                          A("act", lambda e, o_ap=o_ap, i_ap=i_ap, kc=kc: e.activation(out=o_ap, in_=i_ap, func=AF.Identity, scale=modcol[:, s, 4, kc:kc + 1], bias=modcol[:, s, 3, kc:kc + 1]),
                            [bps, b_modcol], pwrites=[bo])
              if moe:
                  A("act", lambda e, hf_=hf_, ts_=ts_: e.activation(out=h2T[:, :, ts_], in_=hf_, func=AF.Copy), [b_hf], pwrites=[b_h2Tc[tile // 4]])
                  p4, bp4 = banks[4]
                  for kc in range(8):
                      A("pe", lambda e, kc=kc, hf_=hf_: e.matmul(p4[:, 0:8], lhsT=hf_[:, kc, :], rhs=rw_sb[:, kc, :], start=(kc == 0), stop=(kc == 7)), [b_hf, b_rw],
                        [bp4] if kc == 0 else (), pwrites=() if kc == 0 else [bp4])
                  A("dve", lambda e: e.tensor_copy(out=lg, in_=p4[:, 0:8]), [bp4], [b_lg])
                  A("dve", lambda e: e.max(out=sm8[:, 0:8], in_=lg), [b_lg], [b_sm8])
                  A("dve", lambda e: e.tensor_scalar(out=sm8[:, 8:9], in0=sm8[:, 0:1], scalar1=-1.0, scalar2=None, op0=ALU.mult), [b_sm8], [b_sm8])
                  A("act", lambda e: e.activation(out=ex8, in_=lg, func=AF.Exp, bias=sm8[:, 8:9]), [b_lg, b_sm8], [b_ex8])
                  A("dve", lambda e: e.tensor_scalar(out=mk8, in0=lg, scalar1=sm8[:, 1:2], scalar2=None, op0=ALU.is_ge), [b_lg, b_sm8], [b_mk8])
                  A("dve", lambda e: e.tensor_tensor(out=mk8, in0=mk8, in1=ex8, op=ALU.mult), [b_mk8, b_ex8], [b_mk8])
                  A("dve", lambda e: e.reduce_sum(out=den[:, 0:1], in_=mk8, axis=mybir.AxisListType.X), [b_mk8], [b_den])
                  A("dve", lambda e: e.reciprocal(out=den[:, 1:2], in_=den[:, 0:1]), [b_den], [b_den])
                  A("dve", lambda e, tile=tile: e.tensor_scalar(out=we_sb[:, tile, :], in0=mk8, scalar1=den[:, 1:2], scalar2=None, op0=ALU.mult), [b_mk8, b_den], pwrites=[b_we])
        if moe:
            specs = [(("moe1", l, e_), ("moe2", l, e_), D_FFE, e_) for e_ in range(NE)]
        else:
            specs = [(("ffn1", l), ("ffn2", l), D_FF, None)]
        slices = []
        for (k1, k2, F, ex) in specs:
            for f0 in range(0, F, 256):
                slices.append((k1, k2, F, ex, f0))
        loaded = {}

        def load(si):
            if si in loaded or si >= len(slices):
                return
            k1, k2, F, ex, f0 = slices[si]
            W1 = wview(k1, D, 2 * F)
            W2 = wview(k2, F, D)
            w1t, b_w1 = w1s[si % 2]
            w2t, b_w2 = w2s[si % 2]
            P.dma("sp", w1t[:, :, 0, :], W1[:, f0:f0 + 256].rearrange("(kc p) n -> p kc n", p=128), pwrites=[b_w1], **wdep(k1, D * 2 * F))
            P.dma("sp", w1t[:, :, 1, :], W1[:, F + f0:F + f0 + 256].rearrange("(kc p) n -> p kc n", p=128), pwrites=[b_w1], **wdep(k1, D * 2 * F))
            P.dma("sp", w2t, W2[f0:f0 + 256, :].rearrange("(fc p) n -> p fc n", p=128), writes=[b_w2], **wdep(k2, F * D))
            loaded[si] = True

        def gu(si, tc, fc):
            load(si)
            w1t, b_w1 = w1s[si % 2]
            aT, b_aT = actT[(si * 4 + tc) % 2]
            cs = slice(tc * 512, (tc + 1) * 512)
            psG, bpsG = banks[(fc % 2) * 2]
            psU, bpsU = banks[(fc % 2) * 2 + 1]
            sg, b_sg = sgs[fc % 2]
            for gu_, (ps, bps) in enumerate(((psG, bpsG), (psU, bpsU))):
                for kc in range(8):
                    A("pe", lambda e: e.matmul(ps, lhsT=w1t[:, kc, gu_, fc * 128:(fc + 1) * 128], rhs=h2T[:, kc, cs], start=(kc == 0), stop=(kc == 7)),
                      [b_w1, b_h2Tc[tc]], [bps] if kc == 0 else (), pwrites=() if kc == 0 else [bps])
            A("act", lambda e: e.activation(out=sg, in_=psG, func=AF.Silu), [bpsG], [b_sg])
            A("dve", lambda e: e.tensor_tensor(out=aT[:, fc, :], in0=psU, in1=sg, op=ALU.mult), [bpsU, b_sg], [b_aT] if fc == 0 else (), pwrites=() if fc == 0 else [b_aT])

        def outp(si, tc, hq):
            k1, k2, F, ex, f0 = slices[si]
            w2t, b_w2 = w2s[si % 2]
            aT, b_aT = actT[(si * 4 + tc) % 2]
            first = (si == 0)
            for t4 in (2 * hq, 2 * hq + 1):
                tile = tc * 4 + t4
                for half in range(2):
                    psO, bpsO = banks[4 + (t4 * 2 + half) % 4]
                    for fc in range(2):
                        A("pe", lambda e: e.matmul(psO, lhsT=aT[:, fc, t4 * 128:(t4 + 1) * 128], rhs=w2t[:, fc, half * 512:(half + 1) * 512], start=(fc == 0), stop=(fc == 1)),
                          [b_aT, b_w2], [bpsO] if fc == 0 else (), pwrites=() if fc == 0 else [bpsO])
                    o_ap = out_acc[:, tile, half * 512:(half + 1) * 512]
                    if moe:
                        wcol = we_sb[:, tile, ex:ex + 1]
                        if first:
                            A("dve", lambda e: e.tensor_scalar(out=o_ap, in0=psO, scalar1=wcol, scalar2=None, op0=ALU.mult), [bpsO, b_we], pwrites=[b_acc[tile]])
                        else:
                            A("dve", lambda e: e.scalar_tensor_tensor(out=o_ap, in0=psO, scalar=wcol, in1=o_ap, op0=ALU.mult, op1=ALU.add), [bpsO, b_we], pwrites=[b_acc[tile]])
                    else:
                        if first:
                            A("dve", lambda e: e.tensor_copy(out=o_ap, in_=psO), [bpsO], pwrites=[b_acc[tile]])
                        else:
                            A("dve", lambda e: e.tensor_tensor(out=o_ap, in0=psO, in1=o_ap, op=ALU.add), [bpsO], pwrites=[b_acc[tile]])

        units = [(si, tc) for si in range(len(slices)) for tc in range(4)]
        h2_tiles(0)
        gu(units[0][0], units[0][1], 0)
        gu(units[0][0], units[0][1], 1)
        for ui, (si, tc) in enumerate(units):
            if ui + 1 < len(units):
                nsi, ntc = units[ui + 1]
                if nsi == 0:
                    h2_tiles(ntc)
                gu(nsi, ntc, 0)
                outp(si, tc, 0)
                gu(nsi, ntc, 1)
                outp(si, tc, 1)
            else:
                outp(si, tc, 0)
                outp(si, tc, 1)
        for tile in range(16):
            xtile, b_x = xt[tile % 2]
            ttile, b_t = tt_[tile % 2]
            r0 = s * T + tile * 128
            P.dma("sp", xtile, xsrc[r0:r0 + 128, :], reads=[b_xsrc], writes=[b_x])
            A("dve", lambda e, tile=tile, ttile=ttile: e.tensor_tensor(out=ttile, in0=out_acc[:, tile, :], in1=g2b, op=ALU.mult), [b_acc[tile], b_g2b], [b_t])
            A("dve", lambda e, xtile=xtile, ttile=ttile: e.scalar_tensor_tensor(out=ttile, in0=xtile, scalar=ALPHA, in1=ttile, op0=ALU.mult, op1=ALU.add), [b_x, b_t], [b_t])
            layer_norm_tile(ttile, b_t, lng, b_lng, lnb, b_lnb)
            P.dma("sp", xdst[r0:r0 + 128, :], ttile, reads=[b_t], pwrites=[b_xdst], owner=b_t)

    cur, b_cur = x_in, Buf("x_in")
    for li, l in enumerate(layers):
        if cfg.get("stop") == "setup":
            break
        ada_phase(li, l)
        P.barrier()
        if cfg.get("stop") == "ada":
            break
        last = (li == len(layers) - 1)
        dst, b_dst = (out, b_out) if last else (xb, b_xb)
        for s in range(NSEQ):
            if cfg.get("mixer_only"):
                if cfg.get("dump") and s > 0:
                    continue
                mixer_phase(li, l, s, cur, b_cur, dst, b_dst)
                P.barrier()
                continue
            if cfg.get("ffn_only"):
                ffn_phase(li, l, s, cur, b_cur, dst, b_dst)
                P.barrier()
                continue
            mixer_phase(li, l, s, cur, b_cur, xa, b_xa)
            P.barrier()
            ffn_phase(li, l, s, xa, b_xa, dst, b_dst)
            P.barrier()
        cur, b_cur = xb, b_xb
    if cfg.get("stop"):
        A("dve", lambda e: e.memset(tt_[0][0], 0.0), [], [tt_[0][1]])
        if cfg["stop"] == "ada":
            A("dve", lambda e: e.tensor_copy(out=tt_[0][0][:, 0:NSEQ * 48], in_=modcol.rearrange("p s a b -> p (s a b)")), [b_modcol], [tt_[0][1]])
        P.dma("sp", out[0:128, :], tt_[0][0], reads=[tt_[0][1]], pwrites=[b_out], owner=tt_[0][1])
        if cfg["stop"] == "ada":
            P.dma("sp", out[128:128 + NSEQ, :], modrow[0:NSEQ, 0:1024], reads=[b_modrow], pwrites=[b_out], owner=b_modrow)
    P.wait_all("sp", [b_out])
    P.emit()
    return nc, NCH


SMALL = ["cmp_pos", "cmp_w2", "conv_w", "conv_b", "conv_ln_g", "conv_ln_b", "ada_b", "ln_g", "ln_b", "router_w"]


def run(inp, cfg, n_cores, trace=False):
    layers = cfg["layers"]
    NSEQ = cfg["nseq"]
    nc, NCH = build(cfg)
    flat, nch = pack_flat(inp, layers)
    consts = host_consts()
    x = np.asarray(inp["x"], np.float32)
    c = np.asarray(inp["c"], np.float32)
    in_maps = []
    for core in range(n_cores):
        m = {"x": np.ascontiguousarray(x[core * NSEQ:(core + 1) * NSEQ].reshape(NSEQ * T, D)),
             "c": np.ascontiguousarray(c[core * NSEQ:(core + 1) * NSEQ])}
        if cfg["gather"]:
            p = ORDER.index(core)
            m["wsh"] = np.ascontiguousarray(flat[:, p].reshape(nch * 512, 1024))
        else:
            m["wfull"] = flat.reshape(nch * 4096, 1024)
        for k in SMALL:
            m[k] = np.ascontiguousarray(np.asarray(inp[k], np.float32))
        m["rel_bias"] = np.ascontiguousarray(np.asarray(inp["rel_bias"], np.float32).reshape(1, 256))
        m.update(consts)
        in_maps.append(m)
    res = run_bass_kernel_spmd(nc, in_maps, core_ids=list(range(n_cores)), trace=trace)
    outs = [r["out"].reshape(NSEQ, T, D) for r in res.results]
    return np.concatenate(outs, 0), res


def kernel(**inputs):
    cfg = dict(nseq=2, layers=[0, 1, 2, 3], gather=True)
    o, _ = run(inputs, cfg, 8)
    return o.astype(np.float32)
```
